# Optimizing a Trainium2 kernel written in Bass

```python
import jax, jax.numpy as jnp
from jax import lax
import numpy as np

D_MODEL = 1024
BATCH = 2
SEQ = 8192
DEPTH = 1

D_MIX = D_MODEL
GLA_HEADS = 4
GLA_VDIM = D_MIX // 2
GLA_DV = GLA_VDIM // GLA_HEADS
GLA_DK = GLA_DV // 2
GLA_KDIM = GLA_HEADS * GLA_DK
GLA_GATE_RANK = 16
GLA_GATE_NORM = 16.0
GLA_CHUNK = 64
POOL_WIDTH = D_MIX - GLA_VDIM
POOL_WINDOWS = (2, 4, 8, 16)
POOL_GROUPS = 4
POOL_GW = POOL_WIDTH // POOL_GROUPS
IN_SIZES = (GLA_KDIM, GLA_KDIM, GLA_VDIM, GLA_VDIM, POOL_WIDTH, GLA_GATE_RANK)
IN_COLS = int(sum(IN_SIZES))
IN_SPLITS = [int(v) for v in np.cumsum(IN_SIZES)[:-1]]
PEER_HEADS = 8
PEER_NKEYS = 128
PEER_NEXPERTS = PEER_NKEYS * PEER_NKEYS
PEER_HALF = 128
PEER_QDIM = 2 * PEER_HALF
PEER_TOPK = 16
PEER_TOK_BLOCK = 128
EPS = 1e-6

kernel_name = 'hymba_gla_pool_peer_adaln_block'


def rmsnorm(x, gain):
    xf = x.astype(jnp.float32)
    y = xf * lax.rsqrt(jnp.mean(xf * xf, axis=-1, keepdims=True) + EPS) * gain.astype(jnp.float32)
    return y.astype(x.dtype)


def modulate(h, shift, scale):
    return h * (1.0 + scale[:, None, :]) + shift[:, None, :]


def gla_chunked(q, k, v, log_a):
    B, S, H, DK = q.shape
    DV = v.shape[-1]
    C = GLA_CHUNK
    N = S // C
    def to_chunks(t):
        return t.astype(jnp.float32).reshape(B, N, C, H, t.shape[-1]).transpose(0, 3, 1, 2, 4)
    q, k, v, log_a = to_chunks(q), to_chunks(k), to_chunks(v), to_chunks(log_a)
    q = q * (DK ** -0.5)
    b = jnp.cumsum(log_a, axis=3)
    b_last = b[:, :, :, -1:, :]
    q_in = q * jnp.exp(b)
    k_in = k * jnp.exp(-b)
    k_st = k * jnp.exp(b_last - b)
    causal = jnp.tril(jnp.ones((C, C), dtype=bool))
    attn = jnp.where(causal, jnp.einsum('bhnid,bhnjd->bhnij', q_in, k_in), 0.0)
    o_intra = jnp.einsum('bhnij,bhnjv->bhniv', attn, v)
    kv = jnp.einsum('bhncd,bhncv->nbhdv', k_st, v)
    decay = jnp.exp(b_last[:, :, :, 0, :]).transpose(2, 0, 1, 3)
    def step(state, inp):
        kv_n, dec_n = inp
        return state * dec_n[..., None] + kv_n, state
    s0 = jnp.zeros((B, H, DK, DV), jnp.float32)
    _, states = lax.scan(step, s0, (kv, decay))
    o_inter = jnp.einsum('bhncd,nbhdv->bhncv', q_in, states)
    o = o_intra + o_inter
    return o.transpose(0, 2, 3, 1, 4).reshape(B, S, H, DV)


def causal_multiscale_pool(p, pool_w, pool_scale):
    B, S, _ = p.shape
    pg = p.astype(jnp.float32).reshape(B, S, POOL_GROUPS, POOL_GW)
    pos = jnp.arange(S)
    outs = []
    for gi, w in enumerate(POOL_WINDOWS):
        xg = pg[:, :, gi]
        cs = jnp.cumsum(xg, axis=1)
        lag = jnp.pad(cs, ((0, 0), (w, 0), (0, 0)))[:, :S]
        count = jnp.minimum(pos + 1, w).astype(jnp.float32)
        outs.append((cs - lag) / count[None, :, None] - xg)
    pooled = jnp.stack(outs, axis=2)
    y = jnp.einsum('bsgc,gcd->bsgd', pooled, pool_w.astype(jnp.float32)).reshape(B, S, POOL_WIDTH)
    return (y * pool_scale.astype(jnp.float32)).astype(p.dtype)


def token_mixer(h, w_in, gla_gate_w, gla_gate_b, gla_norm_g, pool_w, pool_scale, w_out):
    B, S, _ = h.shape
    proj = h @ w_in
    q, k, v, g, p, a_lr = jnp.split(proj, IN_SPLITS, axis=-1)
    log_a = jax.nn.log_sigmoid((a_lr @ gla_gate_w + gla_gate_b).astype(jnp.float32)) / GLA_GATE_NORM
    o = gla_chunked(q.reshape(B, S, GLA_HEADS, GLA_DK), k.reshape(B, S, GLA_HEADS, GLA_DK),
                    v.reshape(B, S, GLA_HEADS, GLA_DV), log_a.reshape(B, S, GLA_HEADS, GLA_DK))
    o = rmsnorm(o, gla_norm_g).astype(h.dtype) * jax.nn.silu(g.reshape(B, S, GLA_HEADS, GLA_DV))
    y_gla = o.reshape(B, S, GLA_VDIM)
    y_pool = causal_multiscale_pool(p, pool_w, pool_scale).astype(h.dtype)
    return jnp.concatenate([y_gla, y_pool], axis=-1) @ w_out


def peer_ffn(h, w_q, sub_k1, sub_k2, expert_u, expert_v):
    B, S, D = h.shape
    T = B * S
    ht = h.reshape(T, D)
    qp = (ht @ w_q).reshape(T, PEER_HEADS, 2, PEER_HALF)
    s1 = jnp.einsum('thk,nk->thn', qp[:, :, 0], sub_k1).astype(jnp.float32)
    s2 = jnp.einsum('thk,nk->thn', qp[:, :, 1], sub_k2).astype(jnp.float32)
    v1, i1 = lax.top_k(s1, PEER_TOPK)
    v2, i2 = lax.top_k(s2, PEER_TOPK)
    cand_s = (v1[..., :, None] + v2[..., None, :]).reshape(T, PEER_HEADS, PEER_TOPK * PEER_TOPK)
    cand_i = (i1[..., :, None] * PEER_NKEYS + i2[..., None, :]).reshape(T, PEER_HEADS, PEER_TOPK * PEER_TOPK)
    top_s, pos = lax.top_k(cand_s, PEER_TOPK)
    idx = jnp.take_along_axis(cand_i, pos, axis=-1)
    gates = jax.nn.softmax(top_s, axis=-1).astype(h.dtype)
    NBLK = T // PEER_TOK_BLOCK
    E = PEER_HEADS * PEER_TOPK
    hb = ht.reshape(NBLK, PEER_TOK_BLOCK, D)
    ib = idx.reshape(NBLK, PEER_TOK_BLOCK, E)
    gb = gates.reshape(NBLK, PEER_TOK_BLOCK, E)
    def expert_block(args):
        xb, eb, wb = args
        act = jax.nn.gelu(jnp.einsum('td,ted->te', xb, expert_u[eb]), approximate=False) * wb
        return jnp.einsum('te,ted->td', act, expert_v[eb])
    out = lax.map(expert_block, (hb, ib, gb))
    return out.reshape(B, S, D)


def setup_inputs(seed: int = 0) -> dict:
    key = jax.random.key(seed)
    ks = jax.random.split(key, 20)
    f32 = jnp.float32
    nrm = lambda k, shape, s: jax.random.normal(k, shape, f32) * s
    L, D = DEPTH, D_MODEL
    return {
        'x': nrm(ks[0], (BATCH, SEQ, D), 1.0),
        'c': nrm(ks[1], (BATCH, D), 1.0),
        'ada_w': nrm(ks[2], (L, D, 6 * D), 0.5 * D ** -0.5),
        'ada_b': nrm(ks[3], (L, 6 * D), 0.02),
        'norm1_g': 1.0 + nrm(ks[4], (L, D), 0.02),
        'w_in': nrm(ks[5], (L, D, IN_COLS), D ** -0.5),
        'gla_gate_w': nrm(ks[6], (L, GLA_GATE_RANK, GLA_KDIM), GLA_GATE_RANK ** -0.5),
        'gla_gate_b': nrm(ks[7], (L, GLA_KDIM), 0.02),
        'gla_norm_g': 1.0 + nrm(ks[8], (L, GLA_DV), 0.02),
        'pool_w': nrm(ks[9], (L, POOL_GROUPS, POOL_GW, POOL_GW), POOL_GW ** -0.5),
        'pool_scale': 1.0 + nrm(ks[10], (L, POOL_WIDTH), 0.02),
        'w_out': nrm(ks[11], (L, D_MIX, D), D_MIX ** -0.5),
        'norm2_g': 1.0 + nrm(ks[12], (L, D), 0.02),
        'peer_wq': nrm(ks[13], (L, D, PEER_HEADS * PEER_QDIM), D ** -0.5),
        'peer_k1': nrm(ks[14], (L, PEER_NKEYS, PEER_HALF), PEER_HALF ** -0.5),
        'peer_k2': nrm(ks[15], (L, PEER_NKEYS, PEER_HALF), PEER_HALF ** -0.5),
        'peer_u': nrm(ks[16], (L, PEER_NEXPERTS, D), D ** -0.5),
        'peer_v': nrm(ks[17], (L, PEER_NEXPERTS, D), PEER_HEADS ** -0.5),
        'final_g': 1.0 + nrm(ks[18], (D,), 0.02),
    }


def reference(x, c, ada_w, ada_b, norm1_g, w_in, gla_gate_w, gla_gate_b, gla_norm_g, pool_w, pool_scale,
              w_out, norm2_g, peer_wq, peer_k1, peer_k2, peer_u, peer_v, final_g):
    for l in range(DEPTH):
        mod = jax.nn.silu(c) @ ada_w[l] + ada_b[l]
        shift1, scale1, gate1, shift2, scale2, gate2 = jnp.split(mod, 6, axis=-1)
        h = modulate(rmsnorm(x, norm1_g[l]), shift1, scale1)
        mix = token_mixer(h, w_in[l], gla_gate_w[l], gla_gate_b[l], gla_norm_g[l], pool_w[l], pool_scale[l], w_out[l])
        x = x + gate1[:, None, :] * mix
        h = modulate(rmsnorm(x, norm2_g[l]), shift2, scale2)
        ffn = peer_ffn(h, peer_wq[l], peer_k1[l], peer_k2[l], peer_u[l], peer_v[l])
        x = x + gate2[:, None, :] * ffn
    return rmsnorm(x, final_g)
```

```python
import numpy as np
from contextlib import ExitStack
import concourse.bass as bass
import concourse.mybir as mybir
from concourse.bass_utils import run_bass_kernel_spmd

F32 = mybir.dt.float32
BF16 = mybir.dt.bfloat16
U32 = mybir.dt.uint32
I32 = mybir.dt.int32
AF = mybir.ActivationFunctionType
ALU = mybir.AluOpType
AX = mybir.AxisListType

D = 1024
NTOK = 2048
NT = NTOK // 128
NPRE = 48
INC = 2064
EPS = 1e-6
NE = 128
TT = 256
GRP = 2


class Inst:
    __slots__ = ("eng", "fn", "deps", "is_dma", "semkey", "ordinal", "sig", "sem", "idx")


class _Rec:
    def __init__(self):
        self.call = None

    def __getattr__(self, name):
        def f(*a, **k):
            self.call = (name, a, k)
            return None
        return f


class Prog:
    ENG = ("pe", "act", "dve", "pool", "sp")
    LIMIT = 20000

    def __init__(self, nc):
        self.nc = nc
        self.lists = {e: [] for e in self.ENG}
        self.last_w = {}
        self.readers = {}
        self.dma_counts = {}
        self.last_dma = {}

    def barrier(self):
        deps = set()
        for e in self.ENG:
            for ins in reversed(self.lists[e]):
                if not ins.is_dma and ins.fn is not None:
                    deps.add(ins)
                    break
        for last in self.last_dma.values():
            deps.add(last)
        for e in self.ENG:
            ins = Inst()
            ins.eng = e
            ins.fn = None
            ins.is_dma = False
            ins.semkey = None
            ins.sem = None
            ins.ordinal = 0
            ins.sig = False
            ins.deps = set(deps)
            self.lists[e].append(ins)
            ins.idx = len(self.lists[e]) - 1
        self.last_w.clear()
        self.readers.clear()

    def defer(self, eng, fn, r=(), w=(), dma=None):
        if fn is not None:
            rec = _Rec()
            fn(rec)
            name_, a_, k_ = rec.call
            fn = (lambda e, name_=name_, a_=a_, k_=k_: getattr(e, name_)(*a_, **k_))
        r = list(r)
        w = list(w)
        return lambda: self.add(eng, fn, r, w, dma, _bound=True)

    def add(self, eng, fn, r=(), w=(), dma=None, _bound=False):
        ins = Inst()
        ins.eng = eng
        if fn is not None and not _bound:
            rec = _Rec()
            fn(rec)
            name_, a_, k_ = rec.call
            fn = (lambda e, name_=name_, a_=a_, k_=k_: getattr(e, name_)(*a_, **k_))
        ins.fn = fn
        ins.is_dma = dma is not None
        ins.semkey = dma
        ins.sem = None
        ins.ordinal = 0
        deps = set()
        for k in r:
            lw = self.last_w.get(k)
            if lw is not None:
                deps.add(lw)
        for k in w:
            lw = self.last_w.get(k)
            if lw is not None:
                deps.add(lw)
            for rd in self.readers.get(k, ()):
                deps.add(rd)
        ins.deps = deps
        for k in r:
            self.readers.setdefault(k, []).append(ins)
        for k in w:
            self.last_w[k] = ins
            self.readers[k] = []
        if dma is not None:
            c = self.dma_counts.get(dma, 0) + 1
            self.dma_counts[dma] = c
            ins.ordinal = c
            self.last_dma[dma] = ins
        ins.sig = False
        self.lists[eng].append(ins)
        ins.idx = len(self.lists[eng]) - 1
        return ins

    def emit(self, stack):
        nc = self.nc
        for e in self.ENG:
            for ins in self.lists[e]:
                for d in ins.deps:
                    if d.is_dma:
                        continue
                    if d.eng == ins.eng == "pe":
                        continue
                    d.sig = True
        semkeys = []
        for e in self.ENG:
            cnt = 0
            epoch = 0
            for ins in self.lists[e]:
                if ins.is_dma or not ins.sig:
                    continue
                cnt += 1
                if cnt > self.LIMIT:
                    epoch += 1
                    cnt = 1
                ins.sem = (e, epoch)
                ins.ordinal = cnt
                if ins.sem not in semkeys:
                    semkeys.append(ins.sem)
        for k in self.dma_counts:
            semkeys.append(("dma", k))
        self.sems = {}
        for i, k in enumerate(semkeys):
            self.sems[k] = stack.enter_context(nc.semaphore("s%d" % i))
        block = stack.enter_context(nc.Block())

        def run(engname, eng):
            waited = {}
            for ins in self.lists[engname]:
                need = {}
                for d in ins.deps:
                    if d.is_dma:
                        key = ("dma", d.semkey)
                        val = 16 * d.ordinal
                    else:
                        if d.eng == engname == "pe":
                            continue
                        key = d.sem
                        val = d.ordinal
                    if need.get(key, 0) < val:
                        need[key] = val
                for key, val in need.items():
                    if waited.get(key, 0) >= val:
                        continue
                    eng.wait_ge(self.sems[key], val)
                    waited[key] = val
                if ins.fn is None:
                    continue
                bi = ins.fn(eng)
                if ins.is_dma:
                    bi.then_inc(self.sems[("dma", ins.semkey)], 16)
                elif ins.sig:
                    bi.then_inc(self.sems[ins.sem], 1)

        @block.tensor
        def _(e):
            run("pe", e)

        @block.scalar
        def _(e):
            run("act", e)

        @block.vector
        def _(e):
            run("dve", e)

        @block.gpsimd
        def _(e):
            run("pool", e)

        @block.sync
        def _(e):
            run("sp", e)


C_ID, C_IOTA, C_TRIL, C_TRIU, C_CAUS, C_ONES, C_NEG, C_IOTA16, C_BA, C_BB = (
    0, 128, 256, 384, 512, 640, 768, 769, 785, 785 + 512)
C_N = 785 + 1024


def make_consts():
    c = np.zeros((128, C_N), np.float32)
    s = np.arange(128)[:, None]
    t = np.arange(128)[None, :]
    c[:, C_ID:C_ID + 128] = (s == t)
    c[:, C_IOTA:C_IOTA + 128] = t
    c[:, C_TRIL:C_TRIL + 128] = np.where(s <= t, -1.0 / 16, 0.0)
    c[:, C_TRIU:C_TRIU + 128] = np.where(s > t, -1.0 / 16, 0.0)
    c[:, C_CAUS:C_CAUS + 128] = (s <= t)
    c[:, C_ONES:C_ONES + 128] = 1.0
    c[:, C_NEG] = -1.0 / 16
    c[:, C_IOTA16:C_IOTA16 + 16] = np.arange(16)[None, :]
    for g, w in enumerate((2, 4, 8, 16)):
        d = t - s
        c[:, C_BA + g * 128:C_BA + (g + 1) * 128] = np.where((d >= 0) & (d < w), 1.0 / w, 0.0) - (s == t)
        d2 = t + 128 - s
        c[:, C_BB + g * 128:C_BB + (g + 1) * 128] = np.where((d2 >= 0) & (d2 < w), 1.0 / w, 0.0)
    return c


def make_core_consts(j):
    c = np.zeros((128, 512 + NPRE), np.float32)
    s = np.arange(128)[:, None]
    t = np.arange(128)[None, :]
    for g, w in enumerate((2, 4, 8, 16)):
        d = t - s
        if j == 0:
            cnt = np.minimum(t + 1, w).astype(np.float32)
        else:
            cnt = np.full_like(t, w).astype(np.float32)
        c[:, g * 128:(g + 1) * 128] = np.where((d >= 0) & (d < w), 1.0 / cnt, 0.0) - (s == t)
    pos = NTOK * j - NPRE * 128 + np.arange(NPRE * 128)
    c[:, 512:] = (pos >= 0).astype(np.float32).reshape(NPRE, 128).T
    return c


def build_nc(phases=("mod", "prep", "sub1", "route", "dense"), dbg=False):
    nc = bass.Bass("TRN2", target_bir_lowering=False)
    din = lambda n, s, d=F32: nc.dram_tensor(n, s, d, kind="ExternalInput").ap()
    x_own = din("x_own", [NTOK, D])
    x_pre = din("x_pre", [NPRE * 128, D])
    cst = din("cst", [128, C_N])
    cst2 = din("cst2", [128, 512 + NPRE])
    c_col = din("c_col", [128, 8])
    ada_w = din("ada_w", [D, 6 * D])
    ada_bT = din("ada_bT", [128, 48])
    ada_b = din("ada_b", [1, 6 * D])
    n1g = din("n1g", [128, 8])
    n2g = din("n2g", [128, 8])
    fg_rep = din("fg_rep", [128, D])
    w_in = din("w_in", [D, INC])
    gw17 = din("gw17", [17, 256])
    gng_rep = din("gng_rep", [128, 128])
    pool_w = din("pool_w", [4, 128, 128])
    pscT = din("pscT", [128, 4])
    w_out = din("w_out", [D, D])
    wq = din("wq", [D, 2048])
    k1 = din("k1", [128, 128])
    k2 = din("k2", [128, 128])
    pu = din("pu", [NE * 128, D])
    pv = din("pv", [NE * 128, D])
    out = nc.dram_tensor("out", [NTOK, D], F32, kind="ExternalOutput").ap()
    skind = "ExternalOutput" if dbg else "Internal"
    UTs = nc.dram_tensor("UTs", [NE, 128, 1024], BF16, kind=skind).ap()
    Vbs = nc.dram_tensor("Vbs", [NE, 128, 1024], BF16, kind=skind).ap()
    x1s = nc.dram_tensor("x1s", [NTOK, D], F32).ap()
    h2s = nc.dram_tensor("h2s", [NT, 128, 1024], BF16).ap()
    dbg_out = {}
    if dbg:
        for n, s in (("d_mod", [128, 32]), ("d_x1", [NTOK, D]), ("d_h2", [NT, 128, 1024]),
                     ("d_rt", [3, 128, NTOK]), ("d_g1", [128, D]), ("d_misc", [128, 2048]), ("d_qp", [128, 2048])):
            dbg_out[n] = nc.dram_tensor(n, s, BF16 if n in ("d_h2", "d_qp") else F32, kind="ExternalOutput").ap()

    with ExitStack() as st:
        def sb(n, cols, d=F32, parts=128):
            return st.enter_context(nc.sbuf_tensor(n, [parts, cols], d))
        P = Prog(nc)
        cap = [None]

        def A(eng, fn, r=(), w=(), dma=None):
            if cap[0] is not None:
                cap[0].append(P.defer(eng, fn, r, w, dma))
                return None
            return P.add(eng, fn, r, w, dma)
        banks = [st.enter_context(nc.psum_tensor("ps%d" % i, [128, 512], F32)) for i in range(8)]

        def bk(i):
            return banks[i][:]

        def bkb(i):
            return banks[i][:].bitcast(BF16)

        cs = sb("cs", C_N)
        cs2 = sb("cs2", 512 + NPRE)
        identb = sb("identb", 128, BF16)
        iotab = sb("iotab", 128, BF16)
        modT = sb("modT", 32)
        gs1 = sb("gs1", 8)
        gs2 = sb("gs2", 8)
        gate1 = sb("gate1", D)
        gate2 = sb("gate2", D)
        fgr = sb("fgr", D)
        gngr = sb("gngr", 128)
        pscs = sb("pscs", 4)
        n1s = sb("n1s", 8)
        n2s = sb("n2s", 8)
        epsc = sb("epsc", 1)
        print("sbuf bytes remaining after persistent:", nc.sbuf_bytes_remaining)
        A("sp", lambda e: e.dma_start(out=cs[:], in_=cst), w=["cs"], dma="c0_1")
        A("sp", lambda e: e.dma_start(out=cs2[:], in_=cst2), w=["cs2"], dma="c0_2")
        A("sp", lambda e: e.dma_start(out=fgr[:], in_=fg_rep), w=["fgr"], dma="c0_3")
        A("sp", lambda e: e.dma_start(out=gngr[:], in_=gng_rep), w=["gngr"], dma="c0_4")
        A("sp", lambda e: e.dma_start(out=pscs[:], in_=pscT), w=["pscs"], dma="c0_5")
        A("sp", lambda e: e.dma_start(out=n1s[:], in_=n1g), w=["n1s"], dma="c0_6")
        A("sp", lambda e: e.dma_start(out=n2s[:], in_=n2g), w=["n2s"], dma="c0_7")
        ident = cs[:, C_ID:C_ID + 128]
        A("dve", lambda e: e.memset(epsc[:], EPS), w=["epsc"])
        A("dve", lambda e: e.tensor_copy(out=identb[:], in_=ident), r=["cs"], w=["identb"])
        A("dve", lambda e: e.tensor_copy(out=iotab[:], in_=cs[:, C_IOTA:C_IOTA + 128]), r=["cs"], w=["iotab"])

        ARENA = NE * TT
        arena = sb("arena", ARENA, BF16)
        TMPF = 26624
        tmp = st.enter_context(nc.sbuf_tensor("tmp", [128, TMPF], F32))
        tmp_views = {F32: tmp, BF16: tmp[:].bitcast(BF16), U32: tmp[:].bitcast(U32), I32: tmp[:].bitcast(I32)}
        ta_off = [0]

        def ta(n, cols, d=F32, parts=128):
            words = (cols + 1) // 2 if d == BF16 else cols
            o = ta_off[0]
            ta_off[0] += words
            assert ta_off[0] <= TMPF, (n, ta_off[0])
            if d == BF16:
                return tmp_views[BF16][0:parts, 2 * o:2 * o + cols]
            return tmp_views[d][0:parts, o:o + cols]

        def phase_start():
            P.barrier()
            ta_off[0] = 0

        def make_prep(pbank=None, deng="pool"):
            ust = [ta("ust%d" % i, 1024) for i in range(2)]
            vst = [ta("vst%d" % i, 1024) for i in range(2)]
            ub = [ta("ub%d" % i, 1024, BF16) for i in range(2)]
            utb = [ta("utb%d" % i, 1024, BF16) for i in range(2)]
            vb = [ta("vb%d" % i, 1024, BF16) for i in range(2)]

            def prep_load(i):
                q = i % 2
                A(deng, lambda e: e.dma_start(out=ust[q][:], in_=pu[i * 128:(i + 1) * 128, :]), w=["ust%d" % q], dma="ust%d" % q)
                A(deng, lambda e: e.dma_start(out=vst[q][:], in_=pv[i * 128:(i + 1) * 128, :]), w=["vst%d" % q], dma="vst%d" % q)

            def prep_tile(i):
                q = i % 2
                if i == 0:
                    prep_load(0)
                if i + 1 < NE:
                    prep_load(i + 1)
                A("act", lambda e: e.copy(out=ub[q][:], in_=ust[q][:]), r=["ust%d" % q], w=["ub%d" % q])
                bn_ = (4 + q) if pbank is None else pbank
                pb = "b%d" % bn_
                for c in range(8):
                    A("pe", lambda e: e.transpose(out=bkb(bn_)[:, c * 128:(c + 1) * 128], in_=ub[q][:, c * 128:(c + 1) * 128], identity=identb[:]),
                      r=["ub%d" % q, "identb"], w=[pb])
                A("act", lambda e: e.copy(out=utb[q][:], in_=bkb(bn_)), r=[pb], w=["utb%d" % q])
                A("act", lambda e: e.copy(out=vb[q][:], in_=vst[q][:]), r=["vst%d" % q], w=["vb%d" % q])
                A(deng, lambda e: e.dma_start(out=UTs[i], in_=utb[q][:]), r=["utb%d" % q], w=["UTs"], dma="sut%d" % q)
                A(deng, lambda e: e.dma_start(out=Vbs[i], in_=vb[q][:]), r=["vb%d" % q], w=["Vbs"], dma="svb%d" % q)
            return prep_tile

        if phases == ("prep",):
            phase_start()
            prep_tile = make_prep()
            for i in range(NE):
                prep_tile(i)

        winb = arena[:, 0:8 * INC].rearrange("p (c n) -> p c n", c=8)
        woutb = arena[:, 8 * INC:8 * INC + 8192].rearrange("p (c n) -> p c n", c=8)
        if "mod" in phases:
            phase_start()
            stage = ta("stage", 4096)
            ccol = ta("ccol", 8)
            sc = ta("sc", 8)
            screp = ta("screp", 1024)
            abT = ta("abT", 48)
            abrow = ta("abrow", 2048, parts=1)
            A("sp", lambda e: e.dma_start(out=ccol[:], in_=c_col), w=["ccol"], dma="c1_8")
            A("sp", lambda e: e.dma_start(out=abT[:], in_=ada_bT), w=["abT"], dma="c1_9")
            A("sp", lambda e: e.dma_start(out=abrow[:, 0:1024], in_=ada_b[:, 2048:3072]), w=["abrow"], dma="c1_10")
            A("sp", lambda e: e.dma_start(out=abrow[:, 1024:2048], in_=ada_b[:, 5120:6144]), w=["abrow"], dma="c1_11")
            A("act", lambda e: e.activation(out=sc[:], in_=ccol[:], func=AF.Silu), r=["ccol"], w=["sc"])
            A("dve", lambda e: e.tensor_copy(out=screp[:].rearrange("p (c m) -> p c m", c=8),
                                             in_=sc[:].unsqueeze(2).to_broadcast([128, 8, 128])), r=["sc"], w=["screp"])
            stageB = ta("stageB", 4096)
            wsts = [stage[:].rearrange("p (c n) -> p c n", c=8), stageB[:].rearrange("p (c n) -> p c n", c=8)]
            adv = ada_w.rearrange("(c p) n -> p c n", p=128)
            ones_row = cs[0:1, C_ONES:C_ONES + 128]
            wstg = [ta("wstg%d" % i, INC) for i in range(2)]
            wiv = w_in.rearrange("(c p) n -> p c n", p=128)
            wov = w_out.rearrange("(c p) n -> p c n", p=128)
            wchunks = [("in", c) for c in range(8)] + [("out", c) for c in range(8)]
            wci = [0]

            def stage_weight():
                if wci[0] >= len(wchunks):
                    return
                kind_, c_ = wchunks[wci[0]]
                k_ = wci[0] % 2
                wci[0] += 1
                if kind_ == "in":
                    A("sp", lambda e: e.dma_start(out=wstg[k_][:, 0:INC], in_=wiv[:, c_, :]), w=["wstg%d" % k_], dma="wstg%d" % k_)
                    A("dve", lambda e: e.tensor_copy(out=winb[:, c_, :], in_=wstg[k_][:, 0:INC]), r=["wstg%d" % k_], w=["winb"])
                else:
                    A("sp", lambda e: e.dma_start(out=wstg[k_][:, 0:1024], in_=wov[:, c_, :]), w=["wstg%d" % k_], dma="wstg%d" % k_)
                    A("dve", lambda e: e.tensor_copy(out=woutb[:, c_, :], in_=wstg[k_][:, 0:1024]), r=["wstg%d" % k_], w=["woutb"])

            def load_blk(blk):
                A("sp", lambda e: e.dma_start(out=wsts[blk % 2], in_=adv[:, :, blk * 512:(blk + 1) * 512]), w=["stage%d" % (blk % 2)], dma="stage%d" % (blk % 2))
            load_blk(0)
            for blk in range(12):
                if blk + 1 < 12:
                    load_blk(blk + 1)
                wst = wsts[blk % 2]
                sres = "stage%d" % (blk % 2)
                stage_weight()
                if blk % 3 != 2:
                    stage_weight()
                kind = blk // 2
                if kind in (2, 5):
                    gt = gate1 if kind == 2 else gate2
                    half = blk % 2
                    for c in range(8):
                        A("pe", lambda e, c=c: e.matmul(bk(2), lhsT=screp[:, c * 128:(c + 1) * 128], rhs=wst[:, c, :], start=(c == 0), stop=False),
                          r=["screp", sres], w=["b2"])
                    A("pe", lambda e, blk=blk: e.matmul(bk(2), lhsT=ones_row, rhs=abrow[0:1, ((blk - 4) if blk < 6 else (blk - 8)) * 512:((blk - 4) if blk < 6 else (blk - 8)) * 512 + 512], start=False, stop=True),
                      r=["cs", "abrow"], w=["b2"])
                    A("dve", lambda e, gt=gt, half=half: e.tensor_copy(out=gt[:, half * 512:(half + 1) * 512], in_=bk(2)), r=["b2"], w=["gate%d" % (1 if kind == 2 else 2)])
                else:
                    qi = {0: 0, 1: 1, 3: 2, 4: 3}[kind]
                    for nchunk in range(4):
                        col = qi * 8 + (blk % 2) * 4 + nchunk
                        for c in range(8):
                            A("pe", lambda e, c=c, nchunk=nchunk, col=col: e.matmul(bk(3)[:, col:col + 1], lhsT=wst[:, c, nchunk * 128:(nchunk + 1) * 128],
                                                                                    rhs=sc[:, c:c + 1], start=(c == 0), stop=(c == 7)),
                              r=[sres, "sc"], w=["b3"])
            while wci[0] < len(wchunks):
                stage_weight()
            for qi, bcol in enumerate((0, 8, 24, 32)):
                A("dve", lambda e, qi=qi, bcol=bcol: e.tensor_tensor(out=modT[:, qi * 8:(qi + 1) * 8], in0=bk(3)[:, qi * 8:(qi + 1) * 8],
                                                                     in1=abT[:, bcol:bcol + 8], op=ALU.add), r=["b3", "abT"], w=["modT"])
            A("dve", lambda e: e.scalar_tensor_tensor(out=gs1[:], in0=modT[:, 8:16], scalar=1.0, in1=n1s[:], op0=ALU.add, op1=ALU.mult),
              r=["modT", "n1s"], w=["gs1"])
            A("dve", lambda e: e.scalar_tensor_tensor(out=gs2[:], in0=modT[:, 24:32], scalar=1.0, in1=n2s[:], op0=ALU.add, op1=ALU.mult),
              r=["modT", "n2s"], w=["gs2"])
            if dbg:
                A("sp", lambda e: e.dma_start(out=dbg_out["d_mod"], in_=modT[:]), r=["modT"], w=["d_mod"], dma="dbg")
                A("sp", lambda e: e.dma_start(out=dbg_out["d_g1"], in_=gate1[:]), r=["gate1"], w=["d_g1"], dma="dbg")
        sh1 = modT[:, 0:8]
        sh2 = modT[:, 16:24]

        if "sub1" in phases:
            phase_start()
            gw = ta("gw", 256, parts=17)
            A("sp", lambda e: e.dma_start(out=gw[:], in_=gw17), w=["gw"], dma="c2_12")
            pwst = ta("pwst", 512)
            pwb = ta("pwb", 512, BF16)
            A("sp", lambda e: e.dma_start(out=pwst[:].rearrange("p (g d) -> p g d", g=4), in_=pool_w.rearrange("g c d -> c g d")), w=["pwst"], dma="c2_13")
            A("dve", lambda e: e.tensor_copy(out=pwb[:], in_=pwst[:]), r=["pwst"], w=["pwb"])
            state = ta("state", 512, parts=64)
            stateb = ta("stateb", 512, BF16, parts=64)
            A("dve", lambda e: e.memset(state[:], 0.0), w=["state"])
            A("dve", lambda e: e.memset(stateb[:], 0.0), w=["stateb"])
            xt2 = [ta("xt2_%d" % i, 2048) for i in range(2)]
            xt = [xt2[i][:, 0:1024] for i in range(2)]
            junk = ta("junk", 1024, BF16)
            ssq = ta("ssq", 8)
            pcur = [ta("pcur%d" % i, 512) for i in range(2)]
            prep_tile = None
            ta_mark = ta_off[0]
            xnbp2 = [ta("xnbp%d" % i, 2048, BF16) for i in range(2)]
            hTp2 = [ta("hTp%d" % i, 2048, BF16) for i in range(2)]
            ssqp2 = [ta("ssqp%d" % i, 8) for i in range(2)]
            sptp = ta("sptp", 512)
            ectp = ta("ectp", 512)
            kstp = ta("kstp", 512, BF16)
            vbfp = ta("vbfp", 1024, BF16)
            decp = ta("decp", 8, parts=64)
            alrp = ta("alrp", 256, parts=17)
            ssqp = ta("ssqp", 8)
            A("dve", lambda e: e.memset(alrp[:], 1.0), w=["alrp"])

            def load_x2(sidx, q):
                A("sp", lambda e: e.dma_start(out=xt2[q][:].rearrange("p (u d) -> p u d", u=2),
                                              in_=x_pre[sidx * 256:(sidx + 1) * 256, :].rearrange("(u p) d -> p u d", p=128)), w=["xt%d" % q], dma="xt%d" % q)

            def prefix_A(sidx, q):
                xres = "xt%d" % q
                xv = xt2[q][:].rearrange("p (u d) -> p u d", u=2)
                xn_, hT_, sq_ = xnbp2[q], hTp2[q], ssqp2[q]
                for u in range(2):
                    A("act", lambda e: e.activation(out=junk[:], in_=xv[:, u, :], func=AF.Square, accum_out=sq_[:, u:u + 1]), r=[xres], w=["junk", "ssqp%d" % q])
                A("act", lambda e: e.activation(out=sq_[:, 2:4], in_=sq_[:, 0:2], func=AF.Ln, scale=1.0 / D, bias=epsc[:, 0:1]), r=["ssqp%d" % q, "epsc"], w=["ssqp1%d" % q])
                A("act", lambda e: e.activation(out=sq_[:, 4:6], in_=sq_[:, 2:4], func=AF.Exp, scale=-0.5), r=["ssqp1%d" % q], w=["ssqp2%d" % q])
                for u in range(2):
                    A("act", lambda e: e.activation(out=xn_[:, u * 1024:(u + 1) * 1024], in_=xv[:, u, :], func=AF.Copy, scale=sq_[:, 4 + u:5 + u]), r=[xres, "ssqp2%d" % q], w=["xnbp%d" % q])
                for u in range(2):
                    for c in range(8):
                        b = c // 4
                        po = ((c % 4) * 2 + u) * 128
                        A("pe", lambda e: e.transpose(out=bkb(b)[:, po:po + 128], in_=xn_[:, u * 1024 + c * 128:u * 1024 + (c + 1) * 128], identity=identb[:]),
                          r=["xnbp%d" % q, "identb"], w=["b%d" % b])
                for c in range(8):
                    b = c // 4
                    A("dve", lambda e: e.tensor_scalar(out=hT_[:, c * 256:(c + 1) * 256], in0=bkb(b)[:, (c % 4) * 256:(c % 4 + 1) * 256],
                                                       scalar1=gs1[:, c:c + 1], scalar2=sh1[:, c:c + 1], op0=ALU.mult, op1=ALU.add),
                      r=["b%d" % b, "gs1", "modT"], w=["hTp%d" % q])

            def prefix_B(sidx, q):
                hT_ = hTp2[q]
                hres = "hTp%d" % q
                last = sidx == NPRE // 2 - 1
                for u in range(2):
                    for c in range(8):
                        A("pe", lambda e: e.matmul(bk(2)[:, u * 256:(u + 1) * 256], lhsT=hT_[:, (c * 2 + u) * 128:(c * 2 + u + 1) * 128], rhs=winb[:, c, 256:512], start=(c == 0), stop=(c == 7)),
                          r=[hres, "winb"], w=["b2"])
                    for c in range(8):
                        A("pe", lambda e: e.matmul(bk(4 + u), lhsT=hT_[:, (c * 2 + u) * 128:(c * 2 + u + 1) * 128], rhs=winb[:, c, 512:1024], start=(c == 0), stop=(c == 7)),
                          r=[hres, "winb"], w=["b%d" % (4 + u)])
                for c in range(8):
                    A("pe", lambda e: e.matmul(bk(3)[0:16, 0:256], lhsT=winb[:, c, 2048:2064], rhs=hT_[:, c * 256:(c + 1) * 256], start=(c == 0), stop=(c == 7)),
                      r=[hres, "winb"], w=["b3"])
                A("act", lambda e: e.copy(out=alrp[0:16, :], in_=bk(3)[0:16, 0:256]), r=["b3"], w=["alrp"])
                for u in range(2):
                    A("pe", lambda e: e.matmul(bk(3)[:, u * 256:(u + 1) * 256], lhsT=alrp[:, u * 128:(u + 1) * 128], rhs=gw[:], start=True, stop=True), r=["alrp", "gw"], w=["b3"])
                A("act", lambda e: e.activation(out=ectp[:], in_=bk(3), func=AF.Exp, scale=-1.0), r=["b3"], w=["ectp"])
                A("act", lambda e: e.activation(out=sptp[:], in_=ectp[:], func=AF.Ln, bias=1.0), r=["ectp"], w=["sptp"])
                for u in range(2):
                    A("pe", lambda e: e.matmul(bk(6)[:, u * 256:(u + 1) * 256], lhsT=cs[:, C_TRIU:C_TRIU + 128], rhs=sptp[:, u * 256:(u + 1) * 256], start=True, stop=True), r=["cs", "sptp"], w=["b6"])
                    for h in range(4):
                        A("pe", lambda e: e.matmul(bk(3)[0:64, u * 4 + h:u * 4 + h + 1], lhsT=sptp[:, u * 256 + h * 64:u * 256 + (h + 1) * 64], rhs=cs[:, C_NEG:C_NEG + 1], start=True, stop=True),
                          r=["cs", "sptp"], w=["b3"])
                A("act", lambda e: e.activation(out=ectp[:], in_=bk(6), func=AF.Exp), r=["b6"], w=["ectp"])
                A("dve", lambda e: e.tensor_tensor(out=kstp[:], in0=bk(2), in1=ectp[:], op=ALU.mult), r=["b2", "ectp"], w=["kstp"])
                A("act", lambda e: e.activation(out=decp[:], in_=bk(3)[0:64, 0:8], func=AF.Exp), r=["b3"], w=["decp"])
                for u in range(2):
                    ti = 2 * sidx + u
                    A("act", lambda e: e.activation(out=vbfp[:, u * 512:(u + 1) * 512], in_=bk(4 + u), func=AF.Copy, scale=cs2[:, 512 + ti:513 + ti]), r=["b%d" % (4 + u), "cs2"], w=["vbfp"])
                if last:
                    for c in range(8):
                        A("pe", lambda e: e.matmul(bk(7), lhsT=hT_[:, (c * 2 + 1) * 128:(c * 2 + 2) * 128], rhs=winb[:, c, 1536:2048], start=(c == 0), stop=(c == 7)),
                          r=[hres, "winb"], w=["b7"])
                    A("act", lambda e: e.activation(out=pcur[1][:], in_=bk(7), func=AF.Copy, scale=cs2[:, 512 + NPRE - 1:512 + NPRE]), r=["b7", "cs2"], w=["pcur1"])
                for u in range(2):
                    for h in range(4):
                        A("pe", lambda e: e.matmul(bk(4 + u)[0:64, h * 128:(h + 1) * 128], lhsT=kstp[:, u * 256 + h * 64:u * 256 + (h + 1) * 64], rhs=vbfp[:, u * 512 + h * 128:u * 512 + (h + 1) * 128], start=True, stop=True),
                          r=["kstp", "vbfp"], w=["b%d" % (4 + u)])
                st3 = state[:].rearrange("p (h v) -> p h v", h=4)
                for u in range(2):
                    A("dve", lambda e: e.tensor_tensor(out=st3, in0=st3, in1=decp[:, u * 4:(u + 1) * 4].unsqueeze(2).to_broadcast([64, 4, 128]), op=ALU.mult), r=["state", "decp"], w=["state"])
                    A("dve", lambda e: e.tensor_tensor(out=state[:], in0=state[:], in1=bk(4 + u)[0:64, :], op=ALU.add), r=["state", "b%d" % (4 + u)], w=["state"])
                if last:
                    A("act", lambda e: e.copy(out=stateb[:], in_=state[:]), r=["state"], w=["stateb"])

            NS = NPRE // 2
            load_x2(0, 0)
            load_x2(1, 1)
            prefix_A(0, 0)
            for sidx in range(NS):
                if sidx + 1 < NS:
                    prefix_A(sidx + 1, (sidx + 1) % 2)
                if sidx + 2 < NS:
                    load_x2(sidx + 2, sidx % 2)
                elif sidx + 2 == NS:
                    A("sp", lambda e: e.dma_start(out=xt[0], in_=x_own[0:128, :]), w=["xt0"], dma="xt0")
                prefix_B(sidx, sidx % 2)
            P.barrier()
            ta_off[0] = ta_mark
            alr = ta("alr", 128, parts=17)
            A("dve", lambda e: e.memset(alr[:], 1.0), w=["alr"])
            xnb = ta("xnb", 1024, BF16)
            hT = ta("hT", 1024, BF16)
            spt = ta("spt", 256)
            ect = ta("ect", 256)
            kst = ta("kst", 256, BF16)
            vbf = ta("vbf", 512, BF16)
            dec = ta("dec", 4, parts=64)
            ebt = ta("ebt", 512, parts=64)
            enbt = ta("enbt", 512, parts=64)
            qin = ta("qin", 512, BF16, parts=64)
            kin = ta("kin", 512, BF16, parts=64)
            attn = ta("attn", 512, BF16)
            sg = ta("sg", 512)
            yg = ta("yg", 512)
            ygb = ta("ygb", 512, BF16)
            yglaT = ta("yglaT", 512, BF16)
            poolT = ta("poolT", 512, BF16)
            ypoolT = ta("ypoolT", 512, BF16)
            x1t = ta("x1t", 1024)
            h2T = ta("h2T", 1024, BF16)
            ssqo = ta("ssqo", 8)

            def load_x(kind, i, q):
                src = x_pre if kind == "pre" else x_own
                A("sp", lambda e: e.dma_start(out=xt[q][:], in_=src[i * 128:(i + 1) * 128, :]), w=["xt%d" % q], dma="xt%d" % q)

            def norm_T(src_ap, src_res, gs, sh, dst, dst_res, bank):
                pb = "b%d" % bank
                A("act", lambda e: e.activation(out=junk[:], in_=src_ap, func=AF.Square, accum_out=ssq[:, 0:1]), r=[src_res], w=["junk", "ssq"])
                A("act", lambda e: e.activation(out=ssq[:, 1:2], in_=ssq[:, 0:1], func=AF.Ln, scale=1.0 / D, bias=epsc[:, 0:1]), r=["ssq", "epsc"], w=["ssq1"])
                A("act", lambda e: e.activation(out=ssq[:, 2:3], in_=ssq[:, 1:2], func=AF.Exp, scale=-0.5), r=["ssq1"], w=["ssq2"])
                A("act", lambda e: e.activation(out=xnb[:], in_=src_ap, func=AF.Copy, scale=ssq[:, 2:3]), r=[src_res, "ssq2"], w=["xnb"])
                for c in range(8):
                    A("pe", lambda e, c=c: e.transpose(out=bkb(bank)[:, c * 128:(c + 1) * 128], in_=xnb[:, c * 128:(c + 1) * 128], identity=identb[:]),
                      r=["xnb", "identb"], w=[pb])
                for c in range(8):
                    eng = "dve" if c % 2 == 0 else "pool"
                    eng = "dve"
                    A(eng, lambda e, c=c: e.tensor_scalar(out=dst[:, c * 128:(c + 1) * 128], in0=bkb(bank)[:, c * 128:(c + 1) * 128],
                                                          scalar1=gs[:, c:c + 1], scalar2=sh[:, c:c + 1], op0=ALU.mult, op1=ALU.add),
                      r=[pb, "gs1", "gs2", "modT"], w=[dst_res])

            def sub1_tile(kind, i, gidx):
                own = kind == "own"
                q = gidx % 2
                last_pre = (kind == "pre" and i == NPRE - 1)
                if own:
                    B = dict(T=0, K=1, V=2, G=3, Pp=4, Q=5, KT=6, M=7)
                else:
                    o = 0
                    B = dict(T=o, K=o + 1, V=o + 2, M=o + 3, Pp=6)
                bn = {k: "b%d" % v for k, v in B.items()}
                xres = "xt%d" % q
                norm_T(xt[q][:], xres, gs1, sh1, hT, "hT", B["T"])
                hTv = hT[:].rearrange("p (c t) -> p c t", c=8)
                def proj(bank, c0, c1, o0=0):
                    for c in range(8):
                        A("pe", lambda e, c=c: e.matmul(bk(bank)[:, o0:o0 + (c1 - c0)], lhsT=hTv[:, c, :], rhs=winb[:, c, c0:c1], start=(c == 0), stop=(c == 7)),
                          r=["hT", "winb"], w=["b%d" % bank])
                proj(B["K"], 256, 512)
                proj(B["V"], 512, 1024)
                if own:
                    proj(B["G"], 1024, 1536)
                if own or last_pre:
                    proj(B["Pp"], 1536, 2048)
                for c in range(8):
                    A("pe", lambda e, c=c: e.matmul(bk(B["M"])[0:16, 0:128], lhsT=winb[:, c, 2048:2064], rhs=hTv[:, c, :], start=(c == 0), stop=(c == 7)),
                      r=["hT", "winb"], w=[bn["M"]])
                if own:
                    for h in range(4):
                        for c in range(8):
                            A("pe", lambda e, c=c, h=h: e.matmul(bk(B["Q"])[0:64, h * 128:(h + 1) * 128], lhsT=winb[:, c, h * 64:(h + 1) * 64], rhs=hTv[:, c, :],
                                                                 start=(c == 0), stop=(c == 7)), r=["hT", "winb"], w=[bn["Q"]])
                    for h in range(4):
                        for c in range(8):
                            A("pe", lambda e, c=c, h=h: e.matmul(bk(B["KT"])[0:64, h * 128:(h + 1) * 128], lhsT=winb[:, c, 256 + h * 64:256 + (h + 1) * 64], rhs=hTv[:, c, :],
                                                                 start=(c == 0), stop=(c == 7)), r=["hT", "winb"], w=[bn["KT"]])
                A("act", lambda e: e.copy(out=alr[0:16, :], in_=bk(B["M"])[0:16, 0:128]), r=[bn["M"]], w=["alr"])
                A("pe", lambda e: e.matmul(bk(B["K"])[:, 256:512], lhsT=alr[:], rhs=gw[:], start=True, stop=True), r=["alr", "gw"], w=[bn["K"]])
                A("act", lambda e: e.activation(out=ect[:], in_=bk(B["K"])[:, 256:512], func=AF.Exp, scale=-1.0), r=[bn["K"]], w=["ect"])
                A("act", lambda e: e.activation(out=spt[:], in_=ect[:], func=AF.Ln, bias=1.0), r=["ect"], w=["spt"])
                A("pe", lambda e: e.matmul(bk(B["M"])[:, 128:384], lhsT=cs[:, C_TRIU:C_TRIU + 128], rhs=spt[:], start=True, stop=True), r=["cs", "spt"], w=[bn["M"]])
                for h in range(4):
                    A("pe", lambda e, h=h: e.matmul(bk(B["M"])[0:64, 384 + h:385 + h], lhsT=spt[:, h * 64:(h + 1) * 64], rhs=cs[:, C_NEG:C_NEG + 1], start=True, stop=True),
                      r=["cs", "spt"], w=[bn["M"]])
                if own:
                    for h in range(4):
                        A("pe", lambda e, h=h: e.matmul(bk(B["T"])[0:64, h * 128:(h + 1) * 128], lhsT=spt[:, h * 64:(h + 1) * 64], rhs=cs[:, C_TRIL:C_TRIL + 128], start=True, stop=True),
                          r=["cs", "spt"], w=[bn["T"]])
                A("act", lambda e: e.activation(out=ect[:], in_=bk(B["M"])[:, 128:384], func=AF.Exp), r=[bn["M"]], w=["ect"])
                A("dve", lambda e: e.tensor_tensor(out=kst[:], in0=bk(B["K"])[:, 0:256], in1=ect[:], op=ALU.mult), r=[bn["K"], "ect"], w=["kst"])
                A("act", lambda e: e.activation(out=dec[:], in_=bk(B["M"])[0:64, 384:388], func=AF.Exp), r=[bn["M"]], w=["dec"])
                if own:
                    A("act", lambda e: e.copy(out=vbf[:], in_=bk(B["V"])), r=[bn["V"]], w=["vbf"])
                else:
                    A("act", lambda e: e.activation(out=vbf[:], in_=bk(B["V"]), func=AF.Copy, scale=cs2[:, 512 + i:513 + i]), r=[bn["V"], "cs2"], w=["vbf"])
                if own:
                    A("act", lambda e: e.activation(out=ebt[:], in_=bk(B["T"])[0:64, :], func=AF.Exp), r=[bn["T"]], w=["ebt"])
                    A("act", lambda e: e.activation(out=enbt[:], in_=bk(B["T"])[0:64, :], func=AF.Exp, scale=-1.0), r=[bn["T"]], w=["enbt"])
                    A("dve", lambda e: e.scalar_tensor_tensor(out=qin[:], in0=bk(B["Q"])[0:64, :], scalar=0.125, in1=ebt[:], op0=ALU.mult, op1=ALU.mult),
                      r=[bn["Q"], "ebt"], w=["qin"])
                    A("dve", lambda e: e.tensor_tensor(out=kin[:], in0=bk(B["KT"])[0:64, :], in1=enbt[:], op=ALU.mult), r=[bn["KT"], "enbt"], w=["kin"])
                    for h in range(4):
                        A("pe", lambda e, h=h: e.matmul(bk(B["M"])[:, h * 128:(h + 1) * 128], lhsT=kin[:, h * 128:(h + 1) * 128], rhs=qin[:, h * 128:(h + 1) * 128], start=True, stop=True),
                          r=["kin", "qin"], w=[bn["M"]])
                    A("dve", lambda e: e.tensor_tensor(out=attn[:].rearrange("p (h i) -> p h i", h=4), in0=bk(B["M"]).rearrange("p (h i) -> p h i", h=4),
                                                       in1=cs[:, C_CAUS:C_CAUS + 128].unsqueeze(1).to_broadcast([128, 4, 128]), op=ALU.mult),
                      r=[bn["M"], "cs"], w=["attn"])
                    for h in range(4):
                        A("pe", lambda e, h=h: e.matmul(bk(B["Q"])[:, h * 128:(h + 1) * 128], lhsT=attn[:, h * 128:(h + 1) * 128], rhs=vbf[:, h * 128:(h + 1) * 128], start=True, stop=False),
                          r=["attn", "vbf"], w=[bn["Q"]])
                        A("pe", lambda e, h=h: e.matmul(bk(B["Q"])[:, h * 128:(h + 1) * 128], lhsT=qin[:, h * 128:(h + 1) * 128], rhs=stateb[:, h * 128:(h + 1) * 128], start=False, stop=True),
                          r=["qin", "stateb"], w=[bn["Q"]])
                KVb = B["KT"] if own else B["T"]
                for h in range(4):
                    A("pe", lambda e, h=h: e.matmul(bk(KVb)[0:64, h * 128:(h + 1) * 128], lhsT=kst[:, h * 64:(h + 1) * 64], rhs=vbf[:, h * 128:(h + 1) * 128], start=True, stop=True),
                      r=["kst", "vbf"], w=["b%d" % KVb])
                for h in range(4):
                    A("dve", lambda e, h=h: e.scalar_tensor_tensor(out=state[:, h * 128:(h + 1) * 128], in0=state[:, h * 128:(h + 1) * 128], scalar=dec[:, h:h + 1],
                                                                   in1=bk(KVb)[0:64, h * 128:(h + 1) * 128], op0=ALU.mult, op1=ALU.add),
                      r=["b%d" % KVb, "dec", "state"], w=["state"])
                A("act", lambda e: e.copy(out=stateb[:], in_=state[:]), r=["state"], w=["stateb"])
                if last_pre:
                    A("act", lambda e: e.activation(out=pcur[1][:], in_=bk(B["Pp"]), func=AF.Copy, scale=cs2[:, 512 + i:513 + i]), r=[bn["Pp"], "cs2"], w=["pcur1"])
                if not own:
                    return
                for h in range(4):
                    A("act", lambda e, h=h: e.activation(out=junk[:, 0:128], in_=bk(B["Q"])[:, h * 128:(h + 1) * 128], func=AF.Square, accum_out=ssqo[:, h:h + 1]),
                      r=[bn["Q"]], w=["junk", "ssqo"])
                A("act", lambda e: e.activation(out=ssqo[:, 4:8], in_=ssqo[:, 0:4], func=AF.Ln, scale=1.0 / 128, bias=epsc[:, 0:1]), r=["ssqo", "epsc"], w=["ssqo1"])
                A("act", lambda e: e.activation(out=ssqo[:, 0:4], in_=ssqo[:, 4:8], func=AF.Exp, scale=-0.5), r=["ssqo1"], w=["ssqo"])
                A("act", lambda e: e.activation(out=sg[:], in_=bk(B["G"]), func=AF.Silu), r=[bn["G"]], w=["sg"])
                for h in range(4):
                    A("dve", lambda e, h=h: e.scalar_tensor_tensor(out=yg[:, h * 128:(h + 1) * 128], in0=bk(B["Q"])[:, h * 128:(h + 1) * 128], scalar=ssqo[:, h:h + 1],
                                                                   in1=sg[:, h * 128:(h + 1) * 128], op0=ALU.mult, op1=ALU.mult),
                      r=[bn["Q"], "ssqo", "sg"], w=["yg"])
                A("dve", lambda e: e.tensor_tensor(out=ygb[:].rearrange("p (h v) -> p h v", h=4), in0=yg[:].rearrange("p (h v) -> p h v", h=4),
                                                   in1=gngr[:].unsqueeze(1).to_broadcast([128, 4, 128]), op=ALU.mult), r=["yg", "gngr"], w=["ygb"])
                for h in range(4):
                    A("pe", lambda e, h=h: e.transpose(out=bkb(B["T"])[:, h * 128:(h + 1) * 128], in_=ygb[:, h * 128:(h + 1) * 128], identity=identb[:]),
                      r=["ygb", "identb"], w=[bn["T"]])
                A("act", lambda e: e.copy(out=yglaT[:], in_=bkb(B["T"])[:, 0:512]), r=[bn["T"]], w=["yglaT"])
                pc = i % 2
                pp = 1 - pc
                A("act", lambda e: e.copy(out=pcur[pc][:], in_=bk(B["Pp"])), r=[bn["Pp"]], w=["pcur%d" % pc])
                for g in range(4):
                    ba = cs2[:, g * 128:(g + 1) * 128] if i == 0 else cs[:, C_BA + g * 128:C_BA + (g + 1) * 128]
                    A("pe", lambda e, g=g, ba=ba: e.matmul(bk(B["K"])[:, g * 128:(g + 1) * 128], lhsT=pcur[pc][:, g * 128:(g + 1) * 128], rhs=ba, start=True, stop=False),
                      r=["pcur%d" % pc, "cs", "cs2"], w=[bn["K"]])
                    A("pe", lambda e, g=g: e.matmul(bk(B["K"])[:, g * 128:(g + 1) * 128], lhsT=pcur[pp][:, g * 128:(g + 1) * 128], rhs=cs[:, C_BB + g * 128:C_BB + (g + 1) * 128], start=False, stop=True),
                      r=["pcur%d" % pp, "cs"], w=[bn["K"]])
                A("act", lambda e: e.copy(out=poolT[:], in_=bk(B["K"])), r=[bn["K"]], w=["poolT"])
                for g in range(4):
                    A("pe", lambda e, g=g: e.matmul(bk(B["Pp"])[:, g * 128:(g + 1) * 128], lhsT=pwb[:, g * 128:(g + 1) * 128], rhs=poolT[:, g * 128:(g + 1) * 128], start=True, stop=True),
                      r=["pwb", "poolT"], w=[bn["Pp"]])
                for g in range(4):
                    A("dve", lambda e, g=g: e.tensor_scalar(out=ypoolT[:, g * 128:(g + 1) * 128], in0=bk(B["Pp"])[:, g * 128:(g + 1) * 128], scalar1=pscs[:, g:g + 1], scalar2=None, op0=ALU.mult),
                      r=[bn["Pp"], "pscs"], w=["ypoolT"])
                for nb in range(2):
                    bank = B["V"] if nb == 0 else B["G"]
                    for c in range(8):
                        lt = yglaT[:, c * 128:(c + 1) * 128] if c < 4 else ypoolT[:, (c - 4) * 128:(c - 3) * 128]
                        A("pe", lambda e, c=c, lt=lt, bank=bank, nb=nb: e.matmul(bk(bank), lhsT=lt, rhs=woutb[:, c, nb * 512:(nb + 1) * 512], start=(c == 0), stop=(c == 7)),
                          r=["yglaT", "ypoolT", "woutb"], w=["b%d" % bank])
                for nb in range(2):
                    bank = B["V"] if nb == 0 else B["G"]
                    A("dve", lambda e, nb=nb, bank=bank: e.tensor_tensor(out=x1t[:, nb * 512:(nb + 1) * 512], in0=bk(bank), in1=gate1[:, nb * 512:(nb + 1) * 512], op=ALU.mult),
                      r=["b%d" % bank, "gate1"], w=["x1t"])
                A("dve", lambda e: e.tensor_tensor(out=x1t[:], in0=x1t[:], in1=xt[q][:], op=ALU.add), r=["x1t", xres], w=["x1t"])
                A("sp", lambda e: e.dma_start(out=x1s[i * 128:(i + 1) * 128, :], in_=x1t[:]), r=["x1t"], w=["x1s"], dma="sx1")
                if dbg:
                    A("sp", lambda e: e.dma_start(out=dbg_out["d_x1"][i * 128:(i + 1) * 128, :], in_=x1t[:]), r=["x1t"], w=["d_x1"], dma="dbg")
                norm_T(x1t[:], "x1t", gs2, sh2, h2T, "h2T", B["T"])
                A("sp", lambda e: e.dma_start(out=h2s[i], in_=h2T[:]), r=["h2T"], w=["h2s"], dma="sh2")

            for i in range(NT):
                if i + 1 < NT:
                    load_x("own", i + 1, (i + 1) % 2)
                sub1_tile("own", i, i)

        WT = sb("WT", NTOK, BF16)
        I1T = sb("I1T", NTOK, BF16)
        I2T = sb("I2T", NTOK, BF16)
        if "route" in phases:
            phase_start()
            stage = ta("stage", 4096)
            wqb = arena[:, 0:8 * 2048].rearrange("p (c n) -> p c n", c=8)
            wqv = wq.rearrange("(c p) n -> p c n", p=128)
            for c in range(8):
                hs = slice((c % 2) * 2048, (c % 2 + 1) * 2048)
                A("sp", lambda e: e.dma_start(out=stage[:, hs], in_=wqv[:, c, :]), w=["stageh%d" % (c % 2)], dma="stageh%d" % (c % 2))
                A("dve", lambda e: e.tensor_copy(out=wqb[:, c, :], in_=stage[:, hs]), r=["stageh%d" % (c % 2)], w=["wqb"])
            kT = ta("kT", 256, BF16)
            for hi, kk in enumerate((k1, k2)):
                A("sp", lambda e, kk=kk: e.dma_start(out=stage[:, 0:128], in_=kk), w=["stage", "stageh0"], dma="stage")
                A("pe", lambda e: e.transpose(out=bk(0)[:, 0:128], in_=stage[:, 0:128], identity=ident), r=["stage", "cs"], w=["b0"])
                A("dve", lambda e, hi=hi: e.tensor_copy(out=kT[:, hi * 128:(hi + 1) * 128], in_=bk(0)[:, 0:128]), r=["b0"], w=["kT"])
            h2t = [ta("h2t%d" % i, 1024, BF16) for i in range(2)]
            qpT = ta("qpT", 2048, BF16)
            ssb = ta("ssb", 2048)
            swk = ta("swk", 2048)
            vv = ta("vv", 256)
            idx = ta("idx", 256, U32)
            cand = ta("cand", 2048)
            cwk = ta("cwk", 2048)
            tsv = ta("tsv", 128)
            pos = ta("pos", 128, U32)
            wg = ta("wg", 128)
            zz = ta("zz", 16)
            ai = ta("ai", 128, I32)
            bi_ = ta("bi", 128, I32)
            af = ta("af", 128)
            bf = ta("bf", 128)
            i1f = ta("i1f", 128)
            i2f = ta("i2f", 128)
            oh = ta("oh", 2048)
            isel = ta("isel", 256)

            def load_h2(n):
                q = n % 2
                A("sp", lambda e: e.dma_start(out=h2t[q][:], in_=h2s[n]), r=["h2s"], w=["h2t%d" % q], dma="h2t%d" % q)
            prep_tile = make_prep(7, "sp") if "prep" in phases else None
            ssb2 = [ssb, stage[:, 0:2048]]
            sbank = lambda j: (4 + j // 4) if j < 12 else 3

            def route_front(n):
                q = n % 2
                ssb_ = ssb2[q]
                hv = h2t[q][:].rearrange("p (c t) -> p c t", c=8)
                if dbg:
                    A("sp", lambda e: e.dma_start(out=dbg_out["d_h2"][n], in_=h2t[q][:]), r=["h2t%d" % q], w=["d_h2"], dma="dbg3")
                for j in range(16):
                    for c in range(8):
                        A("pe", lambda e: e.matmul(bk(j // 4)[:, (j % 4) * 128:(j % 4 + 1) * 128], lhsT=wqb[:, c, j * 128:(j + 1) * 128], rhs=hv[:, c, :], start=(c == 0), stop=(c == 7)),
                          r=["wqb", "h2t%d" % q], w=["b%d" % (j // 4)])
                for b4 in range(4):
                    A("act", lambda e: e.copy(out=qpT[:, b4 * 512:(b4 + 1) * 512], in_=bk(b4)), r=["b%d" % b4], w=["qpT%d" % b4])
                for j in range(16):
                    A("pe", lambda e: e.matmul(bk(sbank(j))[:, (j % 4) * 128:(j % 4 + 1) * 128], lhsT=qpT[:, j * 128:(j + 1) * 128], rhs=kT[:, (j % 2) * 128:(j % 2 + 1) * 128], start=True, stop=True),
                      r=["qpT%d" % (j // 4), "kT"], w=["b%d" % sbank(j)])
                for b4 in range(4):
                    A("act", lambda e: e.copy(out=ssb_[:, b4 * 512:(b4 + 1) * 512], in_=bk(sbank(b4 * 4))), r=["b%d" % sbank(b4 * 4)], w=["ssb%d_%d" % (q, b4)])
                if n + 2 < NT:
                    load_h2(n + 2)

            vvb = [vv, ta("vv_1", 256)]
            idxb = [idx, ta("idx_1", 256, U32)]
            tsvb = [tsv, ta("tsv_1", 128)]
            posb = [pos, ta("pos_1", 128, U32)]

            def back_body(n, lists):
                q = n % 2
                rp = "p%d_" % q
                ssb = ssb2[q]
                vv, idx, tsv, pos = vvb[q], idxb[q], tsvb[q], posb[q]
                cap[0] = lists[0]
                for ph in range(5):
                    for j in range(16):
                        sl = slice(j * 128, (j + 1) * 128)
                        sr = "ssb%d_%d" % (q, j // 4)
                        vr = rp + "vv%d" % j
                        if ph == 0:
                            A("dve", lambda e: e.max(out=vv[:, j * 16:j * 16 + 8], in_=ssb[:, sl]), r=[sr], w=[vr + "a"])
                        elif ph == 1:
                            A("dve", lambda e: e.max_index(out=idx[:, j * 16:j * 16 + 8], in_max=vv[:, j * 16:j * 16 + 8], in_values=ssb[:, sl]), r=[sr, vr + "a"], w=[rp + "idx%da" % j])
                        elif ph == 2:
                            A("dve", lambda e: e.match_replace(out=swk[:, sl], in_to_replace=vv[:, j * 16:j * 16 + 8], in_values=ssb[:, sl], imm_value=-1e30), r=[sr, vr + "a"], w=["swk%d" % j])
                        elif ph == 3:
                            A("dve", lambda e: e.max(out=vv[:, j * 16 + 8:j * 16 + 16], in_=swk[:, sl]), r=["swk%d" % j], w=[vr + "b"])
                        else:
                            A("dve", lambda e: e.max_index(out=idx[:, j * 16 + 8:j * 16 + 16], in_max=vv[:, j * 16 + 8:j * 16 + 16], in_values=swk[:, sl]), r=["swk%d" % j, vr + "b"], w=[rp + "idx%db" % j])
                vvall = [rp + "vv%da" % j for j in range(16)] + [rp + "vv%db" % j for j in range(16)]
                idxall = [rp + "idx%da" % j for j in range(16)] + [rp + "idx%db" % j for j in range(16)]
                vv4 = vv[:].rearrange("p (h s a) -> p h s a", h=8, s=2)
                A("dve", lambda e: e.tensor_tensor(out=cand[:].rearrange("p (h a b) -> p h a b", h=8, a=16),
                                                   in0=vv4[:, :, 0, :].unsqueeze(3).to_broadcast([128, 8, 16, 16]),
                                                   in1=vv4[:, :, 1, :].unsqueeze(2).to_broadcast([128, 8, 16, 16]), op=ALU.add), r=vvall, w=["cand"])
                for ph in range(5):
                    for h in range(8):
                        sl = slice(h * 256, (h + 1) * 256)
                        tr = rp + "tsv%d" % h
                        if ph == 0:
                            A("dve", lambda e: e.max(out=tsv[:, h * 16:h * 16 + 8], in_=cand[:, sl]), r=["cand"], w=[tr + "a"])
                        elif ph == 1:
                            A("dve", lambda e: e.max_index(out=pos[:, h * 16:h * 16 + 8], in_max=tsv[:, h * 16:h * 16 + 8], in_values=cand[:, sl]), r=["cand", tr + "a"], w=[rp + "pos%da" % h])
                        elif ph == 2:
                            A("dve", lambda e: e.match_replace(out=cwk[:, sl], in_to_replace=tsv[:, h * 16:h * 16 + 8], in_values=cand[:, sl], imm_value=-1e30), r=["cand", tr + "a"], w=["cwk%d" % h])
                        elif ph == 3:
                            A("dve", lambda e: e.max(out=tsv[:, h * 16 + 8:h * 16 + 16], in_=cwk[:, sl]), r=["cwk%d" % h], w=[tr + "b"])
                        else:
                            A("dve", lambda e: e.max_index(out=pos[:, h * 16 + 8:h * 16 + 16], in_max=tsv[:, h * 16 + 8:h * 16 + 16], in_values=cwk[:, sl]), r=["cwk%d" % h, tr + "b"], w=[rp + "pos%db" % h])
                tsvall = [rp + "tsv%da" % h for h in range(8)] + [rp + "tsv%db" % h for h in range(8)]
                posall = [rp + "pos%da" % h for h in range(8)] + [rp + "pos%db" % h for h in range(8)]
                cap[0] = lists[1]
                ts3 = tsv[:].rearrange("p (h k) -> p h k", h=8)
                A("dve", lambda e: e.tensor_tensor(out=wg[:].rearrange("p (h k) -> p h k", h=8), in0=ts3, in1=ts3[:, :, 0:1].to_broadcast([128, 8, 16]), op=ALU.subtract), r=tsvall, w=["wg"])
                A("act", lambda e: e.activation(out=wg[:], in_=wg[:], func=AF.Exp), r=["wg"], w=["wg"])
                A("dve", lambda e: e.tensor_reduce(out=zz[:, 0:8], in_=wg[:].rearrange("p (h k) -> p h k", h=8), axis=AX.X, op=ALU.add), r=["wg"], w=["zz"])
                A("dve", lambda e: e.reciprocal(out=zz[:, 8:16], in_=zz[:, 0:8]), r=["zz"], w=["zz1"])
                A("dve", lambda e: e.tensor_tensor(out=wg[:].rearrange("p (h k) -> p h k", h=8), in0=wg[:].rearrange("p (h k) -> p h k", h=8),
                                                   in1=zz[:, 8:16].unsqueeze(2).to_broadcast([128, 8, 16]), op=ALU.mult), r=["wg", "zz1"], w=["wg"])
                A("dve", lambda e: e.tensor_single_scalar(out=ai[:], in_=pos[:].bitcast(I32), scalar=4, op=ALU.logical_shift_right), r=posall, w=["ai"])
                A("dve", lambda e: e.tensor_single_scalar(out=bi_[:], in_=pos[:].bitcast(I32), scalar=15, op=ALU.bitwise_and), r=posall, w=["bi"])
                A("dve", lambda e: e.tensor_copy(out=af[:], in_=ai[:]), r=["ai"], w=["af"])
                A("dve", lambda e: e.tensor_copy(out=bf[:], in_=bi_[:]), r=["bi"], w=["bf"])
                idx4 = idx[:].rearrange("p (h s a) -> p h s a", h=8, s=2)
                A("dve", lambda e: e.tensor_copy(out=i1f[:].rearrange("p (h a) -> p h a", h=8), in_=idx4[:, :, 0, :]), r=idxall, w=["i1f"])
                A("dve", lambda e: e.tensor_copy(out=i2f[:].rearrange("p (h a) -> p h a", h=8), in_=idx4[:, :, 1, :]), r=idxall, w=["i2f"])
                io16 = cs[:, C_IOTA16:C_IOTA16 + 16]
                for which, (cf, inf) in enumerate(((af, i1f), (bf, i2f))):
                    A("dve", lambda e, cf=cf: e.tensor_tensor(out=oh[:].rearrange("p (s a) -> p s a", a=16),
                                                              in0=io16.unsqueeze(1).to_broadcast([128, 128, 16]),
                                                              in1=cf[:].unsqueeze(2).to_broadcast([128, 128, 16]), op=ALU.is_equal), r=["cs", "af", "bf"], w=["oh"])
                    A("dve", lambda e, inf=inf: e.tensor_tensor(out=oh[:].rearrange("p (h k a) -> p h k a", h=8, k=16),
                                                                in0=oh[:].rearrange("p (h k a) -> p h k a", h=8, k=16),
                                                                in1=inf[:].rearrange("p (h a) -> p h a", h=8).unsqueeze(2).to_broadcast([128, 8, 16, 16]), op=ALU.mult),
                      r=["oh", "i1f", "i2f"], w=["oh"])
                    A("dve", lambda e, which=which: e.tensor_reduce(out=isel[:, which * 128:(which + 1) * 128], in_=oh[:].rearrange("p (s a) -> p s a", a=16), axis=AX.X, op=ALU.add),
                      r=["oh"], w=["isel"])
                for which, (src, srcres, dst) in enumerate(((wg[:], "wg", WT), (isel[:, 0:128], "isel", I1T), (isel[:, 128:256], "isel", I2T))):
                    A("pe", lambda e, src=src, which=which: e.transpose(out=bk(7)[:, which * 128:(which + 1) * 128], in_=src, identity=ident), r=[srcres, "cs"], w=["b7"])
                for which, dst in enumerate((WT, I1T, I2T)):
                    A("act", lambda e, which=which, dst=dst, n=n: e.copy(out=dst[:, n * 128:(n + 1) * 128], in_=bk(7)[:, which * 128:(which + 1) * 128]), r=["b7"], w=["rt%d" % which])
                if dbg and n == 0:
                    A("sp", lambda e: e.dma_start(out=dbg_out["d_qp"], in_=qpT[:]), r=["qpT0", "qpT1", "qpT2", "qpT3"], w=["d_qp"], dma="dbg4")
                    offs = 0
                    for nm, src_, res_ in (("vv", vv, "vv"), ("i1f", i1f, "i1f"), ("i2f", i2f, "i2f"), ("af", af, "af"), ("bf", bf, "bf"), ("isel", isel, "isel"), ("wg", wg, "wg"), ("tsv", tsv, "tsv")):
                        w_ = src_.shape[1]
                        A("sp", lambda e, src_=src_, offs=offs, w_=w_: e.dma_start(out=dbg_out["d_misc"][:, offs:offs + w_], in_=src_[:]), r=[res_], w=["d_misc"], dma="dbg2")
                        offs += w_
                    A("sp", lambda e: e.dma_start(out=dbg_out["d_misc"][:, 1280:1536], in_=ssb[:, 0:256]), r=["ssb0_0"], w=["d_misc"], dma="dbg2")
                cap[0] = None

            def run_merged(ops1, ops2):
                for t in ops2:
                    t()
                for t in ops1:
                    t()

            load_h2(0)
            load_h2(1)
            route_front(0)
            if NT > 1:
                route_front(1)
            L0 = [[], []]
            back_body(0, L0)
            run_merged(L0[0], [])
            pend2 = L0[1]
            for n in range(NT):
                if n + 2 < NT:
                    route_front(n + 2)
                if prep_tile is not None:
                    for pi in range(8):
                        prep_tile(n * 8 + pi)
                if n + 1 < NT:
                    Ln = [[], []]
                    back_body(n + 1, Ln)
                    run_merged(Ln[0], pend2)
                    pend2 = Ln[1]
                else:
                    run_merged([], pend2)
            if dbg:
                rtf = cand
                for which, dst in enumerate((WT, I1T, I2T)):
                    A("dve", lambda e, dst=dst: e.tensor_copy(out=rtf[:], in_=dst[:]), r=["rt%d" % which], w=["rtf", "cand"])
                    A("sp", lambda e, which=which: e.dma_start(out=dbg_out["d_rt"][which], in_=rtf[:]), r=["rtf"], w=["d_rt"], dma="dbg")

        if "dense" in phases:
            phase_start()
            Gt = arena[:, 0:NE * TT].rearrange("p (t i) -> p t i", i=NE)
            NB = 3
            utg = [ta("utg%d" % i, GRP * 1024, BF16) for i in range(NB)]
            vg = [ta("vg%d" % i, GRP * 1024, BF16) for i in range(NB)]
            h2tt = ta("h2tt", 8 * TT, BF16)
            OB = 16
            ohA = [ta("ohA%d" % i, OB * 128, BF16) for i in range(2)]
            ohB = [ta("ohB%d" % i, OB * 128, BF16) for i in range(2)]
            gcount = [0]
            iorep = ta("iorep", OB * 128, BF16)
            A("dve", lambda e: e.tensor_copy(out=iorep[:].rearrange("p (i t) -> p i t", t=OB), in_=iotab[:].unsqueeze(2).to_broadcast([128, 128, OB])), r=["iotab"], w=["iorep"])
            gl = [ta("gl%d" % i, TT, BF16) for i in range(2)]
            ga = [ta("ga%d" % i, TT, BF16) for i in range(2)]
            x1l = ta("x1l", 1024)
            x2 = ta("x2", 1024)
            ot = x2
            junk2 = ta("junk2", 1024, BF16)
            ss2 = ta("ss2", 4)
            NG = NE // GRP

            def load_w(T, g, cnt):
                q = cnt % NB
                A("sp", lambda e: e.dma_start(out=utg[q][:].rearrange("p (q n) -> p q n", q=GRP), in_=UTs[g * GRP:(g + 1) * GRP].rearrange("q p n -> p q n")),
                  r=["UTs"], w=["utg%d" % q], dma="utg%d" % q)
                A("sp", lambda e: e.dma_start(out=vg[q][:].rearrange("p (q n) -> p q n", q=GRP), in_=Vbs[g * GRP:(g + 1) * GRP].rearrange("q p n -> p q n")),
                  r=["Vbs"], w=["vg%d" % q], dma="vg%d" % q)

            allg = [(T, g) for T in range(NTOK // TT) for g in range(NG)]
            cnt_load = 0
            for _ in range(NB - 1):
                load_w(allg[cnt_load][0], allg[cnt_load][1], cnt_load)
                cnt_load += 1
            cnt_use = 0
            first_g = True
            for T in range(NTOK // TT):
                t0 = T * TT
                for sub in range(TT // 128):
                    A("sp", lambda e, sub=sub: e.dma_start(out=h2tt[:].rearrange("p (c t) -> p c t", c=8)[:, :, sub * 128:(sub + 1) * 128],
                                                           in_=h2s[T * (TT // 128) + sub].rearrange("p (c t) -> p c t", c=8)), r=["h2s"], w=["h2tt"], dma="h2tt")
                def g_dve(Tn, ob):
                    tk = Tn * TT + ob * OB
                    k = ob % 2
                    oa = ohA[k][:].rearrange("p (i t) -> p i t", t=OB)
                    obv = ohB[k][:].rearrange("p (i t) -> p i t", t=OB)
                    A("dve", lambda e: e.tensor_tensor(out=oa, in0=iorep[:].rearrange("p (i t) -> p i t", t=OB),
                                                       in1=I1T[:, tk:tk + OB].unsqueeze(1).to_broadcast([128, 128, OB]), op=ALU.is_equal),
                      r=["iorep", "rt1"], w=["ohA%d" % k])
                    A("dve", lambda e: e.tensor_tensor(out=oa, in0=oa, in1=WT[:, tk:tk + OB].unsqueeze(1).to_broadcast([128, 128, OB]), op=ALU.mult),
                      r=["ohA%d" % k, "rt0"], w=["ohA%d" % k])
                    A("dve", lambda e: e.tensor_tensor(out=obv, in0=iorep[:].rearrange("p (i t) -> p i t", t=OB),
                                                       in1=I2T[:, tk:tk + OB].unsqueeze(1).to_broadcast([128, 128, OB]), op=ALU.is_equal),
                      r=["iorep", "rt2"], w=["ohB%d" % k])

                def g_pe(ob):
                    k = ob % 2
                    oa = ohA[k][:].rearrange("p (i t) -> p i t", t=OB)
                    obv = ohB[k][:].rearrange("p (i t) -> p i t", t=OB)
                    for t4 in range(OB // 4):
                        gb = 4 + (gcount[0] % 4)
                        gcount[0] += 1
                        for tt in range(4):
                            tl = t4 * 4 + tt
                            A("pe", lambda e: e.matmul(bk(gb)[:, tt * 128:(tt + 1) * 128], lhsT=obv[:, :, tl], rhs=oa[:, :, tl], start=True, stop=True),
                              r=["ohA%d" % k, "ohB%d" % k], w=["b%d" % gb])
                        tl0 = ob * OB + t4 * 4
                        dstv = Gt[:, tl0:tl0 + 4, :]
                        A("act", lambda e: e.copy(out=dstv, in_=bk(gb).rearrange("p (t i) -> p t i", t=4)), r=["b%d" % gb], w=["G"])

                NOB = TT // OB
                if T == 0:
                    g_dve(0, 0)
                    g_dve(0, 1)
                for ob in range(NOB):
                    g_pe(ob)
                    if ob + 2 < NOB:
                        g_dve(T, ob + 2)
                h2v = h2tt[:].rearrange("p (c t) -> p c t", c=8)

                def scores(i1, q, ql):
                    sbk = 4 + (i1 % 2)
                    uv = utg[q][:].rearrange("p (q c e) -> p q c e", q=GRP, c=8)
                    for c in range(8):
                        A("pe", lambda e, c=c: e.matmul(bk(sbk)[:, 0:TT], lhsT=uv[:, ql, c, :], rhs=h2v[:, c, :], start=(c == 0), stop=(c == 7)),
                          r=["utg%d" % q, "h2tt"], w=["b%d" % sbk])
                    A("act", lambda e: e.activation(out=gl[i1 % 2][:], in_=bk(sbk)[:, 0:TT], func=AF.Gelu), r=["b%d" % sbk], w=["gl%d" % (i1 % 2)])
                    A("dve", lambda e: e.tensor_tensor(out=ga[i1 % 2][:], in0=gl[i1 % 2][:], in1=Gt[:, :, i1], op=ALU.mult), r=["gl%d" % (i1 % 2), "G"], w=["ga%d" % (i1 % 2)])

                def vmm(i1, q, ql):
                    vv_ = vg[q][:].rearrange("p (q n) -> p q n", q=GRP)
                    for th in range(TT // 128):
                        for dh in range(2):
                            A("pe", lambda e, th=th, dh=dh: e.matmul(bk(th * 2 + dh), lhsT=ga[i1 % 2][:, th * 128:(th + 1) * 128], rhs=vv_[:, ql, dh * 512:(dh + 1) * 512],
                                                                     start=(i1 == 0), stop=(i1 == NE - 1)),
                              r=["ga%d" % (i1 % 2), "vg%d" % q], w=["b%d" % (th * 2 + dh)])

                for g in range(NG):
                    q = cnt_use % NB
                    for ql in range(GRP):
                        i1 = g * GRP + ql
                        scores(i1, q, ql)
                        if i1 > 0:
                            pq = q if ql > 0 else (cnt_use - 1) % NB
                            vmm(i1 - 1, pq, (ql - 1) % GRP)
                        if ql == 0 and cnt_load < len(allg):
                            load_w(allg[cnt_load][0], allg[cnt_load][1], cnt_load)
                            cnt_load += 1
                        if T + 1 < NTOK // TT and i1 == NE - 12:
                            g_dve(T + 1, 0)
                        if T + 1 < NTOK // TT and i1 == NE - 6:
                            g_dve(T + 1, 1)
                    cnt_use += 1
                vmm(NE - 1, (cnt_use - 1) % NB, GRP - 1)
                for th in range(TT // 128):
                    r0 = t0 + th * 128
                    A("sp", lambda e, r0=r0: e.dma_start(out=x1l[:], in_=x1s[r0:r0 + 128, :]), r=["x1s"], w=["x1l"], dma="x1l")
                    for dh in range(2):
                        A("dve", lambda e, th=th, dh=dh: e.tensor_tensor(out=x2[:, dh * 512:(dh + 1) * 512], in0=bk(th * 2 + dh), in1=gate2[:, dh * 512:(dh + 1) * 512], op=ALU.mult),
                          r=["b%d" % (th * 2 + dh), "gate2"], w=["x2"])
                    A("pool", lambda e: e.tensor_tensor(out=x2[:], in0=x2[:], in1=x1l[:], op=ALU.add), r=["x2", "x1l"], w=["x2"])
                    A("act", lambda e: e.activation(out=junk2[:], in_=x2[:], func=AF.Square, accum_out=ss2[:, 0:1]), r=["x2"], w=["junk2", "ss2"])
                    A("act", lambda e: e.activation(out=ss2[:, 1:2], in_=ss2[:, 0:1], func=AF.Ln, scale=1.0 / D, bias=epsc[:, 0:1]), r=["ss2", "epsc"], w=["ss21"])
                    A("act", lambda e: e.activation(out=ss2[:, 2:3], in_=ss2[:, 1:2], func=AF.Exp, scale=-0.5), r=["ss21"], w=["ss22"])
                    A("dve", lambda e: e.scalar_tensor_tensor(out=ot[:], in0=x2[:], scalar=ss2[:, 2:3], in1=fgr[:], op0=ALU.mult, op1=ALU.mult), r=["x2", "ss22", "fgr"], w=["x2"])
                    A("sp", lambda e, r0=r0: e.dma_start(out=out[r0:r0 + 128, :], in_=ot[:]), r=["x2"], w=["out"], dma="sout")
        fin = ["out"] + list(dbg_out.keys())
        A("sp", None, r=fin)
        P.emit(st)
    return nc


_NC_CACHE = {}


def make_in_maps(inputs):
    f = lambda a: np.ascontiguousarray(np.asarray(a, dtype=np.float32))
    x = f(inputs["x"])
    c = f(inputs["c"])
    cst = make_consts()
    colT = lambda v, k: np.ascontiguousarray(v.reshape(k, 128).T)
    shared = {
        "cst": cst,
        "ada_w": f(inputs["ada_w"])[0],
        "ada_bT": colT(f(inputs["ada_b"])[0], 48),
        "ada_b": f(inputs["ada_b"])[0].reshape(1, 6 * D),
        "n1g": colT(f(inputs["norm1_g"])[0], 8),
        "n2g": colT(f(inputs["norm2_g"])[0], 8),
        "fg_rep": np.ascontiguousarray(np.broadcast_to(f(inputs["final_g"])[None, :], (128, D))),
        "w_in": f(inputs["w_in"])[0],
        "gw17": np.ascontiguousarray(np.concatenate([f(inputs["gla_gate_w"])[0], f(inputs["gla_gate_b"])[0][None, :]], axis=0)),
        "gng_rep": np.ascontiguousarray(np.broadcast_to(f(inputs["gla_norm_g"])[0][None, :], (128, 128))),
        "pool_w": f(inputs["pool_w"])[0],
        "pscT": colT(f(inputs["pool_scale"])[0], 4),
        "w_out": f(inputs["w_out"])[0],
        "wq": f(inputs["peer_wq"])[0],
        "k1": f(inputs["peer_k1"])[0],
        "k2": f(inputs["peer_k2"])[0],
        "pu": f(inputs["peer_u"])[0],
        "pv": f(inputs["peer_v"])[0],
    }
    maps = []
    for core in range(8):
        b, j = core // 4, core % 4
        m = dict(shared)
        m["x_own"] = np.ascontiguousarray(x[b, j * NTOK:(j + 1) * NTOK])
        xp = np.zeros((NPRE * 128, D), np.float32)
        npre = j * NTOK
        if npre > 0:
            xp[NPRE * 128 - npre:] = x[b, 0:npre]
        m["x_pre"] = xp
        m["cst2"] = make_core_consts(j)
        m["c_col"] = colT(c[b], 8)
        maps.append(m)
    return maps


def kernel(**inputs):
    if "nc" not in _NC_CACHE:
        _NC_CACHE["nc"] = build_nc()
    nc = _NC_CACHE["nc"]
    maps = make_in_maps(inputs)
    res = run_bass_kernel_spmd(nc, maps, core_ids=list(range(8)))
    out = np.zeros((2, 8192, D), np.float32)
    for core in range(8):
        b, j = core // 4, core % 4
        out[b, j * NTOK:(j + 1) * NTOK] = res.results[core]["out"]
    return out
```

```python
import numpy as np
from contextlib import ExitStack
import concourse.bass as bass
import concourse.mybir as mybir
from concourse.bass_utils import run_bass_kernel_spmd

F32 = mybir.dt.float32
BF16 = mybir.dt.bfloat16
U32 = mybir.dt.uint32
I32 = mybir.dt.int32
AF = mybir.ActivationFunctionType
ALU = mybir.AluOpType
AX = mybir.AxisListType

D = 1024
NTOK = 2048
NT = NTOK // 128
NPRE = 48
INC = 2064
EPS = 1e-6
NE = 128
TT = 256
GRP = 2


class Inst:
    __slots__ = ("eng", "fn", "deps", "is_dma", "semkey", "ordinal", "sig", "sem", "idx")


class _Rec:
    def __init__(self):
        self.call = None

    def __getattr__(self, name):
        def f(*a, **k):
            self.call = (name, a, k)
            return None
        return f


class Prog:
    ENG = ("pe", "act", "dve", "pool", "sp")
    LIMIT = 20000

    def __init__(self, nc):
        self.nc = nc
        self.lists = {e: [] for e in self.ENG}
        self.last_w = {}
        self.readers = {}
        self.dma_counts = {}
        self.last_dma = {}

    def barrier(self):
        deps = set()
        for e in self.ENG:
            for ins in reversed(self.lists[e]):
                if not ins.is_dma and ins.fn is not None:
                    deps.add(ins)
                    break
        for last in self.last_dma.values():
            deps.add(last)
        for e in self.ENG:
            ins = Inst()
            ins.eng = e
            ins.fn = None
            ins.is_dma = False
            ins.semkey = None
            ins.sem = None
            ins.ordinal = 0
            ins.sig = False
            ins.deps = set(deps)
            self.lists[e].append(ins)
            ins.idx = len(self.lists[e]) - 1
        self.last_w.clear()
        self.readers.clear()

    def defer(self, eng, fn, r=(), w=(), dma=None):
        if fn is not None:
            rec = _Rec()
            fn(rec)
            name_, a_, k_ = rec.call
            fn = (lambda e, name_=name_, a_=a_, k_=k_: getattr(e, name_)(*a_, **k_))
        r = list(r)
        w = list(w)
        return lambda: self.add(eng, fn, r, w, dma, _bound=True)

    def add(self, eng, fn, r=(), w=(), dma=None, _bound=False):
        ins = Inst()
        ins.eng = eng
        if fn is not None and not _bound:
            rec = _Rec()
            fn(rec)
            name_, a_, k_ = rec.call
            fn = (lambda e, name_=name_, a_=a_, k_=k_: getattr(e, name_)(*a_, **k_))
        ins.fn = fn
        ins.is_dma = dma is not None
        ins.semkey = dma
        ins.sem = None
        ins.ordinal = 0
        deps = set()
        for k in r:
            lw = self.last_w.get(k)
            if lw is not None:
                deps.add(lw)
        for k in w:
            lw = self.last_w.get(k)
            if lw is not None:
                deps.add(lw)
            for rd in self.readers.get(k, ()):
                deps.add(rd)
        ins.deps = deps
        for k in r:
            self.readers.setdefault(k, []).append(ins)
        for k in w:
            self.last_w[k] = ins
            self.readers[k] = []
        if dma is not None:
            c = self.dma_counts.get(dma, 0) + 1
            self.dma_counts[dma] = c
            ins.ordinal = c
            self.last_dma[dma] = ins
        ins.sig = False
        self.lists[eng].append(ins)
        ins.idx = len(self.lists[eng]) - 1
        return ins

    def emit(self, stack):
        nc = self.nc
        for e in self.ENG:
            for ins in self.lists[e]:
                for d in ins.deps:
                    if d.is_dma:
                        continue
                    if d.eng == ins.eng == "pe":
                        continue
                    d.sig = True
        semkeys = []
        for e in self.ENG:
            cnt = 0
            epoch = 0
            for ins in self.lists[e]:
                if ins.is_dma or not ins.sig:
                    continue
                cnt += 1
                if cnt > self.LIMIT:
                    epoch += 1
                    cnt = 1
                ins.sem = (e, epoch)
                ins.ordinal = cnt
                if ins.sem not in semkeys:
                    semkeys.append(ins.sem)
        for k in self.dma_counts:
            semkeys.append(("dma", k))
        self.sems = {}
        for i, k in enumerate(semkeys):
            self.sems[k] = stack.enter_context(nc.semaphore("s%d" % i))
        block = stack.enter_context(nc.Block())

        def run(engname, eng):
            waited = {}
            for ins in self.lists[engname]:
                need = {}
                for d in ins.deps:
                    if d.is_dma:
                        key = ("dma", d.semkey)
                        val = 16 * d.ordinal
                    else:
                        if d.eng == engname == "pe":
                            continue
                        key = d.sem
                        val = d.ordinal
                    if need.get(key, 0) < val:
                        need[key] = val
                for key, val in need.items():
                    if waited.get(key, 0) >= val:
                        continue
                    eng.wait_ge(self.sems[key], val)
                    waited[key] = val
                if ins.fn is None:
                    continue
                bi = ins.fn(eng)
                if ins.is_dma:
                    bi.then_inc(self.sems[("dma", ins.semkey)], 16)
                elif ins.sig:
                    bi.then_inc(self.sems[ins.sem], 1)

        @block.tensor
        def _(e):
            run("pe", e)

        @block.scalar
        def _(e):
            run("act", e)

        @block.vector
        def _(e):
            run("dve", e)

        @block.gpsimd
        def _(e):
            run("pool", e)

        @block.sync
        def _(e):
            run("sp", e)


C_ID, C_IOTA, C_TRIL, C_TRIU, C_CAUS, C_ONES, C_NEG, C_IOTA16, C_BA, C_BB = (
    0, 128, 256, 384, 512, 640, 768, 769, 785, 785 + 512)
C_N = 785 + 1024


def make_consts():
    c = np.zeros((128, C_N), np.float32)
    s = np.arange(128)[:, None]
    t = np.arange(128)[None, :]
    c[:, C_ID:C_ID + 128] = (s == t)
    c[:, C_IOTA:C_IOTA + 128] = t
    c[:, C_TRIL:C_TRIL + 128] = np.where(s <= t, -1.0 / 16, 0.0)
    c[:, C_TRIU:C_TRIU + 128] = np.where(s > t, -1.0 / 16, 0.0)
    c[:, C_CAUS:C_CAUS + 128] = (s <= t)
    c[:, C_ONES:C_ONES + 128] = 1.0
    c[:, C_NEG] = -1.0 / 16
    c[:, C_IOTA16:C_IOTA16 + 16] = np.arange(16)[None, :]
    for g, w in enumerate((2, 4, 8, 16)):
        d = t - s
        c[:, C_BA + g * 128:C_BA + (g + 1) * 128] = np.where((d >= 0) & (d < w), 1.0 / w, 0.0) - (s == t)
        d2 = t + 128 - s
        c[:, C_BB + g * 128:C_BB + (g + 1) * 128] = np.where((d2 >= 0) & (d2 < w), 1.0 / w, 0.0)
    return c


def make_core_consts(j):
    c = np.zeros((128, 512 + NPRE), np.float32)
    s = np.arange(128)[:, None]
    t = np.arange(128)[None, :]
    for g, w in enumerate((2, 4, 8, 16)):
        d = t - s
        if j == 0:
            cnt = np.minimum(t + 1, w).astype(np.float32)
        else:
            cnt = np.full_like(t, w).astype(np.float32)
        c[:, g * 128:(g + 1) * 128] = np.where((d >= 0) & (d < w), 1.0 / cnt, 0.0) - (s == t)
    pos = NTOK * j - NPRE * 128 + np.arange(NPRE * 128)
    c[:, 512:] = (pos >= 0).astype(np.float32).reshape(NPRE, 128).T
    return c


def build_nc(phases=("mod", "prep", "sub1", "route", "dense"), dbg=False):
    nc = bass.Bass("TRN2", target_bir_lowering=False)
    din = lambda n, s, d=F32: nc.dram_tensor(n, s, d, kind="ExternalInput").ap()
    x_own = din("x_own", [NTOK, D])
    x_pre = din("x_pre", [NPRE * 128, D])
    cst = din("cst", [128, C_N])
    cst2 = din("cst2", [128, 512 + NPRE])
    c_col = din("c_col", [128, 8])
    ada_w = din("ada_w", [D, 6 * D])
    ada_bT = din("ada_bT", [128, 48])
    ada_b = din("ada_b", [1, 6 * D])
    n1g = din("n1g", [128, 8])
    n2g = din("n2g", [128, 8])
    fg_rep = din("fg_rep", [128, D])
    w_in = din("w_in", [D, INC])
    gw17 = din("gw17", [17, 256])
    gng_rep = din("gng_rep", [128, 128])
    pool_w = din("pool_w", [4, 128, 128])
    pscT = din("pscT", [128, 4])
    w_out = din("w_out", [D, D])
    wq = din("wq", [D, 2048])
    k1 = din("k1", [128, 128])
    k2 = din("k2", [128, 128])
    pu = din("pu", [NE * 128, D])
    pv = din("pv", [NE * 128, D])
    out = nc.dram_tensor("out", [NTOK, D], F32, kind="ExternalOutput").ap()
    skind = "ExternalOutput" if dbg else "Internal"
    UTs = nc.dram_tensor("UTs", [NE, 128, 1024], BF16, kind=skind).ap()
    Vbs = nc.dram_tensor("Vbs", [NE, 128, 1024], BF16, kind=skind).ap()
    x1s = nc.dram_tensor("x1s", [NTOK, D], F32).ap()
    h2s = nc.dram_tensor("h2s", [NT, 128, 1024], BF16).ap()
    dbg_out = {}
    if dbg:
        for n, s in (("d_mod", [128, 32]), ("d_x1", [NTOK, D]), ("d_h2", [NT, 128, 1024]),
                     ("d_rt", [3, 128, NTOK]), ("d_g1", [128, D]), ("d_misc", [128, 2048]), ("d_qp", [128, 2048])):
            dbg_out[n] = nc.dram_tensor(n, s, BF16 if n in ("d_h2", "d_qp") else F32, kind="ExternalOutput").ap()

    with ExitStack() as st:
        def sb(n, cols, d=F32, parts=128):
            return st.enter_context(nc.sbuf_tensor(n, [parts, cols], d))
        P = Prog(nc)
        cap = [None]

        def A(eng, fn, r=(), w=(), dma=None):
            if cap[0] is not None:
                cap[0].append(P.defer(eng, fn, r, w, dma))
                return None
            return P.add(eng, fn, r, w, dma)
        banks = [st.enter_context(nc.psum_tensor("ps%d" % i, [128, 512], F32)) for i in range(8)]

        def bk(i):
            return banks[i][:]

        def bkb(i):
            return banks[i][:].bitcast(BF16)

        cs = sb("cs", C_N)
        cs2 = sb("cs2", 512 + NPRE)
        identb = sb("identb", 128, BF16)
        iotab = sb("iotab", 128, BF16)
        modT = sb("modT", 32)
        gs1 = sb("gs1", 8)
        gs2 = sb("gs2", 8)
        gate1 = sb("gate1", D)
        gate2 = sb("gate2", D)
        fgr = sb("fgr", D)
        gngr = sb("gngr", 128)
        pscs = sb("pscs", 4)
        n1s = sb("n1s", 8)
        n2s = sb("n2s", 8)
        epsc = sb("epsc", 1)
        print("sbuf bytes remaining after persistent:", nc.sbuf_bytes_remaining)
        A("sp", lambda e: e.dma_start(out=cs[:], in_=cst), w=["cs"], dma="c0_1")
        A("sp", lambda e: e.dma_start(out=cs2[:], in_=cst2), w=["cs2"], dma="c0_2")
        A("sp", lambda e: e.dma_start(out=fgr[:], in_=fg_rep), w=["fgr"], dma="c0_3")
        A("sp", lambda e: e.dma_start(out=gngr[:], in_=gng_rep), w=["gngr"], dma="c0_4")
        A("sp", lambda e: e.dma_start(out=pscs[:], in_=pscT), w=["pscs"], dma="c0_5")
        A("sp", lambda e: e.dma_start(out=n1s[:], in_=n1g), w=["n1s"], dma="c0_6")
        A("sp", lambda e: e.dma_start(out=n2s[:], in_=n2g), w=["n2s"], dma="c0_7")
        ident = cs[:, C_ID:C_ID + 128]
        A("dve", lambda e: e.memset(epsc[:], EPS), w=["epsc"])
        A("dve", lambda e: e.tensor_copy(out=identb[:], in_=ident), r=["cs"], w=["identb"])
        A("dve", lambda e: e.tensor_copy(out=iotab[:], in_=cs[:, C_IOTA:C_IOTA + 128]), r=["cs"], w=["iotab"])

        ARENA = NE * TT
        arena = sb("arena", ARENA, BF16)
        TMPF = 26624
        tmp = st.enter_context(nc.sbuf_tensor("tmp", [128, TMPF], F32))
        tmp_views = {F32: tmp, BF16: tmp[:].bitcast(BF16), U32: tmp[:].bitcast(U32), I32: tmp[:].bitcast(I32)}
        ta_off = [0]

        def ta(n, cols, d=F32, parts=128):
            words = (cols + 1) // 2 if d == BF16 else cols
            o = ta_off[0]
            ta_off[0] += words
            assert ta_off[0] <= TMPF, (n, ta_off[0])
            if d == BF16:
                return tmp_views[BF16][0:parts, 2 * o:2 * o + cols]
            return tmp_views[d][0:parts, o:o + cols]

        def phase_start():
            P.barrier()
            ta_off[0] = 0

        def make_prep(pbank=None, deng="pool"):
            ust = [ta("ust%d" % i, 1024) for i in range(2)]
            vst = [ta("vst%d" % i, 1024) for i in range(2)]
            ub = [ta("ub%d" % i, 1024, BF16) for i in range(2)]
            utb = [ta("utb%d" % i, 1024, BF16) for i in range(2)]
            vb = [ta("vb%d" % i, 1024, BF16) for i in range(2)]

            def prep_load(i):
                q = i % 2
                A(deng, lambda e: e.dma_start(out=ust[q][:], in_=pu[i * 128:(i + 1) * 128, :]), w=["ust%d" % q], dma="ust%d" % q)
                A(deng, lambda e: e.dma_start(out=vst[q][:], in_=pv[i * 128:(i + 1) * 128, :]), w=["vst%d" % q], dma="vst%d" % q)

            def prep_tile(i):
                q = i % 2
                if i == 0:
                    prep_load(0)
                if i + 1 < NE:
                    prep_load(i + 1)
                A("act", lambda e: e.copy(out=ub[q][:], in_=ust[q][:]), r=["ust%d" % q], w=["ub%d" % q])
                bn_ = (4 + q) if pbank is None else pbank
                pb = "b%d" % bn_
                for c in range(8):
                    A("pe", lambda e: e.transpose(out=bkb(bn_)[:, c * 128:(c + 1) * 128], in_=ub[q][:, c * 128:(c + 1) * 128], identity=identb[:]),
                      r=["ub%d" % q, "identb"], w=[pb])
                A("act", lambda e: e.copy(out=utb[q][:], in_=bkb(bn_)), r=[pb], w=["utb%d" % q])
                A("act", lambda e: e.copy(out=vb[q][:], in_=vst[q][:]), r=["vst%d" % q], w=["vb%d" % q])
                A(deng, lambda e: e.dma_start(out=UTs[i], in_=utb[q][:]), r=["utb%d" % q], w=["UTs"], dma="sut%d" % q)
                A(deng, lambda e: e.dma_start(out=Vbs[i], in_=vb[q][:]), r=["vb%d" % q], w=["Vbs"], dma="svb%d" % q)
            return prep_tile

        if phases == ("prep",):
            phase_start()
            prep_tile = make_prep()
            for i in range(NE):
                prep_tile(i)

        winb = arena[:, 0:8 * INC].rearrange("p (c n) -> p c n", c=8)
        woutb = arena[:, 8 * INC:8 * INC + 8192].rearrange("p (c n) -> p c n", c=8)
        if "mod" in phases:
            phase_start()
            stage = ta("stage", 4096)
            ccol = ta("ccol", 8)
            sc = ta("sc", 8)
            screp = ta("screp", 1024)
            abT = ta("abT", 48)
            abrow = ta("abrow", 2048, parts=1)
            A("sp", lambda e: e.dma_start(out=ccol[:], in_=c_col), w=["ccol"], dma="c1_8")
            A("sp", lambda e: e.dma_start(out=abT[:], in_=ada_bT), w=["abT"], dma="c1_9")
            A("sp", lambda e: e.dma_start(out=abrow[:, 0:1024], in_=ada_b[:, 2048:3072]), w=["abrow"], dma="c1_10")
            A("sp", lambda e: e.dma_start(out=abrow[:, 1024:2048], in_=ada_b[:, 5120:6144]), w=["abrow"], dma="c1_11")
            A("act", lambda e: e.activation(out=sc[:], in_=ccol[:], func=AF.Silu), r=["ccol"], w=["sc"])
            A("dve", lambda e: e.tensor_copy(out=screp[:].rearrange("p (c m) -> p c m", c=8),
                                             in_=sc[:].unsqueeze(2).to_broadcast([128, 8, 128])), r=["sc"], w=["screp"])
            stageB = ta("stageB", 4096)
            wsts = [stage[:].rearrange("p (c n) -> p c n", c=8), stageB[:].rearrange("p (c n) -> p c n", c=8)]
            adv = ada_w.rearrange("(c p) n -> p c n", p=128)
            ones_row = cs[0:1, C_ONES:C_ONES + 128]
            wstg = [ta("wstg%d" % i, INC) for i in range(2)]
            wiv = w_in.rearrange("(c p) n -> p c n", p=128)
            wov = w_out.rearrange("(c p) n -> p c n", p=128)
            wchunks = [("in", c) for c in range(8)] + [("out", c) for c in range(8)]
            wci = [0]

            def stage_weight():
                if wci[0] >= len(wchunks):
                    return
                kind_, c_ = wchunks[wci[0]]
                k_ = wci[0] % 2
                wci[0] += 1
                if kind_ == "in":
                    A("sp", lambda e: e.dma_start(out=wstg[k_][:, 0:INC], in_=wiv[:, c_, :]), w=["wstg%d" % k_], dma="wstg%d" % k_)
                    A("dve", lambda e: e.tensor_copy(out=winb[:, c_, :], in_=wstg[k_][:, 0:INC]), r=["wstg%d" % k_], w=["winb"])
                else:
                    A("sp", lambda e: e.dma_start(out=wstg[k_][:, 0:1024], in_=wov[:, c_, :]), w=["wstg%d" % k_], dma="wstg%d" % k_)
                    A("dve", lambda e: e.tensor_copy(out=woutb[:, c_, :], in_=wstg[k_][:, 0:1024]), r=["wstg%d" % k_], w=["woutb"])

            def load_blk(blk):
                A("sp", lambda e: e.dma_start(out=wsts[blk % 2], in_=adv[:, :, blk * 512:(blk + 1) * 512]), w=["stage%d" % (blk % 2)], dma="stage%d" % (blk % 2))
            load_blk(0)
            for blk in range(12):
                if blk + 1 < 12:
                    load_blk(blk + 1)
                wst = wsts[blk % 2]
                sres = "stage%d" % (blk % 2)
                stage_weight()
                if blk % 3 != 2:
                    stage_weight()
                kind = blk // 2
                if kind in (2, 5):
                    gt = gate1 if kind == 2 else gate2
                    half = blk % 2
                    for c in range(8):
                        A("pe", lambda e, c=c: e.matmul(bk(2), lhsT=screp[:, c * 128:(c + 1) * 128], rhs=wst[:, c, :], start=(c == 0), stop=False),
                          r=["screp", sres], w=["b2"])
                    A("pe", lambda e, blk=blk: e.matmul(bk(2), lhsT=ones_row, rhs=abrow[0:1, ((blk - 4) if blk < 6 else (blk - 8)) * 512:((blk - 4) if blk < 6 else (blk - 8)) * 512 + 512], start=False, stop=True),
                      r=["cs", "abrow"], w=["b2"])
                    A("dve", lambda e, gt=gt, half=half: e.tensor_copy(out=gt[:, half * 512:(half + 1) * 512], in_=bk(2)), r=["b2"], w=["gate%d" % (1 if kind == 2 else 2)])
                else:
                    qi = {0: 0, 1: 1, 3: 2, 4: 3}[kind]
                    for nchunk in range(4):
                        col = qi * 8 + (blk % 2) * 4 + nchunk
                        for c in range(8):
                            A("pe", lambda e, c=c, nchunk=nchunk, col=col: e.matmul(bk(3)[:, col:col + 1], lhsT=wst[:, c, nchunk * 128:(nchunk + 1) * 128],
                                                                                    rhs=sc[:, c:c + 1], start=(c == 0), stop=(c == 7)),
                              r=[sres, "sc"], w=["b3"])
            while wci[0] < len(wchunks):
                stage_weight()
            for qi, bcol in enumerate((0, 8, 24, 32)):
                A("dve", lambda e, qi=qi, bcol=bcol: e.tensor_tensor(out=modT[:, qi * 8:(qi + 1) * 8], in0=bk(3)[:, qi * 8:(qi + 1) * 8],
                                                                     in1=abT[:, bcol:bcol + 8], op=ALU.add), r=["b3", "abT"], w=["modT"])
            A("dve", lambda e: e.scalar_tensor_tensor(out=gs1[:], in0=modT[:, 8:16], scalar=1.0, in1=n1s[:], op0=ALU.add, op1=ALU.mult),
              r=["modT", "n1s"], w=["gs1"])
            A("dve", lambda e: e.scalar_tensor_tensor(out=gs2[:], in0=modT[:, 24:32], scalar=1.0, in1=n2s[:], op0=ALU.add, op1=ALU.mult),
              r=["modT", "n2s"], w=["gs2"])
            if dbg:
                A("sp", lambda e: e.dma_start(out=dbg_out["d_mod"], in_=modT[:]), r=["modT"], w=["d_mod"], dma="dbg")
                A("sp", lambda e: e.dma_start(out=dbg_out["d_g1"], in_=gate1[:]), r=["gate1"], w=["d_g1"], dma="dbg")
        sh1 = modT[:, 0:8]
        sh2 = modT[:, 16:24]

        if "sub1" in phases:
            phase_start()
            gw = ta("gw", 256, parts=17)
            A("sp", lambda e: e.dma_start(out=gw[:], in_=gw17), w=["gw"], dma="c2_12")
            pwst = ta("pwst", 512)
            pwb = ta("pwb", 512, BF16)
            A("sp", lambda e: e.dma_start(out=pwst[:].rearrange("p (g d) -> p g d", g=4), in_=pool_w.rearrange("g c d -> c g d")), w=["pwst"], dma="c2_13")
            A("dve", lambda e: e.tensor_copy(out=pwb[:], in_=pwst[:]), r=["pwst"], w=["pwb"])
            state = ta("state", 512, parts=64)
            stateb = ta("stateb", 512, BF16, parts=64)
            A("dve", lambda e: e.memset(state[:], 0.0), w=["state"])
            A("dve", lambda e: e.memset(stateb[:], 0.0), w=["stateb"])
            xt2 = [ta("xt2_%d" % i, 2048) for i in range(2)]
            xt = [xt2[i][:, 0:1024] for i in range(2)]
            junk = ta("junk", 1024, BF16)
            ssq = ta("ssq", 8)
            pcur = [ta("pcur%d" % i, 512) for i in range(2)]
            prep_tile = None
            ta_mark = ta_off[0]
            xnbp2 = [ta("xnbp%d" % i, 2048, BF16) for i in range(2)]
            hTp2 = [ta("hTp%d" % i, 2048, BF16) for i in range(2)]
            ssqp2 = [ta("ssqp%d" % i, 8) for i in range(2)]
            sptp = ta("sptp", 512)
            ectp = ta("ectp", 512)
            kstp = ta("kstp", 512, BF16)
            vbfp = ta("vbfp", 1024, BF16)
            decp = ta("decp", 8, parts=64)
            alrp = ta("alrp", 256, parts=17)
            ssqp = ta("ssqp", 8)
            A("dve", lambda e: e.memset(alrp[:], 1.0), w=["alrp"])

            def load_x2(sidx, q):
                A("sp", lambda e: e.dma_start(out=xt2[q][:].rearrange("p (u d) -> p u d", u=2),
                                              in_=x_pre[sidx * 256:(sidx + 1) * 256, :].rearrange("(u p) d -> p u d", p=128)), w=["xt%d" % q], dma="xt%d" % q)

            def prefix_A(sidx, q):
                xres = "xt%d" % q
                xv = xt2[q][:].rearrange("p (u d) -> p u d", u=2)
                xn_, hT_, sq_ = xnbp2[q], hTp2[q], ssqp2[q]
                for u in range(2):
                    A("act", lambda e: e.activation(out=junk[:], in_=xv[:, u, :], func=AF.Square, accum_out=sq_[:, u:u + 1]), r=[xres], w=["junk", "ssqp%d" % q])
                A("act", lambda e: e.activation(out=sq_[:, 2:4], in_=sq_[:, 0:2], func=AF.Ln, scale=1.0 / D, bias=epsc[:, 0:1]), r=["ssqp%d" % q, "epsc"], w=["ssqp1%d" % q])
                A("act", lambda e: e.activation(out=sq_[:, 4:6], in_=sq_[:, 2:4], func=AF.Exp, scale=-0.5), r=["ssqp1%d" % q], w=["ssqp2%d" % q])
                for u in range(2):
                    A("act", lambda e: e.activation(out=xn_[:, u * 1024:(u + 1) * 1024], in_=xv[:, u, :], func=AF.Copy, scale=sq_[:, 4 + u:5 + u]), r=[xres, "ssqp2%d" % q], w=["xnbp%d" % q])
                for u in range(2):
                    for c in range(8):
                        b = c // 4
                        po = ((c % 4) * 2 + u) * 128
                        A("pe", lambda e: e.transpose(out=bkb(b)[:, po:po + 128], in_=xn_[:, u * 1024 + c * 128:u * 1024 + (c + 1) * 128], identity=identb[:]),
                          r=["xnbp%d" % q, "identb"], w=["b%d" % b])
                for c in range(8):
                    b = c // 4
                    A("dve", lambda e: e.tensor_scalar(out=hT_[:, c * 256:(c + 1) * 256], in0=bkb(b)[:, (c % 4) * 256:(c % 4 + 1) * 256],
                                                       scalar1=gs1[:, c:c + 1], scalar2=sh1[:, c:c + 1], op0=ALU.mult, op1=ALU.add),
                      r=["b%d" % b, "gs1", "modT"], w=["hTp%d" % q])

            def prefix_B(sidx, q):
                hT_ = hTp2[q]
                hres = "hTp%d" % q
                last = sidx == NPRE // 2 - 1
                for u in range(2):
                    for c in range(8):
                        A("pe", lambda e: e.matmul(bk(2)[:, u * 256:(u + 1) * 256], lhsT=hT_[:, (c * 2 + u) * 128:(c * 2 + u + 1) * 128], rhs=winb[:, c, 256:512], start=(c == 0), stop=(c == 7)),
                          r=[hres, "winb"], w=["b2"])
                    for c in range(8):
                        A("pe", lambda e: e.matmul(bk(4 + u), lhsT=hT_[:, (c * 2 + u) * 128:(c * 2 + u + 1) * 128], rhs=winb[:, c, 512:1024], start=(c == 0), stop=(c == 7)),
                          r=[hres, "winb"], w=["b%d" % (4 + u)])
                for c in range(8):
                    A("pe", lambda e: e.matmul(bk(3)[0:16, 0:256], lhsT=winb[:, c, 2048:2064], rhs=hT_[:, c * 256:(c + 1) * 256], start=(c == 0), stop=(c == 7)),
                      r=[hres, "winb"], w=["b3"])
                A("act", lambda e: e.copy(out=alrp[0:16, :], in_=bk(3)[0:16, 0:256]), r=["b3"], w=["alrp"])
                for u in range(2):
                    A("pe", lambda e: e.matmul(bk(3)[:, u * 256:(u + 1) * 256], lhsT=alrp[:, u * 128:(u + 1) * 128], rhs=gw[:], start=True, stop=True), r=["alrp", "gw"], w=["b3"])
                A("act", lambda e: e.activation(out=ectp[:], in_=bk(3), func=AF.Exp, scale=-1.0), r=["b3"], w=["ectp"])
                A("act", lambda e: e.activation(out=sptp[:], in_=ectp[:], func=AF.Ln, bias=1.0), r=["ectp"], w=["sptp"])
                for u in range(2):
                    A("pe", lambda e: e.matmul(bk(6)[:, u * 256:(u + 1) * 256], lhsT=cs[:, C_TRIU:C_TRIU + 128], rhs=sptp[:, u * 256:(u + 1) * 256], start=True, stop=True), r=["cs", "sptp"], w=["b6"])
                    for h in range(4):
                        A("pe", lambda e: e.matmul(bk(3)[0:64, u * 4 + h:u * 4 + h + 1], lhsT=sptp[:, u * 256 + h * 64:u * 256 + (h + 1) * 64], rhs=cs[:, C_NEG:C_NEG + 1], start=True, stop=True),
                          r=["cs", "sptp"], w=["b3"])
                A("act", lambda e: e.activation(out=ectp[:], in_=bk(6), func=AF.Exp), r=["b6"], w=["ectp"])
                A("dve", lambda e: e.tensor_tensor(out=kstp[:], in0=bk(2), in1=ectp[:], op=ALU.mult), r=["b2", "ectp"], w=["kstp"])
                A("act", lambda e: e.activation(out=decp[:], in_=bk(3)[0:64, 0:8], func=AF.Exp), r=["b3"], w=["decp"])
                for u in range(2):
                    ti = 2 * sidx + u
                    A("act", lambda e: e.activation(out=vbfp[:, u * 512:(u + 1) * 512], in_=bk(4 + u), func=AF.Copy, scale=cs2[:, 512 + ti:513 + ti]), r=["b%d" % (4 + u), "cs2"], w=["vbfp"])
                if last:
                    for c in range(8):
                        A("pe", lambda e: e.matmul(bk(7), lhsT=hT_[:, (c * 2 + 1) * 128:(c * 2 + 2) * 128], rhs=winb[:, c, 1536:2048], start=(c == 0), stop=(c == 7)),
                          r=[hres, "winb"], w=["b7"])
                    A("act", lambda e: e.activation(out=pcur[1][:], in_=bk(7), func=AF.Copy, scale=cs2[:, 512 + NPRE - 1:512 + NPRE]), r=["b7", "cs2"], w=["pcur1"])
                for u in range(2):
                    for h in range(4):
                        A("pe", lambda e: e.matmul(bk(4 + u)[0:64, h * 128:(h + 1) * 128], lhsT=kstp[:, u * 256 + h * 64:u * 256 + (h + 1) * 64], rhs=vbfp[:, u * 512 + h * 128:u * 512 + (h + 1) * 128], start=True, stop=True),
                          r=["kstp", "vbfp"], w=["b%d" % (4 + u)])
                st3 = state[:].rearrange("p (h v) -> p h v", h=4)
                for u in range(2):
                    A("dve", lambda e: e.tensor_tensor(out=st3, in0=st3, in1=decp[:, u * 4:(u + 1) * 4].unsqueeze(2).to_broadcast([64, 4, 128]), op=ALU.mult), r=["state", "decp"], w=["state"])
                    A("dve", lambda e: e.tensor_tensor(out=state[:], in0=state[:], in1=bk(4 + u)[0:64, :], op=ALU.add), r=["state", "b%d" % (4 + u)], w=["state"])
                if last:
                    A("act", lambda e: e.copy(out=stateb[:], in_=state[:]), r=["state"], w=["stateb"])

            NS = NPRE // 2
            load_x2(0, 0)
            load_x2(1, 1)
            prefix_A(0, 0)
            for sidx in range(NS):
                if sidx + 1 < NS:
                    prefix_A(sidx + 1, (sidx + 1) % 2)
                if sidx + 2 < NS:
                    load_x2(sidx + 2, sidx % 2)
                elif sidx + 2 == NS:
                    A("sp", lambda e: e.dma_start(out=xt[0], in_=x_own[0:128, :]), w=["xt0"], dma="xt0")
                prefix_B(sidx, sidx % 2)
            P.barrier()
            ta_off[0] = ta_mark
            alr = ta("alr", 128, parts=17)
            A("dve", lambda e: e.memset(alr[:], 1.0), w=["alr"])
            xnb = ta("xnb", 1024, BF16)
            hT = ta("hT", 1024, BF16)
            spt = ta("spt", 256)
            ect = ta("ect", 256)
            kst = ta("kst", 256, BF16)
            vbf = ta("vbf", 512, BF16)
            dec = ta("dec", 4, parts=64)
            ebt = ta("ebt", 512, parts=64)
            enbt = ta("enbt", 512, parts=64)
            qin = ta("qin", 512, BF16, parts=64)
            kin = ta("kin", 512, BF16, parts=64)
            attn = ta("attn", 512, BF16)
            sg = ta("sg", 512)
            yg = ta("yg", 512)
            ygb = ta("ygb", 512, BF16)
            yglaT = ta("yglaT", 512, BF16)
            poolT = ta("poolT", 512, BF16)
            ypoolT = ta("ypoolT", 512, BF16)
            x1t = ta("x1t", 1024)
            h2T = ta("h2T", 1024, BF16)
            ssqo = ta("ssqo", 8)

            def load_x(kind, i, q):
                src = x_pre if kind == "pre" else x_own
                A("sp", lambda e: e.dma_start(out=xt[q][:], in_=src[i * 128:(i + 1) * 128, :]), w=["xt%d" % q], dma="xt%d" % q)

            def norm_T(src_ap, src_res, gs, sh, dst, dst_res, bank):
                pb = "b%d" % bank
                A("act", lambda e: e.activation(out=junk[:], in_=src_ap, func=AF.Square, accum_out=ssq[:, 0:1]), r=[src_res], w=["junk", "ssq"])
                A("act", lambda e: e.activation(out=ssq[:, 1:2], in_=ssq[:, 0:1], func=AF.Ln, scale=1.0 / D, bias=epsc[:, 0:1]), r=["ssq", "epsc"], w=["ssq1"])
                A("act", lambda e: e.activation(out=ssq[:, 2:3], in_=ssq[:, 1:2], func=AF.Exp, scale=-0.5), r=["ssq1"], w=["ssq2"])
                A("act", lambda e: e.activation(out=xnb[:], in_=src_ap, func=AF.Copy, scale=ssq[:, 2:3]), r=[src_res, "ssq2"], w=["xnb"])
                for c in range(8):
                    A("pe", lambda e, c=c: e.transpose(out=bkb(bank)[:, c * 128:(c + 1) * 128], in_=xnb[:, c * 128:(c + 1) * 128], identity=identb[:]),
                      r=["xnb", "identb"], w=[pb])
                for c in range(8):
                    eng = "dve" if c % 2 == 0 else "pool"
                    eng = "dve"
                    A(eng, lambda e, c=c: e.tensor_scalar(out=dst[:, c * 128:(c + 1) * 128], in0=bkb(bank)[:, c * 128:(c + 1) * 128],
                                                          scalar1=gs[:, c:c + 1], scalar2=sh[:, c:c + 1], op0=ALU.mult, op1=ALU.add),
                      r=[pb, "gs1", "gs2", "modT"], w=[dst_res])

            def sub1_tile(kind, i, gidx):
                own = kind == "own"
                q = gidx % 2
                last_pre = (kind == "pre" and i == NPRE - 1)
                if own:
                    B = dict(T=0, K=1, V=2, G=3, Pp=4, Q=5, KT=6, M=7)
                else:
                    o = 0
                    B = dict(T=o, K=o + 1, V=o + 2, M=o + 3, Pp=6)
                bn = {k: "b%d" % v for k, v in B.items()}
                xres = "xt%d" % q
                norm_T(xt[q][:], xres, gs1, sh1, hT, "hT", B["T"])
                hTv = hT[:].rearrange("p (c t) -> p c t", c=8)
                def proj(bank, c0, c1, o0=0):
                    for c in range(8):
                        A("pe", lambda e, c=c: e.matmul(bk(bank)[:, o0:o0 + (c1 - c0)], lhsT=hTv[:, c, :], rhs=winb[:, c, c0:c1], start=(c == 0), stop=(c == 7)),
                          r=["hT", "winb"], w=["b%d" % bank])
                proj(B["K"], 256, 512)
                proj(B["V"], 512, 1024)
                if own:
                    proj(B["G"], 1024, 1536)
                if own or last_pre:
                    proj(B["Pp"], 1536, 2048)
                for c in range(8):
                    A("pe", lambda e, c=c: e.matmul(bk(B["M"])[0:16, 0:128], lhsT=winb[:, c, 2048:2064], rhs=hTv[:, c, :], start=(c == 0), stop=(c == 7)),
                      r=["hT", "winb"], w=[bn["M"]])
                if own:
                    for h in range(4):
                        for c in range(8):
                            A("pe", lambda e, c=c, h=h: e.matmul(bk(B["Q"])[0:64, h * 128:(h + 1) * 128], lhsT=winb[:, c, h * 64:(h + 1) * 64], rhs=hTv[:, c, :],
                                                                 start=(c == 0), stop=(c == 7)), r=["hT", "winb"], w=[bn["Q"]])
                    for h in range(4):
                        for c in range(8):
                            A("pe", lambda e, c=c, h=h: e.matmul(bk(B["KT"])[0:64, h * 128:(h + 1) * 128], lhsT=winb[:, c, 256 + h * 64:256 + (h + 1) * 64], rhs=hTv[:, c, :],
                                                                 start=(c == 0), stop=(c == 7)), r=["hT", "winb"], w=[bn["KT"]])
                A("act", lambda e: e.copy(out=alr[0:16, :], in_=bk(B["M"])[0:16, 0:128]), r=[bn["M"]], w=["alr"])
                A("pe", lambda e: e.matmul(bk(B["K"])[:, 256:512], lhsT=alr[:], rhs=gw[:], start=True, stop=True), r=["alr", "gw"], w=[bn["K"]])
                A("act", lambda e: e.activation(out=ect[:], in_=bk(B["K"])[:, 256:512], func=AF.Exp, scale=-1.0), r=[bn["K"]], w=["ect"])
                A("act", lambda e: e.activation(out=spt[:], in_=ect[:], func=AF.Ln, bias=1.0), r=["ect"], w=["spt"])
                A("pe", lambda e: e.matmul(bk(B["M"])[:, 128:384], lhsT=cs[:, C_TRIU:C_TRIU + 128], rhs=spt[:], start=True, stop=True), r=["cs", "spt"], w=[bn["M"]])
                for h in range(4):
                    A("pe", lambda e, h=h: e.matmul(bk(B["M"])[0:64, 384 + h:385 + h], lhsT=spt[:, h * 64:(h + 1) * 64], rhs=cs[:, C_NEG:C_NEG + 1], start=True, stop=True),
                      r=["cs", "spt"], w=[bn["M"]])
                if own:
                    for h in range(4):
                        A("pe", lambda e, h=h: e.matmul(bk(B["T"])[0:64, h * 128:(h + 1) * 128], lhsT=spt[:, h * 64:(h + 1) * 64], rhs=cs[:, C_TRIL:C_TRIL + 128], start=True, stop=True),
                          r=["cs", "spt"], w=[bn["T"]])
                A("act", lambda e: e.activation(out=ect[:], in_=bk(B["M"])[:, 128:384], func=AF.Exp), r=[bn["M"]], w=["ect"])
                A("dve", lambda e: e.tensor_tensor(out=kst[:], in0=bk(B["K"])[:, 0:256], in1=ect[:], op=ALU.mult), r=[bn["K"], "ect"], w=["kst"])
                A("act", lambda e: e.activation(out=dec[:], in_=bk(B["M"])[0:64, 384:388], func=AF.Exp), r=[bn["M"]], w=["dec"])
                if own:
                    A("act", lambda e: e.copy(out=vbf[:], in_=bk(B["V"])), r=[bn["V"]], w=["vbf"])
                else:
                    A("act", lambda e: e.activation(out=vbf[:], in_=bk(B["V"]), func=AF.Copy, scale=cs2[:, 512 + i:513 + i]), r=[bn["V"], "cs2"], w=["vbf"])
                if own:
                    A("act", lambda e: e.activation(out=ebt[:], in_=bk(B["T"])[0:64, :], func=AF.Exp), r=[bn["T"]], w=["ebt"])
                    A("act", lambda e: e.activation(out=enbt[:], in_=bk(B["T"])[0:64, :], func=AF.Exp, scale=-1.0), r=[bn["T"]], w=["enbt"])
                    A("dve", lambda e: e.scalar_tensor_tensor(out=qin[:], in0=bk(B["Q"])[0:64, :], scalar=0.125, in1=ebt[:], op0=ALU.mult, op1=ALU.mult),
                      r=[bn["Q"], "ebt"], w=["qin"])
                    A("dve", lambda e: e.tensor_tensor(out=kin[:], in0=bk(B["KT"])[0:64, :], in1=enbt[:], op=ALU.mult), r=[bn["KT"], "enbt"], w=["kin"])
                    for h in range(4):
                        A("pe", lambda e, h=h: e.matmul(bk(B["M"])[:, h * 128:(h + 1) * 128], lhsT=kin[:, h * 128:(h + 1) * 128], rhs=qin[:, h * 128:(h + 1) * 128], start=True, stop=True),
                          r=["kin", "qin"], w=[bn["M"]])
                    A("dve", lambda e: e.tensor_tensor(out=attn[:].rearrange("p (h i) -> p h i", h=4), in0=bk(B["M"]).rearrange("p (h i) -> p h i", h=4),
                                                       in1=cs[:, C_CAUS:C_CAUS + 128].unsqueeze(1).to_broadcast([128, 4, 128]), op=ALU.mult),
                      r=[bn["M"], "cs"], w=["attn"])
                    for h in range(4):
                        A("pe", lambda e, h=h: e.matmul(bk(B["Q"])[:, h * 128:(h + 1) * 128], lhsT=attn[:, h * 128:(h + 1) * 128], rhs=vbf[:, h * 128:(h + 1) * 128], start=True, stop=False),
                          r=["attn", "vbf"], w=[bn["Q"]])
                        A("pe", lambda e, h=h: e.matmul(bk(B["Q"])[:, h * 128:(h + 1) * 128], lhsT=qin[:, h * 128:(h + 1) * 128], rhs=stateb[:, h * 128:(h + 1) * 128], start=False, stop=True),
                          r=["qin", "stateb"], w=[bn["Q"]])
                KVb = B["KT"] if own else B["T"]
                for h in range(4):
                    A("pe", lambda e, h=h: e.matmul(bk(KVb)[0:64, h * 128:(h + 1) * 128], lhsT=kst[:, h * 64:(h + 1) * 64], rhs=vbf[:, h * 128:(h + 1) * 128], start=True, stop=True),
                      r=["kst", "vbf"], w=["b%d" % KVb])
                for h in range(4):
                    A("dve", lambda e, h=h: e.scalar_tensor_tensor(out=state[:, h * 128:(h + 1) * 128], in0=state[:, h * 128:(h + 1) * 128], scalar=dec[:, h:h + 1],
                                                                   in1=bk(KVb)[0:64, h * 128:(h + 1) * 128], op0=ALU.mult, op1=ALU.add),
                      r=["b%d" % KVb, "dec", "state"], w=["state"])
                A("act", lambda e: e.copy(out=stateb[:], in_=state[:]), r=["state"], w=["stateb"])
                if last_pre:
                    A("act", lambda e: e.activation(out=pcur[1][:], in_=bk(B["Pp"]), func=AF.Copy, scale=cs2[:, 512 + i:513 + i]), r=[bn["Pp"], "cs2"], w=["pcur1"])
                if not own:
                    return
                for h in range(4):
                    A("act", lambda e, h=h: e.activation(out=junk[:, 0:128], in_=bk(B["Q"])[:, h * 128:(h + 1) * 128], func=AF.Square, accum_out=ssqo[:, h:h + 1]),
                      r=[bn["Q"]], w=["junk", "ssqo"])
                A("act", lambda e: e.activation(out=ssqo[:, 4:8], in_=ssqo[:, 0:4], func=AF.Ln, scale=1.0 / 128, bias=epsc[:, 0:1]), r=["ssqo", "epsc"], w=["ssqo1"])
                A("act", lambda e: e.activation(out=ssqo[:, 0:4], in_=ssqo[:, 4:8], func=AF.Exp, scale=-0.5), r=["ssqo1"], w=["ssqo"])
                A("act", lambda e: e.activation(out=sg[:], in_=bk(B["G"]), func=AF.Silu), r=[bn["G"]], w=["sg"])
                for h in range(4):
                    A("dve", lambda e, h=h: e.scalar_tensor_tensor(out=yg[:, h * 128:(h + 1) * 128], in0=bk(B["Q"])[:, h * 128:(h + 1) * 128], scalar=ssqo[:, h:h + 1],
                                                                   in1=sg[:, h * 128:(h + 1) * 128], op0=ALU.mult, op1=ALU.mult),
                      r=[bn["Q"], "ssqo", "sg"], w=["yg"])
                A("dve", lambda e: e.tensor_tensor(out=ygb[:].rearrange("p (h v) -> p h v", h=4), in0=yg[:].rearrange("p (h v) -> p h v", h=4),
                                                   in1=gngr[:].unsqueeze(1).to_broadcast([128, 4, 128]), op=ALU.mult), r=["yg", "gngr"], w=["ygb"])
                for h in range(4):
                    A("pe", lambda e, h=h: e.transpose(out=bkb(B["T"])[:, h * 128:(h + 1) * 128], in_=ygb[:, h * 128:(h + 1) * 128], identity=identb[:]),
                      r=["ygb", "identb"], w=[bn["T"]])
                A("act", lambda e: e.copy(out=yglaT[:], in_=bkb(B["T"])[:, 0:512]), r=[bn["T"]], w=["yglaT"])
                pc = i % 2
                pp = 1 - pc
                A("act", lambda e: e.copy(out=pcur[pc][:], in_=bk(B["Pp"])), r=[bn["Pp"]], w=["pcur%d" % pc])
                for g in range(4):
                    ba = cs2[:, g * 128:(g + 1) * 128] if i == 0 else cs[:, C_BA + g * 128:C_BA + (g + 1) * 128]
                    A("pe", lambda e, g=g, ba=ba: e.matmul(bk(B["K"])[:, g * 128:(g + 1) * 128], lhsT=pcur[pc][:, g * 128:(g + 1) * 128], rhs=ba, start=True, stop=False),
                      r=["pcur%d" % pc, "cs", "cs2"], w=[bn["K"]])
                    A("pe", lambda e, g=g: e.matmul(bk(B["K"])[:, g * 128:(g + 1) * 128], lhsT=pcur[pp][:, g * 128:(g + 1) * 128], rhs=cs[:, C_BB + g * 128:C_BB + (g + 1) * 128], start=False, stop=True),
                      r=["pcur%d" % pp, "cs"], w=[bn["K"]])
                A("act", lambda e: e.copy(out=poolT[:], in_=bk(B["K"])), r=[bn["K"]], w=["poolT"])
                for g in range(4):
                    A("pe", lambda e, g=g: e.matmul(bk(B["Pp"])[:, g * 128:(g + 1) * 128], lhsT=pwb[:, g * 128:(g + 1) * 128], rhs=poolT[:, g * 128:(g + 1) * 128], start=True, stop=True),
                      r=["pwb", "poolT"], w=[bn["Pp"]])
                for g in range(4):
                    A("dve", lambda e, g=g: e.tensor_scalar(out=ypoolT[:, g * 128:(g + 1) * 128], in0=bk(B["Pp"])[:, g * 128:(g + 1) * 128], scalar1=pscs[:, g:g + 1], scalar2=None, op0=ALU.mult),
                      r=[bn["Pp"], "pscs"], w=["ypoolT"])
                for nb in range(2):
                    bank = B["V"] if nb == 0 else B["G"]
                    for c in range(8):
                        lt = yglaT[:, c * 128:(c + 1) * 128] if c < 4 else ypoolT[:, (c - 4) * 128:(c - 3) * 128]
                        A("pe", lambda e, c=c, lt=lt, bank=bank, nb=nb: e.matmul(bk(bank), lhsT=lt, rhs=woutb[:, c, nb * 512:(nb + 1) * 512], start=(c == 0), stop=(c == 7)),
                          r=["yglaT", "ypoolT", "woutb"], w=["b%d" % bank])
                for nb in range(2):
                    bank = B["V"] if nb == 0 else B["G"]
                    A("dve", lambda e, nb=nb, bank=bank: e.tensor_tensor(out=x1t[:, nb * 512:(nb + 1) * 512], in0=bk(bank), in1=gate1[:, nb * 512:(nb + 1) * 512], op=ALU.mult),
                      r=["b%d" % bank, "gate1"], w=["x1t"])
                A("dve", lambda e: e.tensor_tensor(out=x1t[:], in0=x1t[:], in1=xt[q][:], op=ALU.add), r=["x1t", xres], w=["x1t"])
                A("sp", lambda e: e.dma_start(out=x1s[i * 128:(i + 1) * 128, :], in_=x1t[:]), r=["x1t"], w=["x1s"], dma="sx1")
                if dbg:
                    A("sp", lambda e: e.dma_start(out=dbg_out["d_x1"][i * 128:(i + 1) * 128, :], in_=x1t[:]), r=["x1t"], w=["d_x1"], dma="dbg")
                norm_T(x1t[:], "x1t", gs2, sh2, h2T, "h2T", B["T"])
                A("sp", lambda e: e.dma_start(out=h2s[i], in_=h2T[:]), r=["h2T"], w=["h2s"], dma="sh2")

            for i in range(NT):
                if i + 1 < NT:
                    load_x("own", i + 1, (i + 1) % 2)
                sub1_tile("own", i, i)

        WT = sb("WT", NTOK, BF16)
        I1T = sb("I1T", NTOK, BF16)
        I2T = sb("I2T", NTOK, BF16)
        if "route" in phases:
            phase_start()
            stage = ta("stage", 4096)
            wqb = arena[:, 0:8 * 2048].rearrange("p (c n) -> p c n", c=8)
            wqv = wq.rearrange("(c p) n -> p c n", p=128)
            for c in range(8):
                hs = slice((c % 2) * 2048, (c % 2 + 1) * 2048)
                A("sp", lambda e: e.dma_start(out=stage[:, hs], in_=wqv[:, c, :]), w=["stageh%d" % (c % 2)], dma="stageh%d" % (c % 2))
                A("dve", lambda e: e.tensor_copy(out=wqb[:, c, :], in_=stage[:, hs]), r=["stageh%d" % (c % 2)], w=["wqb"])
            kT = ta("kT", 256, BF16)
            for hi, kk in enumerate((k1, k2)):
                A("sp", lambda e, kk=kk: e.dma_start(out=stage[:, 0:128], in_=kk), w=["stage", "stageh0"], dma="stage")
                A("pe", lambda e: e.transpose(out=bk(0)[:, 0:128], in_=stage[:, 0:128], identity=ident), r=["stage", "cs"], w=["b0"])
                A("dve", lambda e, hi=hi: e.tensor_copy(out=kT[:, hi * 128:(hi + 1) * 128], in_=bk(0)[:, 0:128]), r=["b0"], w=["kT"])
            h2t = [ta("h2t%d" % i, 1024, BF16) for i in range(2)]
            qpT = ta("qpT", 2048, BF16)
            ssb = ta("ssb", 2048)
            swk = ta("swk", 2048)
            vv = ta("vv", 256)
            idx = ta("idx", 256, U32)
            cand = ta("cand", 2048)
            cwk = ta("cwk", 2048)
            tsv = ta("tsv", 128)
            pos = ta("pos", 128, U32)
            wg = ta("wg", 128)
            zz = ta("zz", 16)
            ai = ta("ai", 128, I32)
            bi_ = ta("bi", 128, I32)
            af = ta("af", 128)
            bf = ta("bf", 128)
            i1f = ta("i1f", 128)
            i2f = ta("i2f", 128)
            oh = ta("oh", 2048)
            isel = ta("isel", 256)

            def load_h2(n):
                q = n % 2
                A("sp", lambda e: e.dma_start(out=h2t[q][:], in_=h2s[n]), r=["h2s"], w=["h2t%d" % q], dma="h2t%d" % q)
            prep_tile = make_prep(7, "sp") if "prep" in phases else None
            ssb2 = [ssb, stage[:, 0:2048]]
            sbank = lambda j: (4 + j // 4) if j < 12 else 3

            def route_front(n):
                q = n % 2
                ssb_ = ssb2[q]
                hv = h2t[q][:].rearrange("p (c t) -> p c t", c=8)
                if dbg:
                    A("sp", lambda e: e.dma_start(out=dbg_out["d_h2"][n], in_=h2t[q][:]), r=["h2t%d" % q], w=["d_h2"], dma="dbg3")
                for j in range(16):
                    for c in range(8):
                        A("pe", lambda e: e.matmul(bk(j // 4)[:, (j % 4) * 128:(j % 4 + 1) * 128], lhsT=wqb[:, c, j * 128:(j + 1) * 128], rhs=hv[:, c, :], start=(c == 0), stop=(c == 7)),
                          r=["wqb", "h2t%d" % q], w=["b%d" % (j // 4)])
                for b4 in range(4):
                    A("act", lambda e: e.copy(out=qpT[:, b4 * 512:(b4 + 1) * 512], in_=bk(b4)), r=["b%d" % b4], w=["qpT%d" % b4])
                for j in range(16):
                    A("pe", lambda e: e.matmul(bk(sbank(j))[:, (j % 4) * 128:(j % 4 + 1) * 128], lhsT=qpT[:, j * 128:(j + 1) * 128], rhs=kT[:, (j % 2) * 128:(j % 2 + 1) * 128], start=True, stop=True),
                      r=["qpT%d" % (j // 4), "kT"], w=["b%d" % sbank(j)])
                for b4 in range(4):
                    A("act", lambda e: e.copy(out=ssb_[:, b4 * 512:(b4 + 1) * 512], in_=bk(sbank(b4 * 4))), r=["b%d" % sbank(b4 * 4)], w=["ssb%d_%d" % (q, b4)])
                if n + 2 < NT:
                    load_h2(n + 2)

            vvb = [vv, ta("vv_1", 256)]
            idxb = [idx, ta("idx_1", 256, U32)]
            tsvb = [tsv, ta("tsv_1", 128)]
            posb = [pos, ta("pos_1", 128, U32)]

            def back_body(n, lists):
                q = n % 2
                rp = "p%d_" % q
                ssb = ssb2[q]
                vv, idx, tsv, pos = vvb[q], idxb[q], tsvb[q], posb[q]
                cap[0] = lists[0]
                for ph in range(5):
                    for j in range(16):
                        sl = slice(j * 128, (j + 1) * 128)
                        sr = "ssb%d_%d" % (q, j // 4)
                        vr = rp + "vv%d" % j
                        if ph == 0:
                            A("dve", lambda e: e.max(out=vv[:, j * 16:j * 16 + 8], in_=ssb[:, sl]), r=[sr], w=[vr + "a"])
                        elif ph == 1:
                            A("dve", lambda e: e.max_index(out=idx[:, j * 16:j * 16 + 8], in_max=vv[:, j * 16:j * 16 + 8], in_values=ssb[:, sl]), r=[sr, vr + "a"], w=[rp + "idx%da" % j])
                        elif ph == 2:
                            A("dve", lambda e: e.match_replace(out=swk[:, sl], in_to_replace=vv[:, j * 16:j * 16 + 8], in_values=ssb[:, sl], imm_value=-1e30), r=[sr, vr + "a"], w=["swk%d" % j])
                        elif ph == 3:
                            A("dve", lambda e: e.max(out=vv[:, j * 16 + 8:j * 16 + 16], in_=swk[:, sl]), r=["swk%d" % j], w=[vr + "b"])
                        else:
                            A("dve", lambda e: e.max_index(out=idx[:, j * 16 + 8:j * 16 + 16], in_max=vv[:, j * 16 + 8:j * 16 + 16], in_values=swk[:, sl]), r=["swk%d" % j, vr + "b"], w=[rp + "idx%db" % j])
                vvall = [rp + "vv%da" % j for j in range(16)] + [rp + "vv%db" % j for j in range(16)]
                idxall = [rp + "idx%da" % j for j in range(16)] + [rp + "idx%db" % j for j in range(16)]
                vv4 = vv[:].rearrange("p (h s a) -> p h s a", h=8, s=2)
                A("dve", lambda e: e.tensor_tensor(out=cand[:].rearrange("p (h a b) -> p h a b", h=8, a=16),
                                                   in0=vv4[:, :, 0, :].unsqueeze(3).to_broadcast([128, 8, 16, 16]),
                                                   in1=vv4[:, :, 1, :].unsqueeze(2).to_broadcast([128, 8, 16, 16]), op=ALU.add), r=vvall, w=["cand"])
                for ph in range(5):
                    for h in range(8):
                        sl = slice(h * 256, (h + 1) * 256)
                        tr = rp + "tsv%d" % h
                        if ph == 0:
                            A("dve", lambda e: e.max(out=tsv[:, h * 16:h * 16 + 8], in_=cand[:, sl]), r=["cand"], w=[tr + "a"])
                        elif ph == 1:
                            A("dve", lambda e: e.max_index(out=pos[:, h * 16:h * 16 + 8], in_max=tsv[:, h * 16:h * 16 + 8], in_values=cand[:, sl]), r=["cand", tr + "a"], w=[rp + "pos%da" % h])
                        elif ph == 2:
                            A("dve", lambda e: e.match_replace(out=cwk[:, sl], in_to_replace=tsv[:, h * 16:h * 16 + 8], in_values=cand[:, sl], imm_value=-1e30), r=["cand", tr + "a"], w=["cwk%d" % h])
                        elif ph == 3:
                            A("dve", lambda e: e.max(out=tsv[:, h * 16 + 8:h * 16 + 16], in_=cwk[:, sl]), r=["cwk%d" % h], w=[tr + "b"])
                        else:
                            A("dve", lambda e: e.max_index(out=pos[:, h * 16 + 8:h * 16 + 16], in_max=tsv[:, h * 16 + 8:h * 16 + 16], in_values=cwk[:, sl]), r=["cwk%d" % h, tr + "b"], w=[rp + "pos%db" % h])
                tsvall = [rp + "tsv%da" % h for h in range(8)] + [rp + "tsv%db" % h for h in range(8)]
                posall = [rp + "pos%da" % h for h in range(8)] + [rp + "pos%db" % h for h in range(8)]
                cap[0] = lists[1]
                ts3 = tsv[:].rearrange("p (h k) -> p h k", h=8)
                A("dve", lambda e: e.tensor_tensor(out=wg[:].rearrange("p (h k) -> p h k", h=8), in0=ts3, in1=ts3[:, :, 0:1].to_broadcast([128, 8, 16]), op=ALU.subtract), r=tsvall, w=["wg"])
                A("act", lambda e: e.activation(out=wg[:], in_=wg[:], func=AF.Exp), r=["wg"], w=["wg"])
                A("dve", lambda e: e.tensor_reduce(out=zz[:, 0:8], in_=wg[:].rearrange("p (h k) -> p h k", h=8), axis=AX.X, op=ALU.add), r=["wg"], w=["zz"])
                A("dve", lambda e: e.reciprocal(out=zz[:, 8:16], in_=zz[:, 0:8]), r=["zz"], w=["zz1"])
                A("dve", lambda e: e.tensor_tensor(out=wg[:].rearrange("p (h k) -> p h k", h=8), in0=wg[:].rearrange("p (h k) -> p h k", h=8),
                                                   in1=zz[:, 8:16].unsqueeze(2).to_broadcast([128, 8, 16]), op=ALU.mult), r=["wg", "zz1"], w=["wg"])
                A("dve", lambda e: e.tensor_single_scalar(out=ai[:], in_=pos[:].bitcast(I32), scalar=4, op=ALU.logical_shift_right), r=posall, w=["ai"])
                A("dve", lambda e: e.tensor_single_scalar(out=bi_[:], in_=pos[:].bitcast(I32), scalar=15, op=ALU.bitwise_and), r=posall, w=["bi"])
                A("dve", lambda e: e.tensor_copy(out=af[:], in_=ai[:]), r=["ai"], w=["af"])
                A("dve", lambda e: e.tensor_copy(out=bf[:], in_=bi_[:]), r=["bi"], w=["bf"])
                idx4 = idx[:].rearrange("p (h s a) -> p h s a", h=8, s=2)
                A("dve", lambda e: e.tensor_copy(out=i1f[:].rearrange("p (h a) -> p h a", h=8), in_=idx4[:, :, 0, :]), r=idxall, w=["i1f"])
                A("dve", lambda e: e.tensor_copy(out=i2f[:].rearrange("p (h a) -> p h a", h=8), in_=idx4[:, :, 1, :]), r=idxall, w=["i2f"])
                io16 = cs[:, C_IOTA16:C_IOTA16 + 16]
                for which, (cf, inf) in enumerate(((af, i1f), (bf, i2f))):
                    A("dve", lambda e, cf=cf: e.tensor_tensor(out=oh[:].rearrange("p (s a) -> p s a", a=16),
                                                              in0=io16.unsqueeze(1).to_broadcast([128, 128, 16]),
                                                              in1=cf[:].unsqueeze(2).to_broadcast([128, 128, 16]), op=ALU.is_equal), r=["cs", "af", "bf"], w=["oh"])
                    A("dve", lambda e, inf=inf: e.tensor_tensor(out=oh[:].rearrange("p (h k a) -> p h k a", h=8, k=16),
                                                                in0=oh[:].rearrange("p (h k a) -> p h k a", h=8, k=16),
                                                                in1=inf[:].rearrange("p (h a) -> p h a", h=8).unsqueeze(2).to_broadcast([128, 8, 16, 16]), op=ALU.mult),
                      r=["oh", "i1f", "i2f"], w=["oh"])
                    A("dve", lambda e, which=which: e.tensor_reduce(out=isel[:, which * 128:(which + 1) * 128], in_=oh[:].rearrange("p (s a) -> p s a", a=16), axis=AX.X, op=ALU.add),
                      r=["oh"], w=["isel"])
                for which, (src, srcres, dst) in enumerate(((wg[:], "wg", WT), (isel[:, 0:128], "isel", I1T), (isel[:, 128:256], "isel", I2T))):
                    A("pe", lambda e, src=src, which=which: e.transpose(out=bk(7)[:, which * 128:(which + 1) * 128], in_=src, identity=ident), r=[srcres, "cs"], w=["b7"])
                for which, dst in enumerate((WT, I1T, I2T)):
                    A("act", lambda e, which=which, dst=dst, n=n: e.copy(out=dst[:, n * 128:(n + 1) * 128], in_=bk(7)[:, which * 128:(which + 1) * 128]), r=["b7"], w=["rt%d" % which])
                if dbg and n == 0:
                    A("sp", lambda e: e.dma_start(out=dbg_out["d_qp"], in_=qpT[:]), r=["qpT0", "qpT1", "qpT2", "qpT3"], w=["d_qp"], dma="dbg4")
                    offs = 0
                    for nm, src_, res_ in (("vv", vv, "vv"), ("i1f", i1f, "i1f"), ("i2f", i2f, "i2f"), ("af", af, "af"), ("bf", bf, "bf"), ("isel", isel, "isel"), ("wg", wg, "wg"), ("tsv", tsv, "tsv")):
                        w_ = src_.shape[1]
                        A("sp", lambda e, src_=src_, offs=offs, w_=w_: e.dma_start(out=dbg_out["d_misc"][:, offs:offs + w_], in_=src_[:]), r=[res_], w=["d_misc"], dma="dbg2")
                        offs += w_
                    A("sp", lambda e: e.dma_start(out=dbg_out["d_misc"][:, 1280:1536], in_=ssb[:, 0:256]), r=["ssb0_0"], w=["d_misc"], dma="dbg2")
                cap[0] = None

            def run_merged(ops1, ops2):
                for t in ops2:
                    t()
                for t in ops1:
                    t()

            load_h2(0)
            load_h2(1)
            route_front(0)
            if NT > 1:
                route_front(1)
            L0 = [[], []]
            back_body(0, L0)
            run_merged(L0[0], [])
            pend2 = L0[1]
            for n in range(NT):
                if n + 2 < NT:
                    route_front(n + 2)
                if prep_tile is not None:
                    for pi in range(8):
                        prep_tile(n * 8 + pi)
                if n + 1 < NT:
                    Ln = [[], []]
                    back_body(n + 1, Ln)
                    run_merged(Ln[0], pend2)
                    pend2 = Ln[1]
                else:
                    run_merged([], pend2)
            if dbg:
                rtf = cand
                for which, dst in enumerate((WT, I1T, I2T)):
                    A("dve", lambda e, dst=dst: e.tensor_copy(out=rtf[:], in_=dst[:]), r=["rt%d" % which], w=["rtf", "cand"])
                    A("sp", lambda e, which=which: e.dma_start(out=dbg_out["d_rt"][which], in_=rtf[:]), r=["rtf"], w=["d_rt"], dma="dbg")

        if "dense" in phases:
            phase_start()
            Gt = arena[:, 0:NE * TT].rearrange("p (t i) -> p t i", i=NE)
            NB = 3
            utg = [ta("utg%d" % i, GRP * 1024, BF16) for i in range(NB)]
            vg = [ta("vg%d" % i, GRP * 1024, BF16) for i in range(NB)]
            h2tt = ta("h2tt", 8 * TT, BF16)
            OB = 16
            NOH = 4
            ohA = [ta("ohA%d" % i, OB * 128, BF16) for i in range(NOH)]
            ohB = [ta("ohB%d" % i, OB * 128, BF16) for i in range(NOH)]
            gcount = [0]
            iorep = ta("iorep", OB * 128, BF16)
            A("dve", lambda e: e.tensor_copy(out=iorep[:].rearrange("p (i t) -> p i t", t=OB), in_=iotab[:].unsqueeze(2).to_broadcast([128, 128, OB])), r=["iotab"], w=["iorep"])
            gl = [ta("gl%d" % i, TT, BF16) for i in range(2)]
            ga = [ta("ga%d" % i, TT, BF16) for i in range(2)]
            x1l = ta("x1l", 1024)
            x2 = ta("x2", 1024)
            ot = x2
            junk2 = ta("junk2", 1024, BF16)
            ss2 = ta("ss2", 4)
            NG = NE // GRP

            def load_w(T, g, cnt):
                q = cnt % NB
                A("sp", lambda e: e.dma_start(out=utg[q][:].rearrange("p (q n) -> p q n", q=GRP), in_=UTs[g * GRP:(g + 1) * GRP].rearrange("q p n -> p q n")),
                  r=["UTs"], w=["utg%d" % q], dma="utg%d" % q)
                A("sp", lambda e: e.dma_start(out=vg[q][:].rearrange("p (q n) -> p q n", q=GRP), in_=Vbs[g * GRP:(g + 1) * GRP].rearrange("q p n -> p q n")),
                  r=["Vbs"], w=["vg%d" % q], dma="vg%d" % q)

            allg = [(T, g) for T in range(NTOK // TT) for g in range(NG)]
            cnt_load = 0
            for _ in range(NB - 1):
                load_w(allg[cnt_load][0], allg[cnt_load][1], cnt_load)
                cnt_load += 1
            cnt_use = 0
            first_g = True
            for T in range(NTOK // TT):
                t0 = T * TT
                for sub in range(TT // 128):
                    A("sp", lambda e, sub=sub: e.dma_start(out=h2tt[:].rearrange("p (c t) -> p c t", c=8)[:, :, sub * 128:(sub + 1) * 128],
                                                           in_=h2s[T * (TT // 128) + sub].rearrange("p (c t) -> p c t", c=8)), r=["h2s"], w=["h2tt"], dma="h2tt")
                def g_dve(Tn, ob, parts=(0, 1, 2)):
                    tk = Tn * TT + ob * OB
                    k = ob % NOH
                    oa = ohA[k][:].rearrange("p (i t) -> p i t", t=OB)
                    obv = ohB[k][:].rearrange("p (i t) -> p i t", t=OB)
                    if 0 in parts:
                        A("dve", lambda e: e.tensor_tensor(out=oa, in0=iorep[:].rearrange("p (i t) -> p i t", t=OB),
                                                           in1=I1T[:, tk:tk + OB].unsqueeze(1).to_broadcast([128, 128, OB]), op=ALU.is_equal),
                          r=["iorep", "rt1"], w=["ohA%d" % k])
                    if 1 in parts:
                        A("dve", lambda e: e.tensor_tensor(out=oa, in0=oa, in1=WT[:, tk:tk + OB].unsqueeze(1).to_broadcast([128, 128, OB]), op=ALU.mult),
                          r=["ohA%d" % k, "rt0"], w=["ohA%d" % k])
                    if 2 in parts:
                        A("dve", lambda e: e.tensor_tensor(out=obv, in0=iorep[:].rearrange("p (i t) -> p i t", t=OB),
                                                           in1=I2T[:, tk:tk + OB].unsqueeze(1).to_broadcast([128, 128, OB]), op=ALU.is_equal),
                          r=["iorep", "rt2"], w=["ohB%d" % k])

                def g_pe(ob):
                    k = ob % NOH
                    oa = ohA[k][:].rearrange("p (i t) -> p i t", t=OB)
                    obv = ohB[k][:].rearrange("p (i t) -> p i t", t=OB)
                    for t4 in range(OB // 4):
                        gb = 4 + (gcount[0] % 4)
                        gcount[0] += 1
                        for tt in range(4):
                            tl = t4 * 4 + tt
                            A("pe", lambda e: e.matmul(bk(gb)[:, tt * 128:(tt + 1) * 128], lhsT=obv[:, :, tl], rhs=oa[:, :, tl], start=True, stop=True),
                              r=["ohA%d" % k, "ohB%d" % k], w=["b%d" % gb])
                        tl0 = ob * OB + t4 * 4
                        dstv = Gt[:, tl0:tl0 + 4, :]
                        A("act", lambda e: e.copy(out=dstv, in_=bk(gb).rearrange("p (t i) -> p t i", t=4)), r=["b%d" % gb], w=["G"])

                NOB = TT // OB
                if T == 0:
                    for ob in range(NOH):
                        g_dve(0, ob)
                for ob in range(NOB):
                    g_pe(ob)
                    if ob + NOH < NOB:
                        g_dve(T, ob + NOH)
                early = {}
                for jj in range(NOH * 3):
                    early[NE - 2 - 2 * (NOH * 3 - 1 - jj)] = (jj // 3, jj % 3)
                h2v = h2tt[:].rearrange("p (c t) -> p c t", c=8)

                def scores(i1, q, ql):
                    sbk = 4 + (i1 % 2)
                    uv = utg[q][:].rearrange("p (q c e) -> p q c e", q=GRP, c=8)
                    for c in range(8):
                        A("pe", lambda e, c=c: e.matmul(bk(sbk)[:, 0:TT], lhsT=uv[:, ql, c, :], rhs=h2v[:, c, :], start=(c == 0), stop=(c == 7)),
                          r=["utg%d" % q, "h2tt"], w=["b%d" % sbk])
                    A("act", lambda e: e.activation(out=gl[i1 % 2][:], in_=bk(sbk)[:, 0:TT], func=AF.Gelu), r=["b%d" % sbk], w=["gl%d" % (i1 % 2)])
                    A("dve", lambda e: e.tensor_tensor(out=ga[i1 % 2][:], in0=gl[i1 % 2][:], in1=Gt[:, :, i1], op=ALU.mult), r=["gl%d" % (i1 % 2), "G"], w=["ga%d" % (i1 % 2)])

                def vmm(i1, q, ql):
                    vv_ = vg[q][:].rearrange("p (q n) -> p q n", q=GRP)
                    for th in range(TT // 128):
                        for dh in range(2):
                            A("pe", lambda e, th=th, dh=dh: e.matmul(bk(th * 2 + dh), lhsT=ga[i1 % 2][:, th * 128:(th + 1) * 128], rhs=vv_[:, ql, dh * 512:(dh + 1) * 512],
                                                                     start=(i1 == 0), stop=(i1 == NE - 1)),
                              r=["ga%d" % (i1 % 2), "vg%d" % q], w=["b%d" % (th * 2 + dh)])

                for g in range(NG):
                    q = cnt_use % NB
                    for ql in range(GRP):
                        i1 = g * GRP + ql
                        scores(i1, q, ql)
                        if i1 > 0:
                            pq = q if ql > 0 else (cnt_use - 1) % NB
                            vmm(i1 - 1, pq, (ql - 1) % GRP)
                        if ql == 0 and cnt_load < len(allg):
                            load_w(allg[cnt_load][0], allg[cnt_load][1], cnt_load)
                            cnt_load += 1
                        if T + 1 < NTOK // TT and i1 in early:
                            g_dve(T + 1, early[i1][0], parts=(early[i1][1],))
                    cnt_use += 1
                vmm(NE - 1, (cnt_use - 1) % NB, GRP - 1)
                for th in range(TT // 128):
                    r0 = t0 + th * 128
                    A("sp", lambda e, r0=r0: e.dma_start(out=x1l[:], in_=x1s[r0:r0 + 128, :]), r=["x1s"], w=["x1l"], dma="x1l")
                    for dh in range(2):
                        A("dve", lambda e, th=th, dh=dh: e.tensor_tensor(out=x2[:, dh * 512:(dh + 1) * 512], in0=bk(th * 2 + dh), in1=gate2[:, dh * 512:(dh + 1) * 512], op=ALU.mult),
                          r=["b%d" % (th * 2 + dh), "gate2"], w=["x2"])
                    A("pool", lambda e: e.tensor_tensor(out=x2[:], in0=x2[:], in1=x1l[:], op=ALU.add), r=["x2", "x1l"], w=["x2"])
                    A("act", lambda e: e.activation(out=junk2[:], in_=x2[:], func=AF.Square, accum_out=ss2[:, 0:1]), r=["x2"], w=["junk2", "ss2"])
                    A("act", lambda e: e.activation(out=ss2[:, 1:2], in_=ss2[:, 0:1], func=AF.Ln, scale=1.0 / D, bias=epsc[:, 0:1]), r=["ss2", "epsc"], w=["ss21"])
                    A("act", lambda e: e.activation(out=ss2[:, 2:3], in_=ss2[:, 1:2], func=AF.Exp, scale=-0.5), r=["ss21"], w=["ss22"])
                    A("dve", lambda e: e.scalar_tensor_tensor(out=ot[:], in0=x2[:], scalar=ss2[:, 2:3], in1=fgr[:], op0=ALU.mult, op1=ALU.mult), r=["x2", "ss22", "fgr"], w=["x2"])
                    A("sp", lambda e, r0=r0: e.dma_start(out=out[r0:r0 + 128, :], in_=ot[:]), r=["x2"], w=["out"], dma="sout")
        fin = ["out"] + list(dbg_out.keys())
        A("sp", None, r=fin)
        P.emit(st)
    return nc


_NC_CACHE = {}


def make_in_maps(inputs):
    f = lambda a: np.ascontiguousarray(np.asarray(a, dtype=np.float32))
    x = f(inputs["x"])
    c = f(inputs["c"])
    cst = make_consts()
    colT = lambda v, k: np.ascontiguousarray(v.reshape(k, 128).T)
    shared = {
        "cst": cst,
        "ada_w": f(inputs["ada_w"])[0],
        "ada_bT": colT(f(inputs["ada_b"])[0], 48),
        "ada_b": f(inputs["ada_b"])[0].reshape(1, 6 * D),
        "n1g": colT(f(inputs["norm1_g"])[0], 8),
        "n2g": colT(f(inputs["norm2_g"])[0], 8),
        "fg_rep": np.ascontiguousarray(np.broadcast_to(f(inputs["final_g"])[None, :], (128, D))),
        "w_in": f(inputs["w_in"])[0],
        "gw17": np.ascontiguousarray(np.concatenate([f(inputs["gla_gate_w"])[0], f(inputs["gla_gate_b"])[0][None, :]], axis=0)),
        "gng_rep": np.ascontiguousarray(np.broadcast_to(f(inputs["gla_norm_g"])[0][None, :], (128, 128))),
        "pool_w": f(inputs["pool_w"])[0],
        "pscT": colT(f(inputs["pool_scale"])[0], 4),
        "w_out": f(inputs["w_out"])[0],
        "wq": f(inputs["peer_wq"])[0],
        "k1": f(inputs["peer_k1"])[0],
        "k2": f(inputs["peer_k2"])[0],
        "pu": f(inputs["peer_u"])[0],
        "pv": f(inputs["peer_v"])[0],
    }
    maps = []
    for core in range(8):
        b, j = core // 4, core % 4
        m = dict(shared)
        m["x_own"] = np.ascontiguousarray(x[b, j * NTOK:(j + 1) * NTOK])
        xp = np.zeros((NPRE * 128, D), np.float32)
        npre = j * NTOK
        if npre > 0:
            xp[NPRE * 128 - npre:] = x[b, 0:npre]
        m["x_pre"] = xp
        m["cst2"] = make_core_consts(j)
        m["c_col"] = colT(c[b], 8)
        maps.append(m)
    return maps


def kernel(**inputs):
    if "nc" not in _NC_CACHE:
        _NC_CACHE["nc"] = build_nc()
    nc = _NC_CACHE["nc"]
    maps = make_in_maps(inputs)
    res = run_bass_kernel_spmd(nc, maps, core_ids=list(range(8)))
    out = np.zeros((2, 8192, D), np.float32)
    for core in range(8):
        b, j = core // 4, core % 4
        out[b, j * NTOK:(j + 1) * NTOK] = res.results[core]["out"]
    return out
```

```python
import numpy as np
from contextlib import ExitStack
import concourse.bass as bass
import concourse.mybir as mybir
from concourse.bass_utils import run_bass_kernel_spmd

F32 = mybir.dt.float32
BF16 = mybir.dt.bfloat16
U32 = mybir.dt.uint32
I32 = mybir.dt.int32
AF = mybir.ActivationFunctionType
ALU = mybir.AluOpType
AX = mybir.AxisListType

D = 1024
NTOK = 2048
NT = NTOK // 128
NPRE = 48
INC = 2064
EPS = 1e-6
NE = 128
TT = 256
GRP = 2


class Inst:
    __slots__ = ("eng", "fn", "deps", "is_dma", "semkey", "ordinal", "sig", "sem", "idx")


class _Rec:
    def __init__(self):
        self.call = None

    def __getattr__(self, name):
        def f(*a, **k):
            self.call = (name, a, k)
            return None
        return f


class Prog:
    ENG = ("pe", "act", "dve", "pool", "sp")
    LIMIT = 20000

    def __init__(self, nc):
        self.nc = nc
        self.lists = {e: [] for e in self.ENG}
        self.last_w = {}
        self.readers = {}
        self.dma_counts = {}
        self.last_dma = {}

    def barrier(self):
        deps = set()
        for e in self.ENG:
            for ins in reversed(self.lists[e]):
                if not ins.is_dma and ins.fn is not None:
                    deps.add(ins)
                    break
        for last in self.last_dma.values():
            deps.add(last)
        for e in self.ENG:
            ins = Inst()
            ins.eng = e
            ins.fn = None
            ins.is_dma = False
            ins.semkey = None
            ins.sem = None
            ins.ordinal = 0
            ins.sig = False
            ins.deps = set(deps)
            self.lists[e].append(ins)
            ins.idx = len(self.lists[e]) - 1
        self.last_w.clear()
        self.readers.clear()

    def defer(self, eng, fn, r=(), w=(), dma=None):
        if fn is not None:
            rec = _Rec()
            fn(rec)
            name_, a_, k_ = rec.call
            fn = (lambda e, name_=name_, a_=a_, k_=k_: getattr(e, name_)(*a_, **k_))
        r = list(r)
        w = list(w)
        return lambda: self.add(eng, fn, r, w, dma, _bound=True)

    def add(self, eng, fn, r=(), w=(), dma=None, _bound=False):
        ins = Inst()
        ins.eng = eng
        if fn is not None and not _bound:
            rec = _Rec()
            fn(rec)
            name_, a_, k_ = rec.call
            fn = (lambda e, name_=name_, a_=a_, k_=k_: getattr(e, name_)(*a_, **k_))
        ins.fn = fn
        ins.is_dma = dma is not None
        ins.semkey = dma
        ins.sem = None
        ins.ordinal = 0
        deps = set()
        for k in r:
            lw = self.last_w.get(k)
            if lw is not None:
                deps.add(lw)
        for k in w:
            lw = self.last_w.get(k)
            if lw is not None:
                deps.add(lw)
            for rd in self.readers.get(k, ()):
                deps.add(rd)
        ins.deps = deps
        for k in r:
            self.readers.setdefault(k, []).append(ins)
        for k in w:
            self.last_w[k] = ins
            self.readers[k] = []
        if dma is not None:
            c = self.dma_counts.get(dma, 0) + 1
            self.dma_counts[dma] = c
            ins.ordinal = c
            self.last_dma[dma] = ins
        ins.sig = False
        self.lists[eng].append(ins)
        ins.idx = len(self.lists[eng]) - 1
        return ins

    def emit(self, stack):
        nc = self.nc
        for e in self.ENG:
            for ins in self.lists[e]:
                for d in ins.deps:
                    if d.is_dma:
                        continue
                    if d.eng == ins.eng == "pe":
                        continue
                    d.sig = True
        semkeys = []
        for e in self.ENG:
            cnt = 0
            epoch = 0
            for ins in self.lists[e]:
                if ins.is_dma or not ins.sig:
                    continue
                cnt += 1
                if cnt > self.LIMIT:
                    epoch += 1
                    cnt = 1
                ins.sem = (e, epoch)
                ins.ordinal = cnt
                if ins.sem not in semkeys:
                    semkeys.append(ins.sem)
        for k in self.dma_counts:
            semkeys.append(("dma", k))
        self.sems = {}
        for i, k in enumerate(semkeys):
            self.sems[k] = stack.enter_context(nc.semaphore("s%d" % i))
        block = stack.enter_context(nc.Block())

        def run(engname, eng):
            waited = {}
            for ins in self.lists[engname]:
                need = {}
                for d in ins.deps:
                    if d.is_dma:
                        key = ("dma", d.semkey)
                        val = 16 * d.ordinal
                    else:
                        if d.eng == engname == "pe":
                            continue
                        key = d.sem
                        val = d.ordinal
                    if need.get(key, 0) < val:
                        need[key] = val
                for key, val in need.items():
                    if waited.get(key, 0) >= val:
                        continue
                    eng.wait_ge(self.sems[key], val)
                    waited[key] = val
                if ins.fn is None:
                    continue
                bi = ins.fn(eng)
                if ins.is_dma:
                    bi.then_inc(self.sems[("dma", ins.semkey)], 16)
                elif ins.sig:
                    bi.then_inc(self.sems[ins.sem], 1)

        @block.tensor
        def _(e):
            run("pe", e)

        @block.scalar
        def _(e):
            run("act", e)

        @block.vector
        def _(e):
            run("dve", e)

        @block.gpsimd
        def _(e):
            run("pool", e)

        @block.sync
        def _(e):
            run("sp", e)


C_ID, C_IOTA, C_TRIL, C_TRIU, C_CAUS, C_ONES, C_NEG, C_IOTA16, C_BA, C_BB = (
    0, 128, 256, 384, 512, 640, 768, 769, 785, 785 + 512)
C_N = 785 + 1024


def make_consts():
    c = np.zeros((128, C_N), np.float32)
    s = np.arange(128)[:, None]
    t = np.arange(128)[None, :]
    c[:, C_ID:C_ID + 128] = (s == t)
    c[:, C_IOTA:C_IOTA + 128] = t
    c[:, C_TRIL:C_TRIL + 128] = np.where(s <= t, -1.0 / 16, 0.0)
    c[:, C_TRIU:C_TRIU + 128] = np.where(s > t, -1.0 / 16, 0.0)
    c[:, C_CAUS:C_CAUS + 128] = (s <= t)
    c[:, C_ONES:C_ONES + 128] = 1.0
    c[:, C_NEG] = -1.0 / 16
    c[:, C_IOTA16:C_IOTA16 + 16] = np.arange(16)[None, :]
    for g, w in enumerate((2, 4, 8, 16)):
        d = t - s
        c[:, C_BA + g * 128:C_BA + (g + 1) * 128] = np.where((d >= 0) & (d < w), 1.0 / w, 0.0) - (s == t)
        d2 = t + 128 - s
        c[:, C_BB + g * 128:C_BB + (g + 1) * 128] = np.where((d2 >= 0) & (d2 < w), 1.0 / w, 0.0)
    return c


def make_core_consts(j):
    c = np.zeros((128, 512 + NPRE), np.float32)
    s = np.arange(128)[:, None]
    t = np.arange(128)[None, :]
    for g, w in enumerate((2, 4, 8, 16)):
        d = t - s
        if j == 0:
            cnt = np.minimum(t + 1, w).astype(np.float32)
        else:
            cnt = np.full_like(t, w).astype(np.float32)
        c[:, g * 128:(g + 1) * 128] = np.where((d >= 0) & (d < w), 1.0 / cnt, 0.0) - (s == t)
    pos = NTOK * j - NPRE * 128 + np.arange(NPRE * 128)
    c[:, 512:] = (pos >= 0).astype(np.float32).reshape(NPRE, 128).T
    return c


def build_nc(phases=("mod", "prep", "sub1", "route", "dense"), dbg=False):
    nc = bass.Bass("TRN2", target_bir_lowering=False)
    din = lambda n, s, d=F32: nc.dram_tensor(n, s, d, kind="ExternalInput").ap()
    x_own = din("x_own", [NTOK, D])
    x_pre = din("x_pre", [NPRE * 128, D])
    cst = din("cst", [128, C_N])
    cst2 = din("cst2", [128, 512 + NPRE])
    c_col = din("c_col", [128, 8])
    ada_w = din("ada_w", [D, 6 * D])
    ada_bT = din("ada_bT", [128, 48])
    ada_b = din("ada_b", [1, 6 * D])
    n1g = din("n1g", [128, 8])
    n2g = din("n2g", [128, 8])
    fg_rep = din("fg_rep", [128, D])
    w_in = din("w_in", [D, INC])
    gw17 = din("gw17", [17, 256])
    gng_rep = din("gng_rep", [128, 128])
    pool_w = din("pool_w", [4, 128, 128])
    pscT = din("pscT", [128, 4])
    w_out = din("w_out", [D, D])
    wq = din("wq", [D, 2048])
    k1 = din("k1", [128, 128])
    k2 = din("k2", [128, 128])
    pu = din("pu", [NE * 128, D])
    pv = din("pv", [NE * 128, D])
    out = nc.dram_tensor("out", [NTOK, D], F32, kind="ExternalOutput").ap()
    skind = "ExternalOutput" if dbg else "Internal"
    UTs = nc.dram_tensor("UTs", [NE, 128, 1024], BF16, kind=skind).ap()
    Vbs = nc.dram_tensor("Vbs", [NE, 128, 1024], BF16, kind=skind).ap()
    x1s = nc.dram_tensor("x1s", [NTOK, D], F32).ap()
    h2s = nc.dram_tensor("h2s", [NT, 128, 1024], BF16).ap()
    dbg_out = {}
    if dbg:
        for n, s in (("d_mod", [128, 32]), ("d_x1", [NTOK, D]), ("d_h2", [NT, 128, 1024]),
                     ("d_rt", [3, 128, NTOK]), ("d_g1", [128, D]), ("d_misc", [128, 2048]), ("d_qp", [128, 2048])):
            dbg_out[n] = nc.dram_tensor(n, s, BF16 if n in ("d_h2", "d_qp") else F32, kind="ExternalOutput").ap()

    with ExitStack() as st:
        def sb(n, cols, d=F32, parts=128):
            return st.enter_context(nc.sbuf_tensor(n, [parts, cols], d))
        P = Prog(nc)
        cap = [None]

        def A(eng, fn, r=(), w=(), dma=None):
            if cap[0] is not None:
                cap[0].append(P.defer(eng, fn, r, w, dma))
                return None
            return P.add(eng, fn, r, w, dma)
        banks = [st.enter_context(nc.psum_tensor("ps%d" % i, [128, 512], F32)) for i in range(8)]

        def bk(i):
            return banks[i][:]

        def bkb(i):
            return banks[i][:].bitcast(BF16)

        cs = sb("cs", C_N)
        cs2 = sb("cs2", 512 + NPRE)
        identb = sb("identb", 128, BF16)
        iotab = sb("iotab", 128, BF16)
        modT = sb("modT", 32)
        gs1 = sb("gs1", 8)
        gs2 = sb("gs2", 8)
        gate1 = sb("gate1", D)
        gate2 = sb("gate2", D)
        fgr = sb("fgr", D)
        gngr = sb("gngr", 128)
        pscs = sb("pscs", 4)
        n1s = sb("n1s", 8)
        n2s = sb("n2s", 8)
        epsc = sb("epsc", 1)
        print("sbuf bytes remaining after persistent:", nc.sbuf_bytes_remaining)
        A("sp", lambda e: e.dma_start(out=cs[:], in_=cst), w=["cs"], dma="c0_1")
        A("sp", lambda e: e.dma_start(out=cs2[:], in_=cst2), w=["cs2"], dma="c0_2")
        A("sp", lambda e: e.dma_start(out=fgr[:], in_=fg_rep), w=["fgr"], dma="c0_3")
        A("sp", lambda e: e.dma_start(out=gngr[:], in_=gng_rep), w=["gngr"], dma="c0_4")
        A("sp", lambda e: e.dma_start(out=pscs[:], in_=pscT), w=["pscs"], dma="c0_5")
        A("sp", lambda e: e.dma_start(out=n1s[:], in_=n1g), w=["n1s"], dma="c0_6")
        A("sp", lambda e: e.dma_start(out=n2s[:], in_=n2g), w=["n2s"], dma="c0_7")
        ident = cs[:, C_ID:C_ID + 128]
        A("dve", lambda e: e.memset(epsc[:], EPS), w=["epsc"])
        A("dve", lambda e: e.tensor_copy(out=identb[:], in_=ident), r=["cs"], w=["identb"])
        A("dve", lambda e: e.tensor_copy(out=iotab[:], in_=cs[:, C_IOTA:C_IOTA + 128]), r=["cs"], w=["iotab"])

        ARENA = NE * TT
        arena = sb("arena", ARENA, BF16)
        TMPF = 26624
        tmp = st.enter_context(nc.sbuf_tensor("tmp", [128, TMPF], F32))
        tmp_views = {F32: tmp, BF16: tmp[:].bitcast(BF16), U32: tmp[:].bitcast(U32), I32: tmp[:].bitcast(I32)}
        ta_off = [0]

        def ta(n, cols, d=F32, parts=128):
            words = (cols + 1) // 2 if d == BF16 else cols
            o = ta_off[0]
            ta_off[0] += words
            assert ta_off[0] <= TMPF, (n, ta_off[0])
            if d == BF16:
                return tmp_views[BF16][0:parts, 2 * o:2 * o + cols]
            return tmp_views[d][0:parts, o:o + cols]

        def phase_start():
            P.barrier()
            ta_off[0] = 0

        def make_prep(pbank=None, deng="pool"):
            ust = [ta("ust%d" % i, 1024) for i in range(2)]
            vst = [ta("vst%d" % i, 1024) for i in range(2)]
            ub = [ta("ub%d" % i, 1024, BF16) for i in range(2)]
            utb = [ta("utb%d" % i, 1024, BF16) for i in range(2)]
            vb = [ta("vb%d" % i, 1024, BF16) for i in range(2)]

            def prep_load(i):
                q = i % 2
                A(deng, lambda e: e.dma_start(out=ust[q][:], in_=pu[i * 128:(i + 1) * 128, :]), w=["ust%d" % q], dma="ust%d" % q)
                A(deng, lambda e: e.dma_start(out=vst[q][:], in_=pv[i * 128:(i + 1) * 128, :]), w=["vst%d" % q], dma="vst%d" % q)

            def prep_tile(i):
                q = i % 2
                if i == 0:
                    prep_load(0)
                if i + 1 < NE:
                    prep_load(i + 1)
                A("act", lambda e: e.copy(out=ub[q][:], in_=ust[q][:]), r=["ust%d" % q], w=["ub%d" % q])
                bn_ = (4 + q) if pbank is None else pbank
                pb = "b%d" % bn_
                for c in range(8):
                    A("pe", lambda e: e.transpose(out=bkb(bn_)[:, c * 128:(c + 1) * 128], in_=ub[q][:, c * 128:(c + 1) * 128], identity=identb[:]),
                      r=["ub%d" % q, "identb"], w=[pb])
                A("act", lambda e: e.copy(out=utb[q][:], in_=bkb(bn_)), r=[pb], w=["utb%d" % q])
                A("act", lambda e: e.copy(out=vb[q][:], in_=vst[q][:]), r=["vst%d" % q], w=["vb%d" % q])
                A(deng, lambda e: e.dma_start(out=UTs[i], in_=utb[q][:]), r=["utb%d" % q], w=["UTs"], dma="sut%d" % q)
                A(deng, lambda e: e.dma_start(out=Vbs[i], in_=vb[q][:]), r=["vb%d" % q], w=["Vbs"], dma="svb%d" % q)
            return prep_tile

        if phases == ("prep",):
            phase_start()
            prep_tile = make_prep()
            for i in range(NE):
                prep_tile(i)

        winb = arena[:, 0:8 * INC].rearrange("p (c n) -> p c n", c=8)
        woutb = arena[:, 8 * INC:8 * INC + 8192].rearrange("p (c n) -> p c n", c=8)
        if "mod" in phases:
            phase_start()
            stage = ta("stage", 4096)
            ccol = ta("ccol", 8)
            sc = ta("sc", 8)
            screp = ta("screp", 1024)
            abT = ta("abT", 48)
            abrow = ta("abrow", 2048, parts=1)
            A("sp", lambda e: e.dma_start(out=ccol[:], in_=c_col), w=["ccol"], dma="c1_8")
            A("sp", lambda e: e.dma_start(out=abT[:], in_=ada_bT), w=["abT"], dma="c1_9")
            A("sp", lambda e: e.dma_start(out=abrow[:, 0:1024], in_=ada_b[:, 2048:3072]), w=["abrow"], dma="c1_10")
            A("sp", lambda e: e.dma_start(out=abrow[:, 1024:2048], in_=ada_b[:, 5120:6144]), w=["abrow"], dma="c1_11")
            A("act", lambda e: e.activation(out=sc[:], in_=ccol[:], func=AF.Silu), r=["ccol"], w=["sc"])
            A("dve", lambda e: e.tensor_copy(out=screp[:].rearrange("p (c m) -> p c m", c=8),
                                             in_=sc[:].unsqueeze(2).to_broadcast([128, 8, 128])), r=["sc"], w=["screp"])
            stageB = ta("stageB", 4096)
            wsts = [stage[:].rearrange("p (c n) -> p c n", c=8), stageB[:].rearrange("p (c n) -> p c n", c=8)]
            adv = ada_w.rearrange("(c p) n -> p c n", p=128)
            ones_row = cs[0:1, C_ONES:C_ONES + 128]
            wstg = [ta("wstg%d" % i, INC) for i in range(2)]
            wiv = w_in.rearrange("(c p) n -> p c n", p=128)
            wov = w_out.rearrange("(c p) n -> p c n", p=128)
            wchunks = [("in", c) for c in range(8)] + [("out", c) for c in range(8)]
            wci = [0]

            def stage_weight():
                if wci[0] >= len(wchunks):
                    return
                kind_, c_ = wchunks[wci[0]]
                k_ = wci[0] % 2
                wci[0] += 1
                if kind_ == "in":
                    A("sp", lambda e: e.dma_start(out=wstg[k_][:, 0:INC], in_=wiv[:, c_, :]), w=["wstg%d" % k_], dma="wstg%d" % k_)
                    A("dve", lambda e: e.tensor_copy(out=winb[:, c_, :], in_=wstg[k_][:, 0:INC]), r=["wstg%d" % k_], w=["winb"])
                else:
                    A("sp", lambda e: e.dma_start(out=wstg[k_][:, 0:1024], in_=wov[:, c_, :]), w=["wstg%d" % k_], dma="wstg%d" % k_)
                    A("dve", lambda e: e.tensor_copy(out=woutb[:, c_, :], in_=wstg[k_][:, 0:1024]), r=["wstg%d" % k_], w=["woutb"])

            def load_blk(blk):
                A("sp", lambda e: e.dma_start(out=wsts[blk % 2], in_=adv[:, :, blk * 512:(blk + 1) * 512]), w=["stage%d" % (blk % 2)], dma="stage%d" % (blk % 2))
            load_blk(0)
            for blk in range(12):
                if blk + 1 < 12:
                    load_blk(blk + 1)
                wst = wsts[blk % 2]
                sres = "stage%d" % (blk % 2)
                stage_weight()
                if blk % 3 != 2:
                    stage_weight()
                kind = blk // 2
                if kind in (2, 5):
                    gt = gate1 if kind == 2 else gate2
                    half = blk % 2
                    for c in range(8):
                        A("pe", lambda e, c=c: e.matmul(bk(2), lhsT=screp[:, c * 128:(c + 1) * 128], rhs=wst[:, c, :], start=(c == 0), stop=False),
                          r=["screp", sres], w=["b2"])
                    A("pe", lambda e, blk=blk: e.matmul(bk(2), lhsT=ones_row, rhs=abrow[0:1, ((blk - 4) if blk < 6 else (blk - 8)) * 512:((blk - 4) if blk < 6 else (blk - 8)) * 512 + 512], start=False, stop=True),
                      r=["cs", "abrow"], w=["b2"])
                    A("dve", lambda e, gt=gt, half=half: e.tensor_copy(out=gt[:, half * 512:(half + 1) * 512], in_=bk(2)), r=["b2"], w=["gate%d" % (1 if kind == 2 else 2)])
                else:
                    qi = {0: 0, 1: 1, 3: 2, 4: 3}[kind]
                    for nchunk in range(4):
                        col = qi * 8 + (blk % 2) * 4 + nchunk
                        for c in range(8):
                            A("pe", lambda e, c=c, nchunk=nchunk, col=col: e.matmul(bk(3)[:, col:col + 1], lhsT=wst[:, c, nchunk * 128:(nchunk + 1) * 128],
                                                                                    rhs=sc[:, c:c + 1], start=(c == 0), stop=(c == 7)),
                              r=[sres, "sc"], w=["b3"])
            while wci[0] < len(wchunks):
                stage_weight()
            for qi, bcol in enumerate((0, 8, 24, 32)):
                A("dve", lambda e, qi=qi, bcol=bcol: e.tensor_tensor(out=modT[:, qi * 8:(qi + 1) * 8], in0=bk(3)[:, qi * 8:(qi + 1) * 8],
                                                                     in1=abT[:, bcol:bcol + 8], op=ALU.add), r=["b3", "abT"], w=["modT"])
            A("dve", lambda e: e.scalar_tensor_tensor(out=gs1[:], in0=modT[:, 8:16], scalar=1.0, in1=n1s[:], op0=ALU.add, op1=ALU.mult),
              r=["modT", "n1s"], w=["gs1"])
            A("dve", lambda e: e.scalar_tensor_tensor(out=gs2[:], in0=modT[:, 24:32], scalar=1.0, in1=n2s[:], op0=ALU.add, op1=ALU.mult),
              r=["modT", "n2s"], w=["gs2"])
            if dbg:
                A("sp", lambda e: e.dma_start(out=dbg_out["d_mod"], in_=modT[:]), r=["modT"], w=["d_mod"], dma="dbg")
                A("sp", lambda e: e.dma_start(out=dbg_out["d_g1"], in_=gate1[:]), r=["gate1"], w=["d_g1"], dma="dbg")
        sh1 = modT[:, 0:8]
        sh2 = modT[:, 16:24]

        if "sub1" in phases:
            phase_start()
            gw = ta("gw", 256, parts=17)
            A("sp", lambda e: e.dma_start(out=gw[:], in_=gw17), w=["gw"], dma="c2_12")
            pwst = ta("pwst", 512)
            pwb = ta("pwb", 512, BF16)
            A("sp", lambda e: e.dma_start(out=pwst[:].rearrange("p (g d) -> p g d", g=4), in_=pool_w.rearrange("g c d -> c g d")), w=["pwst"], dma="c2_13")
            A("dve", lambda e: e.tensor_copy(out=pwb[:], in_=pwst[:]), r=["pwst"], w=["pwb"])
            state = ta("state", 512, parts=64)
            stateb = ta("stateb", 512, BF16, parts=64)
            A("dve", lambda e: e.memset(state[:], 0.0), w=["state"])
            A("dve", lambda e: e.memset(stateb[:], 0.0), w=["stateb"])
            xt2 = [ta("xt2_%d" % i, 2048) for i in range(2)]
            xt = [xt2[i][:, 0:1024] for i in range(2)]
            junk = ta("junk", 1024, BF16)
            ssq = ta("ssq", 8)
            pcur = [ta("pcur%d" % i, 512) for i in range(2)]
            prep_tile = None
            ta_mark = ta_off[0]
            xnbp2 = [ta("xnbp%d" % i, 2048, BF16) for i in range(2)]
            hTp2 = [ta("hTp%d" % i, 2048, BF16) for i in range(2)]
            ssqp2 = [ta("ssqp%d" % i, 8) for i in range(2)]
            sptp = ta("sptp", 512)
            ectp = ta("ectp", 512)
            kstp = ta("kstp", 512, BF16)
            vbfp = ta("vbfp", 1024, BF16)
            decp = ta("decp", 8, parts=64)
            alrp = ta("alrp", 256, parts=17)
            ssqp = ta("ssqp", 8)
            A("dve", lambda e: e.memset(alrp[:], 1.0), w=["alrp"])

            def load_x2(sidx, q):
                A("sp", lambda e: e.dma_start(out=xt2[q][:].rearrange("p (u d) -> p u d", u=2),
                                              in_=x_pre[sidx * 256:(sidx + 1) * 256, :].rearrange("(u p) d -> p u d", p=128)), w=["xt%d" % q], dma="xt%d" % q)

            def prefix_A(sidx, q):
                xres = "xt%d" % q
                xv = xt2[q][:].rearrange("p (u d) -> p u d", u=2)
                xn_, hT_, sq_ = xnbp2[q], hTp2[q], ssqp2[q]
                for u in range(2):
                    A("act", lambda e: e.activation(out=junk[:], in_=xv[:, u, :], func=AF.Square, accum_out=sq_[:, u:u + 1]), r=[xres], w=["junk", "ssqp%d" % q])
                A("act", lambda e: e.activation(out=sq_[:, 2:4], in_=sq_[:, 0:2], func=AF.Ln, scale=1.0 / D, bias=epsc[:, 0:1]), r=["ssqp%d" % q, "epsc"], w=["ssqp1%d" % q])
                A("act", lambda e: e.activation(out=sq_[:, 4:6], in_=sq_[:, 2:4], func=AF.Exp, scale=-0.5), r=["ssqp1%d" % q], w=["ssqp2%d" % q])
                for u in range(2):
                    A("act", lambda e: e.activation(out=xn_[:, u * 1024:(u + 1) * 1024], in_=xv[:, u, :], func=AF.Copy, scale=sq_[:, 4 + u:5 + u]), r=[xres, "ssqp2%d" % q], w=["xnbp%d" % q])
                for u in range(2):
                    for c in range(8):
                        b = c // 4
                        po = ((c % 4) * 2 + u) * 128
                        A("pe", lambda e: e.transpose(out=bkb(b)[:, po:po + 128], in_=xn_[:, u * 1024 + c * 128:u * 1024 + (c + 1) * 128], identity=identb[:]),
                          r=["xnbp%d" % q, "identb"], w=["b%d" % b])
                for c in range(8):
                    b = c // 4
                    A("dve", lambda e: e.tensor_scalar(out=hT_[:, c * 256:(c + 1) * 256], in0=bkb(b)[:, (c % 4) * 256:(c % 4 + 1) * 256],
                                                       scalar1=gs1[:, c:c + 1], scalar2=sh1[:, c:c + 1], op0=ALU.mult, op1=ALU.add),
                      r=["b%d" % b, "gs1", "modT"], w=["hTp%d" % q])

            def prefix_B(sidx, q):
                hT_ = hTp2[q]
                hres = "hTp%d" % q
                last = sidx == NPRE // 2 - 1
                for u in range(2):
                    for c in range(8):
                        A("pe", lambda e: e.matmul(bk(2)[:, u * 256:(u + 1) * 256], lhsT=hT_[:, (c * 2 + u) * 128:(c * 2 + u + 1) * 128], rhs=winb[:, c, 256:512], start=(c == 0), stop=(c == 7)),
                          r=[hres, "winb"], w=["b2"])
                    for c in range(8):
                        A("pe", lambda e: e.matmul(bk(4 + u), lhsT=hT_[:, (c * 2 + u) * 128:(c * 2 + u + 1) * 128], rhs=winb[:, c, 512:1024], start=(c == 0), stop=(c == 7)),
                          r=[hres, "winb"], w=["b%d" % (4 + u)])
                for c in range(8):
                    A("pe", lambda e: e.matmul(bk(3)[0:16, 0:256], lhsT=winb[:, c, 2048:2064], rhs=hT_[:, c * 256:(c + 1) * 256], start=(c == 0), stop=(c == 7)),
                      r=[hres, "winb"], w=["b3"])
                A("act", lambda e: e.copy(out=alrp[0:16, :], in_=bk(3)[0:16, 0:256]), r=["b3"], w=["alrp"])
                for u in range(2):
                    A("pe", lambda e: e.matmul(bk(3)[:, u * 256:(u + 1) * 256], lhsT=alrp[:, u * 128:(u + 1) * 128], rhs=gw[:], start=True, stop=True), r=["alrp", "gw"], w=["b3"])
                A("act", lambda e: e.activation(out=ectp[:], in_=bk(3), func=AF.Exp, scale=-1.0), r=["b3"], w=["ectp"])
                A("act", lambda e: e.activation(out=sptp[:], in_=ectp[:], func=AF.Ln, bias=1.0), r=["ectp"], w=["sptp"])
                for u in range(2):
                    A("pe", lambda e: e.matmul(bk(6)[:, u * 256:(u + 1) * 256], lhsT=cs[:, C_TRIU:C_TRIU + 128], rhs=sptp[:, u * 256:(u + 1) * 256], start=True, stop=True), r=["cs", "sptp"], w=["b6"])
                    for h in range(4):
                        A("pe", lambda e: e.matmul(bk(3)[0:64, u * 4 + h:u * 4 + h + 1], lhsT=sptp[:, u * 256 + h * 64:u * 256 + (h + 1) * 64], rhs=cs[:, C_NEG:C_NEG + 1], start=True, stop=True),
                          r=["cs", "sptp"], w=["b3"])
                A("act", lambda e: e.activation(out=ectp[:], in_=bk(6), func=AF.Exp), r=["b6"], w=["ectp"])
                A("dve", lambda e: e.tensor_tensor(out=kstp[:], in0=bk(2), in1=ectp[:], op=ALU.mult), r=["b2", "ectp"], w=["kstp"])
                A("act", lambda e: e.activation(out=decp[:], in_=bk(3)[0:64, 0:8], func=AF.Exp), r=["b3"], w=["decp"])
                for u in range(2):
                    ti = 2 * sidx + u
                    A("act", lambda e: e.activation(out=vbfp[:, u * 512:(u + 1) * 512], in_=bk(4 + u), func=AF.Copy, scale=cs2[:, 512 + ti:513 + ti]), r=["b%d" % (4 + u), "cs2"], w=["vbfp"])
                if last:
                    for c in range(8):
                        A("pe", lambda e: e.matmul(bk(7), lhsT=hT_[:, (c * 2 + 1) * 128:(c * 2 + 2) * 128], rhs=winb[:, c, 1536:2048], start=(c == 0), stop=(c == 7)),
                          r=[hres, "winb"], w=["b7"])
                    A("act", lambda e: e.activation(out=pcur[1][:], in_=bk(7), func=AF.Copy, scale=cs2[:, 512 + NPRE - 1:512 + NPRE]), r=["b7", "cs2"], w=["pcur1"])
                for u in range(2):
                    for h in range(4):
                        A("pe", lambda e: e.matmul(bk(4 + u)[0:64, h * 128:(h + 1) * 128], lhsT=kstp[:, u * 256 + h * 64:u * 256 + (h + 1) * 64], rhs=vbfp[:, u * 512 + h * 128:u * 512 + (h + 1) * 128], start=True, stop=True),
                          r=["kstp", "vbfp"], w=["b%d" % (4 + u)])
                st3 = state[:].rearrange("p (h v) -> p h v", h=4)
                for u in range(2):
                    A("dve", lambda e: e.tensor_tensor(out=st3, in0=st3, in1=decp[:, u * 4:(u + 1) * 4].unsqueeze(2).to_broadcast([64, 4, 128]), op=ALU.mult), r=["state", "decp"], w=["state"])
                    A("dve", lambda e: e.tensor_tensor(out=state[:], in0=state[:], in1=bk(4 + u)[0:64, :], op=ALU.add), r=["state", "b%d" % (4 + u)], w=["state"])
                if last:
                    A("act", lambda e: e.copy(out=stateb[:], in_=state[:]), r=["state"], w=["stateb"])

            NS = NPRE // 2
            load_x2(0, 0)
            load_x2(1, 1)
            prefix_A(0, 0)
            for sidx in range(NS):
                if sidx + 1 < NS:
                    prefix_A(sidx + 1, (sidx + 1) % 2)
                if sidx + 2 < NS:
                    load_x2(sidx + 2, sidx % 2)
                elif sidx + 2 == NS:
                    A("sp", lambda e: e.dma_start(out=xt[0], in_=x_own[0:128, :]), w=["xt0"], dma="xt0")
                prefix_B(sidx, sidx % 2)
            P.barrier()
            ta_off[0] = ta_mark
            alr = ta("alr", 128, parts=17)
            A("dve", lambda e: e.memset(alr[:], 1.0), w=["alr"])
            xnb = ta("xnb", 1024, BF16)
            hT = ta("hT", 1024, BF16)
            spt = ta("spt", 256)
            ect = ta("ect", 256)
            kst = ta("kst", 256, BF16)
            vbf = ta("vbf", 512, BF16)
            dec = ta("dec", 4, parts=64)
            ebt = ta("ebt", 512, parts=64)
            enbt = ta("enbt", 512, parts=64)
            qin = ta("qin", 512, BF16, parts=64)
            kin = ta("kin", 512, BF16, parts=64)
            attn = ta("attn", 512, BF16)
            sg = ta("sg", 512)
            yg = ta("yg", 512)
            ygb = ta("ygb", 512, BF16)
            yglaT = ta("yglaT", 512, BF16)
            poolT = ta("poolT", 512, BF16)
            ypoolT = ta("ypoolT", 512, BF16)
            x1t = ta("x1t", 1024)
            h2T = ta("h2T", 1024, BF16)
            ssqo = ta("ssqo", 8)

            def load_x(kind, i, q):
                src = x_pre if kind == "pre" else x_own
                A("sp", lambda e: e.dma_start(out=xt[q][:], in_=src[i * 128:(i + 1) * 128, :]), w=["xt%d" % q], dma="xt%d" % q)

            def norm_T(src_ap, src_res, gs, sh, dst, dst_res, bank):
                pb = "b%d" % bank
                A("act", lambda e: e.activation(out=junk[:], in_=src_ap, func=AF.Square, accum_out=ssq[:, 0:1]), r=[src_res], w=["junk", "ssq"])
                A("act", lambda e: e.activation(out=ssq[:, 1:2], in_=ssq[:, 0:1], func=AF.Ln, scale=1.0 / D, bias=epsc[:, 0:1]), r=["ssq", "epsc"], w=["ssq1"])
                A("act", lambda e: e.activation(out=ssq[:, 2:3], in_=ssq[:, 1:2], func=AF.Exp, scale=-0.5), r=["ssq1"], w=["ssq2"])
                A("act", lambda e: e.activation(out=xnb[:], in_=src_ap, func=AF.Copy, scale=ssq[:, 2:3]), r=[src_res, "ssq2"], w=["xnb"])
                for c in range(8):
                    A("pe", lambda e, c=c: e.transpose(out=bkb(bank)[:, c * 128:(c + 1) * 128], in_=xnb[:, c * 128:(c + 1) * 128], identity=identb[:]),
                      r=["xnb", "identb"], w=[pb])
                for c in range(8):
                    eng = "dve" if c % 2 == 0 else "pool"
                    eng = "dve"
                    A(eng, lambda e, c=c: e.tensor_scalar(out=dst[:, c * 128:(c + 1) * 128], in0=bkb(bank)[:, c * 128:(c + 1) * 128],
                                                          scalar1=gs[:, c:c + 1], scalar2=sh[:, c:c + 1], op0=ALU.mult, op1=ALU.add),
                      r=[pb, "gs1", "gs2", "modT"], w=[dst_res])

            def sub1_tile(kind, i, gidx):
                own = kind == "own"
                q = gidx % 2
                last_pre = (kind == "pre" and i == NPRE - 1)
                if own:
                    B = dict(T=0, K=1, V=2, G=3, Pp=4, Q=5, KT=6, M=7)
                else:
                    o = 0
                    B = dict(T=o, K=o + 1, V=o + 2, M=o + 3, Pp=6)
                bn = {k: "b%d" % v for k, v in B.items()}
                xres = "xt%d" % q
                norm_T(xt[q][:], xres, gs1, sh1, hT, "hT", B["T"])
                hTv = hT[:].rearrange("p (c t) -> p c t", c=8)
                def proj(bank, c0, c1, o0=0):
                    for c in range(8):
                        A("pe", lambda e, c=c: e.matmul(bk(bank)[:, o0:o0 + (c1 - c0)], lhsT=hTv[:, c, :], rhs=winb[:, c, c0:c1], start=(c == 0), stop=(c == 7)),
                          r=["hT", "winb"], w=["b%d" % bank])
                proj(B["K"], 256, 512)
                proj(B["V"], 512, 1024)
                if own:
                    proj(B["G"], 1024, 1536)
                if own or last_pre:
                    proj(B["Pp"], 1536, 2048)
                for c in range(8):
                    A("pe", lambda e, c=c: e.matmul(bk(B["M"])[0:16, 0:128], lhsT=winb[:, c, 2048:2064], rhs=hTv[:, c, :], start=(c == 0), stop=(c == 7)),
                      r=["hT", "winb"], w=[bn["M"]])
                if own:
                    for h in range(4):
                        for c in range(8):
                            A("pe", lambda e, c=c, h=h: e.matmul(bk(B["Q"])[0:64, h * 128:(h + 1) * 128], lhsT=winb[:, c, h * 64:(h + 1) * 64], rhs=hTv[:, c, :],
                                                                 start=(c == 0), stop=(c == 7)), r=["hT", "winb"], w=[bn["Q"]])
                    for h in range(4):
                        for c in range(8):
                            A("pe", lambda e, c=c, h=h: e.matmul(bk(B["KT"])[0:64, h * 128:(h + 1) * 128], lhsT=winb[:, c, 256 + h * 64:256 + (h + 1) * 64], rhs=hTv[:, c, :],
                                                                 start=(c == 0), stop=(c == 7)), r=["hT", "winb"], w=[bn["KT"]])
                A("act", lambda e: e.copy(out=alr[0:16, :], in_=bk(B["M"])[0:16, 0:128]), r=[bn["M"]], w=["alr"])
                A("pe", lambda e: e.matmul(bk(B["K"])[:, 256:512], lhsT=alr[:], rhs=gw[:], start=True, stop=True), r=["alr", "gw"], w=[bn["K"]])
                A("act", lambda e: e.activation(out=ect[:], in_=bk(B["K"])[:, 256:512], func=AF.Exp, scale=-1.0), r=[bn["K"]], w=["ect"])
                A("act", lambda e: e.activation(out=spt[:], in_=ect[:], func=AF.Ln, bias=1.0), r=["ect"], w=["spt"])
                A("pe", lambda e: e.matmul(bk(B["M"])[:, 128:384], lhsT=cs[:, C_TRIU:C_TRIU + 128], rhs=spt[:], start=True, stop=True), r=["cs", "spt"], w=[bn["M"]])
                for h in range(4):
                    A("pe", lambda e, h=h: e.matmul(bk(B["M"])[0:64, 384 + h:385 + h], lhsT=spt[:, h * 64:(h + 1) * 64], rhs=cs[:, C_NEG:C_NEG + 1], start=True, stop=True),
                      r=["cs", "spt"], w=[bn["M"]])
                if own:
                    for h in range(4):
                        A("pe", lambda e, h=h: e.matmul(bk(B["T"])[0:64, h * 128:(h + 1) * 128], lhsT=spt[:, h * 64:(h + 1) * 64], rhs=cs[:, C_TRIL:C_TRIL + 128], start=True, stop=True),
                          r=["cs", "spt"], w=[bn["T"]])
                A("act", lambda e: e.activation(out=ect[:], in_=bk(B["M"])[:, 128:384], func=AF.Exp), r=[bn["M"]], w=["ect"])
                A("dve", lambda e: e.tensor_tensor(out=kst[:], in0=bk(B["K"])[:, 0:256], in1=ect[:], op=ALU.mult), r=[bn["K"], "ect"], w=["kst"])
                A("act", lambda e: e.activation(out=dec[:], in_=bk(B["M"])[0:64, 384:388], func=AF.Exp), r=[bn["M"]], w=["dec"])
                if own:
                    A("act", lambda e: e.copy(out=vbf[:], in_=bk(B["V"])), r=[bn["V"]], w=["vbf"])
                else:
                    A("act", lambda e: e.activation(out=vbf[:], in_=bk(B["V"]), func=AF.Copy, scale=cs2[:, 512 + i:513 + i]), r=[bn["V"], "cs2"], w=["vbf"])
                if own:
                    A("act", lambda e: e.activation(out=ebt[:], in_=bk(B["T"])[0:64, :], func=AF.Exp), r=[bn["T"]], w=["ebt"])
                    A("act", lambda e: e.activation(out=enbt[:], in_=bk(B["T"])[0:64, :], func=AF.Exp, scale=-1.0), r=[bn["T"]], w=["enbt"])
                    A("dve", lambda e: e.scalar_tensor_tensor(out=qin[:], in0=bk(B["Q"])[0:64, :], scalar=0.125, in1=ebt[:], op0=ALU.mult, op1=ALU.mult),
                      r=[bn["Q"], "ebt"], w=["qin"])
                    A("dve", lambda e: e.tensor_tensor(out=kin[:], in0=bk(B["KT"])[0:64, :], in1=enbt[:], op=ALU.mult), r=[bn["KT"], "enbt"], w=["kin"])
                    for h in range(4):
                        A("pe", lambda e, h=h: e.matmul(bk(B["M"])[:, h * 128:(h + 1) * 128], lhsT=kin[:, h * 128:(h + 1) * 128], rhs=qin[:, h * 128:(h + 1) * 128], start=True, stop=True),
                          r=["kin", "qin"], w=[bn["M"]])
                    A("dve", lambda e: e.tensor_tensor(out=attn[:].rearrange("p (h i) -> p h i", h=4), in0=bk(B["M"]).rearrange("p (h i) -> p h i", h=4),
                                                       in1=cs[:, C_CAUS:C_CAUS + 128].unsqueeze(1).to_broadcast([128, 4, 128]), op=ALU.mult),
                      r=[bn["M"], "cs"], w=["attn"])
                    for h in range(4):
                        A("pe", lambda e, h=h: e.matmul(bk(B["Q"])[:, h * 128:(h + 1) * 128], lhsT=attn[:, h * 128:(h + 1) * 128], rhs=vbf[:, h * 128:(h + 1) * 128], start=True, stop=False),
                          r=["attn", "vbf"], w=[bn["Q"]])
                        A("pe", lambda e, h=h: e.matmul(bk(B["Q"])[:, h * 128:(h + 1) * 128], lhsT=qin[:, h * 128:(h + 1) * 128], rhs=stateb[:, h * 128:(h + 1) * 128], start=False, stop=True),
                          r=["qin", "stateb"], w=[bn["Q"]])
                KVb = B["KT"] if own else B["T"]
                for h in range(4):
                    A("pe", lambda e, h=h: e.matmul(bk(KVb)[0:64, h * 128:(h + 1) * 128], lhsT=kst[:, h * 64:(h + 1) * 64], rhs=vbf[:, h * 128:(h + 1) * 128], start=True, stop=True),
                      r=["kst", "vbf"], w=["b%d" % KVb])
                for h in range(4):
                    A("dve", lambda e, h=h: e.scalar_tensor_tensor(out=state[:, h * 128:(h + 1) * 128], in0=state[:, h * 128:(h + 1) * 128], scalar=dec[:, h:h + 1],
                                                                   in1=bk(KVb)[0:64, h * 128:(h + 1) * 128], op0=ALU.mult, op1=ALU.add),
                      r=["b%d" % KVb, "dec", "state"], w=["state"])
                A("act", lambda e: e.copy(out=stateb[:], in_=state[:]), r=["state"], w=["stateb"])
                if last_pre:
                    A("act", lambda e: e.activation(out=pcur[1][:], in_=bk(B["Pp"]), func=AF.Copy, scale=cs2[:, 512 + i:513 + i]), r=[bn["Pp"], "cs2"], w=["pcur1"])
                if not own:
                    return
                for h in range(4):
                    A("act", lambda e, h=h: e.activation(out=junk[:, 0:128], in_=bk(B["Q"])[:, h * 128:(h + 1) * 128], func=AF.Square, accum_out=ssqo[:, h:h + 1]),
                      r=[bn["Q"]], w=["junk", "ssqo"])
                A("act", lambda e: e.activation(out=ssqo[:, 4:8], in_=ssqo[:, 0:4], func=AF.Ln, scale=1.0 / 128, bias=epsc[:, 0:1]), r=["ssqo", "epsc"], w=["ssqo1"])
                A("act", lambda e: e.activation(out=ssqo[:, 0:4], in_=ssqo[:, 4:8], func=AF.Exp, scale=-0.5), r=["ssqo1"], w=["ssqo"])
                A("act", lambda e: e.activation(out=sg[:], in_=bk(B["G"]), func=AF.Silu), r=[bn["G"]], w=["sg"])
                for h in range(4):
                    A("dve", lambda e, h=h: e.scalar_tensor_tensor(out=yg[:, h * 128:(h + 1) * 128], in0=bk(B["Q"])[:, h * 128:(h + 1) * 128], scalar=ssqo[:, h:h + 1],
                                                                   in1=sg[:, h * 128:(h + 1) * 128], op0=ALU.mult, op1=ALU.mult),
                      r=[bn["Q"], "ssqo", "sg"], w=["yg"])
                A("dve", lambda e: e.tensor_tensor(out=ygb[:].rearrange("p (h v) -> p h v", h=4), in0=yg[:].rearrange("p (h v) -> p h v", h=4),
                                                   in1=gngr[:].unsqueeze(1).to_broadcast([128, 4, 128]), op=ALU.mult), r=["yg", "gngr"], w=["ygb"])
                for h in range(4):
                    A("pe", lambda e, h=h: e.transpose(out=bkb(B["T"])[:, h * 128:(h + 1) * 128], in_=ygb[:, h * 128:(h + 1) * 128], identity=identb[:]),
                      r=["ygb", "identb"], w=[bn["T"]])
                A("act", lambda e: e.copy(out=yglaT[:], in_=bkb(B["T"])[:, 0:512]), r=[bn["T"]], w=["yglaT"])
                pc = i % 2
                pp = 1 - pc
                A("act", lambda e: e.copy(out=pcur[pc][:], in_=bk(B["Pp"])), r=[bn["Pp"]], w=["pcur%d" % pc])
                for g in range(4):
                    ba = cs2[:, g * 128:(g + 1) * 128] if i == 0 else cs[:, C_BA + g * 128:C_BA + (g + 1) * 128]
                    A("pe", lambda e, g=g, ba=ba: e.matmul(bk(B["K"])[:, g * 128:(g + 1) * 128], lhsT=pcur[pc][:, g * 128:(g + 1) * 128], rhs=ba, start=True, stop=False),
                      r=["pcur%d" % pc, "cs", "cs2"], w=[bn["K"]])
                    A("pe", lambda e, g=g: e.matmul(bk(B["K"])[:, g * 128:(g + 1) * 128], lhsT=pcur[pp][:, g * 128:(g + 1) * 128], rhs=cs[:, C_BB + g * 128:C_BB + (g + 1) * 128], start=False, stop=True),
                      r=["pcur%d" % pp, "cs"], w=[bn["K"]])
                A("act", lambda e: e.copy(out=poolT[:], in_=bk(B["K"])), r=[bn["K"]], w=["poolT"])
                for g in range(4):
                    A("pe", lambda e, g=g: e.matmul(bk(B["Pp"])[:, g * 128:(g + 1) * 128], lhsT=pwb[:, g * 128:(g + 1) * 128], rhs=poolT[:, g * 128:(g + 1) * 128], start=True, stop=True),
                      r=["pwb", "poolT"], w=[bn["Pp"]])
                for g in range(4):
                    A("dve", lambda e, g=g: e.tensor_scalar(out=ypoolT[:, g * 128:(g + 1) * 128], in0=bk(B["Pp"])[:, g * 128:(g + 1) * 128], scalar1=pscs[:, g:g + 1], scalar2=None, op0=ALU.mult),
                      r=[bn["Pp"], "pscs"], w=["ypoolT"])
                for nb in range(2):
                    bank = B["V"] if nb == 0 else B["G"]
                    for c in range(8):
                        lt = yglaT[:, c * 128:(c + 1) * 128] if c < 4 else ypoolT[:, (c - 4) * 128:(c - 3) * 128]
                        A("pe", lambda e, c=c, lt=lt, bank=bank, nb=nb: e.matmul(bk(bank), lhsT=lt, rhs=woutb[:, c, nb * 512:(nb + 1) * 512], start=(c == 0), stop=(c == 7)),
                          r=["yglaT", "ypoolT", "woutb"], w=["b%d" % bank])
                for nb in range(2):
                    bank = B["V"] if nb == 0 else B["G"]
                    A("dve", lambda e, nb=nb, bank=bank: e.tensor_tensor(out=x1t[:, nb * 512:(nb + 1) * 512], in0=bk(bank), in1=gate1[:, nb * 512:(nb + 1) * 512], op=ALU.mult),
                      r=["b%d" % bank, "gate1"], w=["x1t"])
                A("dve", lambda e: e.tensor_tensor(out=x1t[:], in0=x1t[:], in1=xt[q][:], op=ALU.add), r=["x1t", xres], w=["x1t"])
                A("sp", lambda e: e.dma_start(out=x1s[i * 128:(i + 1) * 128, :], in_=x1t[:]), r=["x1t"], w=["x1s"], dma="sx1")
                if dbg:
                    A("sp", lambda e: e.dma_start(out=dbg_out["d_x1"][i * 128:(i + 1) * 128, :], in_=x1t[:]), r=["x1t"], w=["d_x1"], dma="dbg")
                norm_T(x1t[:], "x1t", gs2, sh2, h2T, "h2T", B["T"])
                A("sp", lambda e: e.dma_start(out=h2s[i], in_=h2T[:]), r=["h2T"], w=["h2s"], dma="sh2")

            for i in range(NT):
                if i + 1 < NT:
                    load_x("own", i + 1, (i + 1) % 2)
                sub1_tile("own", i, i)

        WT = sb("WT", NTOK, BF16)
        I1T = sb("I1T", NTOK, BF16)
        I2T = sb("I2T", NTOK, BF16)
        if "route" in phases:
            phase_start()
            stage = ta("stage", 4096)
            wqb = arena[:, 0:8 * 2048].rearrange("p (c n) -> p c n", c=8)
            wqv = wq.rearrange("(c p) n -> p c n", p=128)
            for c in range(8):
                hs = slice((c % 2) * 2048, (c % 2 + 1) * 2048)
                A("sp", lambda e: e.dma_start(out=stage[:, hs], in_=wqv[:, c, :]), w=["stageh%d" % (c % 2)], dma="stageh%d" % (c % 2))
                A("dve", lambda e: e.tensor_copy(out=wqb[:, c, :], in_=stage[:, hs]), r=["stageh%d" % (c % 2)], w=["wqb"])
            kT = ta("kT", 256, BF16)
            for hi, kk in enumerate((k1, k2)):
                A("sp", lambda e, kk=kk: e.dma_start(out=stage[:, 0:128], in_=kk), w=["stage", "stageh0"], dma="stage")
                A("pe", lambda e: e.transpose(out=bk(0)[:, 0:128], in_=stage[:, 0:128], identity=ident), r=["stage", "cs"], w=["b0"])
                A("dve", lambda e, hi=hi: e.tensor_copy(out=kT[:, hi * 128:(hi + 1) * 128], in_=bk(0)[:, 0:128]), r=["b0"], w=["kT"])
            h2t = [ta("h2t%d" % i, 1024, BF16) for i in range(2)]
            qpT = ta("qpT", 2048, BF16)
            ssb = ta("ssb", 2048)
            swk = ta("swk", 2048)
            vv = ta("vv", 256)
            idx = ta("idx", 256, U32)
            cand = ta("cand", 2048)
            cwk = ta("cwk", 2048)
            tsv = ta("tsv", 128)
            pos = ta("pos", 128, U32)
            wg = ta("wg", 128)
            zz = ta("zz", 16)
            ai = ta("ai", 128, I32)
            bi_ = ta("bi", 128, I32)
            af = ta("af", 128)
            bf = ta("bf", 128)
            i1f = ta("i1f", 128)
            i2f = ta("i2f", 128)
            oh = ta("oh", 2048)
            isel = ta("isel", 256)

            def load_h2(n):
                q = n % 2
                A("sp", lambda e: e.dma_start(out=h2t[q][:], in_=h2s[n]), r=["h2s"], w=["h2t%d" % q], dma="h2t%d" % q)
            prep_tile = make_prep(7, "sp") if "prep" in phases else None
            ssb2 = [ssb, stage[:, 0:2048]]
            sbank = lambda j: (4 + j // 4) if j < 12 else 3

            def route_front(n):
                q = n % 2
                ssb_ = ssb2[q]
                hv = h2t[q][:].rearrange("p (c t) -> p c t", c=8)
                if dbg:
                    A("sp", lambda e: e.dma_start(out=dbg_out["d_h2"][n], in_=h2t[q][:]), r=["h2t%d" % q], w=["d_h2"], dma="dbg3")
                for j in range(16):
                    for c in range(8):
                        A("pe", lambda e: e.matmul(bk(j // 4)[:, (j % 4) * 128:(j % 4 + 1) * 128], lhsT=wqb[:, c, j * 128:(j + 1) * 128], rhs=hv[:, c, :], start=(c == 0), stop=(c == 7)),
                          r=["wqb", "h2t%d" % q], w=["b%d" % (j // 4)])
                for b4 in range(4):
                    A("act", lambda e: e.copy(out=qpT[:, b4 * 512:(b4 + 1) * 512], in_=bk(b4)), r=["b%d" % b4], w=["qpT%d" % b4])
                for j in range(16):
                    A("pe", lambda e: e.matmul(bk(sbank(j))[:, (j % 4) * 128:(j % 4 + 1) * 128], lhsT=qpT[:, j * 128:(j + 1) * 128], rhs=kT[:, (j % 2) * 128:(j % 2 + 1) * 128], start=True, stop=True),
                      r=["qpT%d" % (j // 4), "kT"], w=["b%d" % sbank(j)])
                for b4 in range(4):
                    A("act", lambda e: e.copy(out=ssb_[:, b4 * 512:(b4 + 1) * 512], in_=bk(sbank(b4 * 4))), r=["b%d" % sbank(b4 * 4)], w=["ssb%d_%d" % (q, b4)])
                if n + 2 < NT:
                    load_h2(n + 2)

            vvb = [vv, ta("vv_1", 256)]
            idxb = [idx, ta("idx_1", 256, U32)]
            tsvb = [tsv, ta("tsv_1", 128)]
            posb = [pos, ta("pos_1", 128, U32)]

            def back_body(n, lists):
                q = n % 2
                rp = "p%d_" % q
                ssb = ssb2[q]
                vv, idx, tsv, pos = vvb[q], idxb[q], tsvb[q], posb[q]
                cap[0] = lists[0]
                for ph in range(5):
                    for j in range(16):
                        sl = slice(j * 128, (j + 1) * 128)
                        sr = "ssb%d_%d" % (q, j // 4)
                        vr = rp + "vv%d" % j
                        if ph == 0:
                            A("dve", lambda e: e.max(out=vv[:, j * 16:j * 16 + 8], in_=ssb[:, sl]), r=[sr], w=[vr + "a"])
                        elif ph == 1:
                            A("dve", lambda e: e.max_index(out=idx[:, j * 16:j * 16 + 8], in_max=vv[:, j * 16:j * 16 + 8], in_values=ssb[:, sl]), r=[sr, vr + "a"], w=[rp + "idx%da" % j])
                        elif ph == 2:
                            A("dve", lambda e: e.match_replace(out=swk[:, sl], in_to_replace=vv[:, j * 16:j * 16 + 8], in_values=ssb[:, sl], imm_value=-1e30), r=[sr, vr + "a"], w=["swk%d" % j])
                        elif ph == 3:
                            A("dve", lambda e: e.max(out=vv[:, j * 16 + 8:j * 16 + 16], in_=swk[:, sl]), r=["swk%d" % j], w=[vr + "b"])
                        else:
                            A("dve", lambda e: e.max_index(out=idx[:, j * 16 + 8:j * 16 + 16], in_max=vv[:, j * 16 + 8:j * 16 + 16], in_values=swk[:, sl]), r=["swk%d" % j, vr + "b"], w=[rp + "idx%db" % j])
                vvall = [rp + "vv%da" % j for j in range(16)] + [rp + "vv%db" % j for j in range(16)]
                idxall = [rp + "idx%da" % j for j in range(16)] + [rp + "idx%db" % j for j in range(16)]
                vv4 = vv[:].rearrange("p (h s a) -> p h s a", h=8, s=2)
                A("dve", lambda e: e.tensor_tensor(out=cand[:].rearrange("p (h a b) -> p h a b", h=8, a=16),
                                                   in0=vv4[:, :, 0, :].unsqueeze(3).to_broadcast([128, 8, 16, 16]),
                                                   in1=vv4[:, :, 1, :].unsqueeze(2).to_broadcast([128, 8, 16, 16]), op=ALU.add), r=vvall, w=["cand"])
                for ph in range(5):
                    for h in range(8):
                        sl = slice(h * 256, (h + 1) * 256)
                        tr = rp + "tsv%d" % h
                        if ph == 0:
                            A("dve", lambda e: e.max(out=tsv[:, h * 16:h * 16 + 8], in_=cand[:, sl]), r=["cand"], w=[tr + "a"])
                        elif ph == 1:
                            A("dve", lambda e: e.max_index(out=pos[:, h * 16:h * 16 + 8], in_max=tsv[:, h * 16:h * 16 + 8], in_values=cand[:, sl]), r=["cand", tr + "a"], w=[rp + "pos%da" % h])
                        elif ph == 2:
                            A("dve", lambda e: e.match_replace(out=cwk[:, sl], in_to_replace=tsv[:, h * 16:h * 16 + 8], in_values=cand[:, sl], imm_value=-1e30), r=["cand", tr + "a"], w=["cwk%d" % h])
                        elif ph == 3:
                            A("dve", lambda e: e.max(out=tsv[:, h * 16 + 8:h * 16 + 16], in_=cwk[:, sl]), r=["cwk%d" % h], w=[tr + "b"])
                        else:
                            A("dve", lambda e: e.max_index(out=pos[:, h * 16 + 8:h * 16 + 16], in_max=tsv[:, h * 16 + 8:h * 16 + 16], in_values=cwk[:, sl]), r=["cwk%d" % h, tr + "b"], w=[rp + "pos%db" % h])
                tsvall = [rp + "tsv%da" % h for h in range(8)] + [rp + "tsv%db" % h for h in range(8)]
                posall = [rp + "pos%da" % h for h in range(8)] + [rp + "pos%db" % h for h in range(8)]
                cap[0] = lists[1]
                ts3 = tsv[:].rearrange("p (h k) -> p h k", h=8)
                A("dve", lambda e: e.tensor_tensor(out=wg[:].rearrange("p (h k) -> p h k", h=8), in0=ts3, in1=ts3[:, :, 0:1].to_broadcast([128, 8, 16]), op=ALU.subtract), r=tsvall, w=["wg"])
                A("act", lambda e: e.activation(out=wg[:], in_=wg[:], func=AF.Exp), r=["wg"], w=["wg"])
                A("dve", lambda e: e.tensor_reduce(out=zz[:, 0:8], in_=wg[:].rearrange("p (h k) -> p h k", h=8), axis=AX.X, op=ALU.add), r=["wg"], w=["zz"])
                A("dve", lambda e: e.reciprocal(out=zz[:, 8:16], in_=zz[:, 0:8]), r=["zz"], w=["zz1"])
                A("dve", lambda e: e.tensor_tensor(out=wg[:].rearrange("p (h k) -> p h k", h=8), in0=wg[:].rearrange("p (h k) -> p h k", h=8),
                                                   in1=zz[:, 8:16].unsqueeze(2).to_broadcast([128, 8, 16]), op=ALU.mult), r=["wg", "zz1"], w=["wg"])
                A("dve", lambda e: e.tensor_single_scalar(out=ai[:], in_=pos[:].bitcast(I32), scalar=4, op=ALU.logical_shift_right), r=posall, w=["ai"])
                A("dve", lambda e: e.tensor_single_scalar(out=bi_[:], in_=pos[:].bitcast(I32), scalar=15, op=ALU.bitwise_and), r=posall, w=["bi"])
                A("dve", lambda e: e.tensor_copy(out=af[:], in_=ai[:]), r=["ai"], w=["af"])
                A("dve", lambda e: e.tensor_copy(out=bf[:], in_=bi_[:]), r=["bi"], w=["bf"])
                idx4 = idx[:].rearrange("p (h s a) -> p h s a", h=8, s=2)
                A("dve", lambda e: e.tensor_copy(out=i1f[:].rearrange("p (h a) -> p h a", h=8), in_=idx4[:, :, 0, :]), r=idxall, w=["i1f"])
                A("dve", lambda e: e.tensor_copy(out=i2f[:].rearrange("p (h a) -> p h a", h=8), in_=idx4[:, :, 1, :]), r=idxall, w=["i2f"])
                io16 = cs[:, C_IOTA16:C_IOTA16 + 16]
                for which, (cf, inf) in enumerate(((af, i1f), (bf, i2f))):
                    A("dve", lambda e, cf=cf: e.tensor_tensor(out=oh[:].rearrange("p (s a) -> p s a", a=16),
                                                              in0=io16.unsqueeze(1).to_broadcast([128, 128, 16]),
                                                              in1=cf[:].unsqueeze(2).to_broadcast([128, 128, 16]), op=ALU.is_equal), r=["cs", "af", "bf"], w=["oh"])
                    A("dve", lambda e, inf=inf: e.tensor_tensor(out=oh[:].rearrange("p (h k a) -> p h k a", h=8, k=16),
                                                                in0=oh[:].rearrange("p (h k a) -> p h k a", h=8, k=16),
                                                                in1=inf[:].rearrange("p (h a) -> p h a", h=8).unsqueeze(2).to_broadcast([128, 8, 16, 16]), op=ALU.mult),
                      r=["oh", "i1f", "i2f"], w=["oh"])
                    A("dve", lambda e, which=which: e.tensor_reduce(out=isel[:, which * 128:(which + 1) * 128], in_=oh[:].rearrange("p (s a) -> p s a", a=16), axis=AX.X, op=ALU.add),
                      r=["oh"], w=["isel"])
                for which, (src, srcres, dst) in enumerate(((wg[:], "wg", WT), (isel[:, 0:128], "isel", I1T), (isel[:, 128:256], "isel", I2T))):
                    A("pe", lambda e, src=src, which=which: e.transpose(out=bk(7)[:, which * 128:(which + 1) * 128], in_=src, identity=ident), r=[srcres, "cs"], w=["b7"])
                for which, dst in enumerate((WT, I1T, I2T)):
                    A("act", lambda e, which=which, dst=dst, n=n: e.copy(out=dst[:, n * 128:(n + 1) * 128], in_=bk(7)[:, which * 128:(which + 1) * 128]), r=["b7"], w=["rt%d" % which])
                if dbg and n == 0:
                    A("sp", lambda e: e.dma_start(out=dbg_out["d_qp"], in_=qpT[:]), r=["qpT0", "qpT1", "qpT2", "qpT3"], w=["d_qp"], dma="dbg4")
                    offs = 0
                    for nm, src_, res_ in (("vv", vv, "vv"), ("i1f", i1f, "i1f"), ("i2f", i2f, "i2f"), ("af", af, "af"), ("bf", bf, "bf"), ("isel", isel, "isel"), ("wg", wg, "wg"), ("tsv", tsv, "tsv")):
                        w_ = src_.shape[1]
                        A("sp", lambda e, src_=src_, offs=offs, w_=w_: e.dma_start(out=dbg_out["d_misc"][:, offs:offs + w_], in_=src_[:]), r=[res_], w=["d_misc"], dma="dbg2")
                        offs += w_
                    A("sp", lambda e: e.dma_start(out=dbg_out["d_misc"][:, 1280:1536], in_=ssb[:, 0:256]), r=["ssb0_0"], w=["d_misc"], dma="dbg2")
                cap[0] = None

            def run_merged(ops1, ops2):
                for t in ops2:
                    t()
                for t in ops1:
                    t()

            load_h2(0)
            load_h2(1)
            route_front(0)
            if NT > 1:
                route_front(1)
            L0 = [[], []]
            back_body(0, L0)
            run_merged(L0[0], [])
            pend2 = L0[1]
            for n in range(NT):
                if n + 2 < NT:
                    route_front(n + 2)
                if prep_tile is not None:
                    for pi in range(8):
                        prep_tile(n * 8 + pi)
                if n + 1 < NT:
                    Ln = [[], []]
                    back_body(n + 1, Ln)
                    run_merged(Ln[0], pend2)
                    pend2 = Ln[1]
                else:
                    run_merged([], pend2)
            if dbg:
                rtf = cand
                for which, dst in enumerate((WT, I1T, I2T)):
                    A("dve", lambda e, dst=dst: e.tensor_copy(out=rtf[:], in_=dst[:]), r=["rt%d" % which], w=["rtf", "cand"])
                    A("sp", lambda e, which=which: e.dma_start(out=dbg_out["d_rt"][which], in_=rtf[:]), r=["rtf"], w=["d_rt"], dma="dbg")

        if "dense" in phases:
            phase_start()
            Gt = arena[:, 0:NE * TT].rearrange("p (t i) -> p t i", i=NE)
            NB = 3
            utg = [ta("utg%d" % i, GRP * 1024, BF16) for i in range(NB)]
            vg = [ta("vg%d" % i, GRP * 1024, BF16) for i in range(NB)]
            h2tt = ta("h2tt", 8 * TT, BF16)
            OB = 16
            NOH = 6
            ohA = [ta("ohA%d" % i, OB * 128, BF16) for i in range(NOH)]
            ohB = [ta("ohB%d" % i, OB * 128, BF16) for i in range(NOH)]
            gcount = [0]
            iorep = ta("iorep", OB * 128, BF16)
            A("dve", lambda e: e.tensor_copy(out=iorep[:].rearrange("p (i t) -> p i t", t=OB), in_=iotab[:].unsqueeze(2).to_broadcast([128, 128, OB])), r=["iotab"], w=["iorep"])
            gl = [ta("gl%d" % i, TT, BF16) for i in range(2)]
            ga = [ta("ga%d" % i, TT, BF16) for i in range(2)]
            x1l = ta("x1l", 1024)
            x2 = ta("x2", 1024)
            ot = x2
            junk2 = ta("junk2", 1024, BF16)
            ss2 = ta("ss2", 4)
            NG = NE // GRP

            def load_w(T, g, cnt):
                q = cnt % NB
                A("sp", lambda e: e.dma_start(out=utg[q][:].rearrange("p (q n) -> p q n", q=GRP), in_=UTs[g * GRP:(g + 1) * GRP].rearrange("q p n -> p q n")),
                  r=["UTs"], w=["utg%d" % q], dma="utg%d" % q)
                A("sp", lambda e: e.dma_start(out=vg[q][:].rearrange("p (q n) -> p q n", q=GRP), in_=Vbs[g * GRP:(g + 1) * GRP].rearrange("q p n -> p q n")),
                  r=["Vbs"], w=["vg%d" % q], dma="vg%d" % q)

            allg = [(T, g) for T in range(NTOK // TT) for g in range(NG)]
            cnt_load = 0
            for _ in range(NB - 1):
                load_w(allg[cnt_load][0], allg[cnt_load][1], cnt_load)
                cnt_load += 1
            cnt_use = 0
            first_g = True
            for T in range(NTOK // TT):
                t0 = T * TT
                for sub in range(TT // 128):
                    A("sp", lambda e, sub=sub: e.dma_start(out=h2tt[:].rearrange("p (c t) -> p c t", c=8)[:, :, sub * 128:(sub + 1) * 128],
                                                           in_=h2s[T * (TT // 128) + sub].rearrange("p (c t) -> p c t", c=8)), r=["h2s"], w=["h2tt"], dma="h2tt")
                def g_dve(Tn, ob, parts=(0, 1, 2)):
                    tk = Tn * TT + ob * OB
                    k = ob % NOH
                    oa = ohA[k][:].rearrange("p (i t) -> p i t", t=OB)
                    obv = ohB[k][:].rearrange("p (i t) -> p i t", t=OB)
                    if 0 in parts:
                        A("dve", lambda e: e.tensor_tensor(out=oa, in0=iorep[:].rearrange("p (i t) -> p i t", t=OB),
                                                           in1=I1T[:, tk:tk + OB].unsqueeze(1).to_broadcast([128, 128, OB]), op=ALU.is_equal),
                          r=["iorep", "rt1"], w=["ohA%d" % k])
                    if 1 in parts:
                        A("dve", lambda e: e.tensor_tensor(out=oa, in0=oa, in1=WT[:, tk:tk + OB].unsqueeze(1).to_broadcast([128, 128, OB]), op=ALU.mult),
                          r=["ohA%d" % k, "rt0"], w=["ohA%d" % k])
                    if 2 in parts:
                        A("dve", lambda e: e.tensor_tensor(out=obv, in0=iorep[:].rearrange("p (i t) -> p i t", t=OB),
                                                           in1=I2T[:, tk:tk + OB].unsqueeze(1).to_broadcast([128, 128, OB]), op=ALU.is_equal),
                          r=["iorep", "rt2"], w=["ohB%d" % k])

                def g_pe(ob):
                    k = ob % NOH
                    oa = ohA[k][:].rearrange("p (i t) -> p i t", t=OB)
                    obv = ohB[k][:].rearrange("p (i t) -> p i t", t=OB)
                    for t4 in range(OB // 4):
                        gb = 4 + (gcount[0] % 4)
                        gcount[0] += 1
                        for tt in range(4):
                            tl = t4 * 4 + tt
                            A("pe", lambda e: e.matmul(bk(gb)[:, tt * 128:(tt + 1) * 128], lhsT=obv[:, :, tl], rhs=oa[:, :, tl], start=True, stop=True),
                              r=["ohA%d" % k, "ohB%d" % k], w=["b%d" % gb])
                        tl0 = ob * OB + t4 * 4
                        dstv = Gt[:, tl0:tl0 + 4, :]
                        A("act", lambda e: e.copy(out=dstv, in_=bk(gb).rearrange("p (t i) -> p t i", t=4)), r=["b%d" % gb], w=["G"])

                NOB = TT // OB
                if T == 0:
                    for ob in range(NOH):
                        g_dve(0, ob)
                for ob in range(NOB):
                    g_pe(ob)
                    if ob + NOH < NOB:
                        g_dve(T, ob + NOH)
                early = {}
                for jj in range(NOH * 3):
                    early[NE - 2 - 2 * (NOH * 3 - 1 - jj)] = (jj // 3, jj % 3)
                h2v = h2tt[:].rearrange("p (c t) -> p c t", c=8)

                def scores(i1, q, ql):
                    sbk = 4 + (i1 % 2)
                    uv = utg[q][:].rearrange("p (q c e) -> p q c e", q=GRP, c=8)
                    for c in range(8):
                        A("pe", lambda e, c=c: e.matmul(bk(sbk)[:, 0:TT], lhsT=uv[:, ql, c, :], rhs=h2v[:, c, :], start=(c == 0), stop=(c == 7)),
                          r=["utg%d" % q, "h2tt"], w=["b%d" % sbk])
                    A("act", lambda e: e.activation(out=gl[i1 % 2][:], in_=bk(sbk)[:, 0:TT], func=AF.Gelu), r=["b%d" % sbk], w=["gl%d" % (i1 % 2)])
                    A("dve", lambda e: e.tensor_tensor(out=ga[i1 % 2][:], in0=gl[i1 % 2][:], in1=Gt[:, :, i1], op=ALU.mult), r=["gl%d" % (i1 % 2), "G"], w=["ga%d" % (i1 % 2)])

                def vmm(i1, q, ql):
                    vv_ = vg[q][:].rearrange("p (q n) -> p q n", q=GRP)
                    for th in range(TT // 128):
                        for dh in range(2):
                            A("pe", lambda e, th=th, dh=dh: e.matmul(bk(th * 2 + dh), lhsT=ga[i1 % 2][:, th * 128:(th + 1) * 128], rhs=vv_[:, ql, dh * 512:(dh + 1) * 512],
                                                                     start=(i1 == 0), stop=(i1 == NE - 1)),
                              r=["ga%d" % (i1 % 2), "vg%d" % q], w=["b%d" % (th * 2 + dh)])

                for g in range(NG):
                    q = cnt_use % NB
                    for ql in range(GRP):
                        i1 = g * GRP + ql
                        scores(i1, q, ql)
                        if i1 > 0:
                            pq = q if ql > 0 else (cnt_use - 1) % NB
                            vmm(i1 - 1, pq, (ql - 1) % GRP)
                        if ql == 0 and cnt_load < len(allg):
                            load_w(allg[cnt_load][0], allg[cnt_load][1], cnt_load)
                            cnt_load += 1
                        if T + 1 < NTOK // TT and i1 in early:
                            g_dve(T + 1, early[i1][0], parts=(early[i1][1],))
                    cnt_use += 1
                vmm(NE - 1, (cnt_use - 1) % NB, GRP - 1)
                for th in range(TT // 128):
                    r0 = t0 + th * 128
                    A("sp", lambda e, r0=r0: e.dma_start(out=x1l[:], in_=x1s[r0:r0 + 128, :]), r=["x1s"], w=["x1l"], dma="x1l")
                    for dh in range(2):
                        A("dve", lambda e, th=th, dh=dh: e.tensor_tensor(out=x2[:, dh * 512:(dh + 1) * 512], in0=bk(th * 2 + dh), in1=gate2[:, dh * 512:(dh + 1) * 512], op=ALU.mult),
                          r=["b%d" % (th * 2 + dh), "gate2"], w=["x2"])
                    A("pool", lambda e: e.tensor_tensor(out=x2[:], in0=x2[:], in1=x1l[:], op=ALU.add), r=["x2", "x1l"], w=["x2"])
                    A("act", lambda e: e.activation(out=junk2[:], in_=x2[:], func=AF.Square, accum_out=ss2[:, 0:1]), r=["x2"], w=["junk2", "ss2"])
                    A("act", lambda e: e.activation(out=ss2[:, 1:2], in_=ss2[:, 0:1], func=AF.Ln, scale=1.0 / D, bias=epsc[:, 0:1]), r=["ss2", "epsc"], w=["ss21"])
                    A("act", lambda e: e.activation(out=ss2[:, 2:3], in_=ss2[:, 1:2], func=AF.Exp, scale=-0.5), r=["ss21"], w=["ss22"])
                    A("dve", lambda e: e.scalar_tensor_tensor(out=ot[:], in0=x2[:], scalar=ss2[:, 2:3], in1=fgr[:], op0=ALU.mult, op1=ALU.mult), r=["x2", "ss22", "fgr"], w=["x2"])
                    A("sp", lambda e, r0=r0: e.dma_start(out=out[r0:r0 + 128, :], in_=ot[:]), r=["x2"], w=["out"], dma="sout")
        fin = ["out"] + list(dbg_out.keys())
        A("sp", None, r=fin)
        P.emit(st)
    return nc


_NC_CACHE = {}


def make_in_maps(inputs):
    f = lambda a: np.ascontiguousarray(np.asarray(a, dtype=np.float32))
    x = f(inputs["x"])
    c = f(inputs["c"])
    cst = make_consts()
    colT = lambda v, k: np.ascontiguousarray(v.reshape(k, 128).T)
    shared = {
        "cst": cst,
        "ada_w": f(inputs["ada_w"])[0],
        "ada_bT": colT(f(inputs["ada_b"])[0], 48),
        "ada_b": f(inputs["ada_b"])[0].reshape(1, 6 * D),
        "n1g": colT(f(inputs["norm1_g"])[0], 8),
        "n2g": colT(f(inputs["norm2_g"])[0], 8),
        "fg_rep": np.ascontiguousarray(np.broadcast_to(f(inputs["final_g"])[None, :], (128, D))),
        "w_in": f(inputs["w_in"])[0],
        "gw17": np.ascontiguousarray(np.concatenate([f(inputs["gla_gate_w"])[0], f(inputs["gla_gate_b"])[0][None, :]], axis=0)),
        "gng_rep": np.ascontiguousarray(np.broadcast_to(f(inputs["gla_norm_g"])[0][None, :], (128, 128))),
        "pool_w": f(inputs["pool_w"])[0],
        "pscT": colT(f(inputs["pool_scale"])[0], 4),
        "w_out": f(inputs["w_out"])[0],
        "wq": f(inputs["peer_wq"])[0],
        "k1": f(inputs["peer_k1"])[0],
        "k2": f(inputs["peer_k2"])[0],
        "pu": f(inputs["peer_u"])[0],
        "pv": f(inputs["peer_v"])[0],
    }
    maps = []
    for core in range(8):
        b, j = core // 4, core % 4
        m = dict(shared)
        m["x_own"] = np.ascontiguousarray(x[b, j * NTOK:(j + 1) * NTOK])
        xp = np.zeros((NPRE * 128, D), np.float32)
        npre = j * NTOK
        if npre > 0:
            xp[NPRE * 128 - npre:] = x[b, 0:npre]
        m["x_pre"] = xp
        m["cst2"] = make_core_consts(j)
        m["c_col"] = colT(c[b], 8)
        maps.append(m)
    return maps


def kernel(**inputs):
    if "nc" not in _NC_CACHE:
        _NC_CACHE["nc"] = build_nc()
    nc = _NC_CACHE["nc"]
    maps = make_in_maps(inputs)
    res = run_bass_kernel_spmd(nc, maps, core_ids=list(range(8)))
    out = np.zeros((2, 8192, D), np.float32)
    for core in range(8):
        b, j = core // 4, core % 4
        out[b, j * NTOK:(j + 1) * NTOK] = res.results[core]["out"]
    return out
```
